# Optimizing a Trainium2 kernel written in Bass

```python
import math
import jax, jax.numpy as jnp
from jax import lax
import numpy as np

D_MODEL = 2048
BATCH = 4
SEQ = 2048
DEPTH = 2

HEAD_DIM = 128
SB_HEADS = 8
SB_WIDTH = SB_HEADS * HEAD_DIM
MLA_HEADS = 8
MLA_Q_LORA = 512
MLA_KV_LORA = 256
MLA_NOPE = 128
MLA_ROPE = 64
MLA_V = 128
MLA_WIDTH = MLA_HEADS * MLA_V
EVEN_IN_WIDTH = 3 * SB_WIDTH + MLA_Q_LORA + MLA_KV_LORA + MLA_ROPE
EVEN_OUT_WIDTH = SB_WIDTH + MLA_WIDTH
RET_HEADS = 8
RET_QK = 256
RET_V = 512
RET_QK_WIDTH = RET_HEADS * RET_QK
RET_V_WIDTH = RET_HEADS * RET_V
ODD_IN_WIDTH = 2 * RET_QK_WIDTH + 2 * RET_V_WIDTH
N_GROUPS = 4
EXPERTS_PER_GROUP = 8
N_EXPERTS = N_GROUPS * EXPERTS_PER_GROUP
TOP_K_IN_GROUP = 2
EXPERT_HIDDEN = 512
BLOCK = 128
CHUNK = 128
ROPE_BASE = 10000.0
EPS = 1e-6

kernel_name = "hybrid_sbattn_mla_retnet_hmoe_adaln"


def rms_norm(x, g):
    xf = x.astype(jnp.float32)
    y = xf * lax.rsqrt(jnp.mean(xf * xf, axis=-1, keepdims=True) + EPS)
    return (y * g.astype(jnp.float32)).astype(x.dtype)


def rope(x, positions):
    d = x.shape[-1]
    half = d // 2
    inv_freq = jnp.exp(-math.log(ROPE_BASE) * jnp.arange(half, dtype=jnp.float32) / half)
    ang = positions.astype(jnp.float32)[..., None] * inv_freq
    ang = ang.reshape(ang.shape[:2] + (1,) * (x.ndim - 3) + (half,))
    cos, sin = jnp.cos(ang), jnp.sin(ang)
    xf = x.astype(jnp.float32)
    x1, x2 = xf[..., :half], xf[..., half:]
    return jnp.concatenate([x1 * cos - x2 * sin, x1 * sin + x2 * cos], axis=-1).astype(x.dtype)


def to_blocks(t, blk):
    B, S, H, d = t.shape
    return t.reshape(B, S // blk, blk, H, d).transpose(1, 0, 3, 2, 4)


def from_blocks(o):
    NB, B, H, blk, d = o.shape
    return o.transpose(1, 0, 3, 2, 4).reshape(B, NB * blk, H * d)


def adaln(cond, w_mod, b_mod):
    mod = (cond @ w_mod + b_mod)[:, None, :]
    shift, scale, gate = jnp.split(mod, 3, axis=-1)
    return shift, scale, gate


def stick_breaking_attention(q, k, v):
    B, S, H, dh = q.shape
    kh = k.transpose(0, 2, 1, 3)
    vh = v.transpose(0, 2, 1, 3)
    kpos = jnp.arange(S)
    inv_sqrt = dh ** -0.5

    def block(args):
        qb, start = args
        z = jnp.einsum("bhqd,bhkd->bhqk", qb, kh).astype(jnp.float32) * inv_sqrt
        qpos = start + jnp.arange(BLOCK)
        strict = kpos[None, :] < qpos[:, None]
        log_keep = jnp.where(strict, jax.nn.log_sigmoid(-z), 0.0)
        after = lax.cumsum(log_keep, axis=3, reverse=True) - log_keep
        a = jnp.where(strict, jnp.exp(jax.nn.log_sigmoid(z) + after), 0.0)
        return jnp.einsum("bhqk,bhkd->bhqd", a.astype(vh.dtype), vh)

    starts = jnp.arange(S // BLOCK, dtype=jnp.int32) * BLOCK
    o = lax.map(block, (to_blocks(q, BLOCK), starts))
    return from_blocks(o)


def mla_attention(q_nope, q_rope, k_nope, k_rope, v):
    B, S, H, _ = q_nope.shape
    kn = k_nope.transpose(0, 2, 1, 3)
    vh = v.transpose(0, 2, 1, 3)
    kpos = jnp.arange(S)
    scale = (MLA_NOPE + MLA_ROPE) ** -0.5

    def block(args):
        qn, qr, start = args
        z = (jnp.einsum("bhqd,bhkd->bhqk", qn, kn)
             + jnp.einsum("bhqr,bkr->bhqk", qr, k_rope)).astype(jnp.float32) * scale
        qpos = start + jnp.arange(BLOCK)
        causal = kpos[None, :] <= qpos[:, None]
        p = jax.nn.softmax(jnp.where(causal, z, -jnp.inf), axis=-1)
        return jnp.einsum("bhqk,bhkd->bhqd", p.astype(vh.dtype), vh)

    starts = jnp.arange(S // BLOCK, dtype=jnp.int32) * BLOCK
    o = lax.map(block, (to_blocks(q_nope, BLOCK), to_blocks(q_rope, BLOCK), starts))
    return from_blocks(o)


def hybrid_attention(h, positions, w_in, q_norm, w_q_up, kv_norm, w_kv_up, w_out):
    B, S, _ = h.shape
    proj = h @ w_in
    cuts = [SB_WIDTH, 2 * SB_WIDTH, 3 * SB_WIDTH,
            3 * SB_WIDTH + MLA_Q_LORA, 3 * SB_WIDTH + MLA_Q_LORA + MLA_KV_LORA]
    sb_q, sb_k, sb_v, c_q, c_kv, k_rope = jnp.split(proj, cuts, axis=-1)
    o_sb = stick_breaking_attention(sb_q.reshape(B, S, SB_HEADS, HEAD_DIM),
                                    sb_k.reshape(B, S, SB_HEADS, HEAD_DIM),
                                    sb_v.reshape(B, S, SB_HEADS, HEAD_DIM))
    q = (rms_norm(c_q, q_norm) @ w_q_up).reshape(B, S, MLA_HEADS, MLA_NOPE + MLA_ROPE)
    q_nope, q_rope = q[..., :MLA_NOPE], rope(q[..., MLA_NOPE:], positions)
    kv = (rms_norm(c_kv, kv_norm) @ w_kv_up).reshape(B, S, MLA_HEADS, MLA_NOPE + MLA_V)
    k_nope, v = kv[..., :MLA_NOPE], kv[..., MLA_NOPE:]
    k_rope = rope(k_rope, positions)
    o_mla = mla_attention(q_nope, q_rope, k_nope, k_rope, v)
    return jnp.concatenate([o_sb, o_mla], axis=-1) @ w_out


def retention(h, positions, w_in, w_out):
    B, S, _ = h.shape
    proj = h @ w_in
    q, k, v, g = jnp.split(proj, [RET_QK_WIDTH, 2 * RET_QK_WIDTH, 2 * RET_QK_WIDTH + RET_V_WIDTH], axis=-1)
    q = rope(q.reshape(B, S, RET_HEADS, RET_QK), positions)
    k = rope(k.reshape(B, S, RET_HEADS, RET_QK), positions) * (RET_QK ** -0.5)
    v = v.reshape(B, S, RET_HEADS, RET_V)

    log_gamma = jnp.log1p(-jnp.exp2(-5.0 - jnp.arange(RET_HEADS, dtype=jnp.float32)))
    idx = jnp.arange(CHUNK, dtype=jnp.float32)
    rel = idx[:, None] - idx[None, :]
    intra = jnp.where(rel >= 0, jnp.exp(jnp.maximum(rel, 0.0) * log_gamma[:, None, None]), 0.0)
    q_decay = jnp.exp((idx + 1.0) * log_gamma[:, None])
    k_decay = jnp.exp((CHUNK - 1.0 - idx) * log_gamma[:, None])
    chunk_decay = jnp.exp(CHUNK * log_gamma)

    def step(state, inputs):
        qc, kc, vc = (t.astype(jnp.float32) for t in inputs)
        scores = jnp.einsum("bhnd,bhmd->bhnm", qc, kc) * intra
        inner = jnp.einsum("bhnm,bhme->bhne", scores, vc)
        cross = jnp.einsum("bhnd,bhde->bhne", qc, state) * q_decay[None, :, :, None]
        new_state = (state * chunk_decay[None, :, None, None]
                     + jnp.einsum("bhmd,bhme->bhde", kc * k_decay[None, :, :, None], vc))
        return new_state, inner + cross

    state0 = jnp.zeros((B, RET_HEADS, RET_QK, RET_V), jnp.float32)
    _, o = lax.scan(step, state0, (to_blocks(q, CHUNK), to_blocks(k, CHUNK), to_blocks(v, CHUNK)))
    o = o.transpose(1, 0, 3, 2, 4).reshape(B, S, RET_HEADS, RET_V)
    o = o * lax.rsqrt(jnp.mean(o * o, axis=-1, keepdims=True) + EPS)
    o = o.reshape(B, S, RET_V_WIDTH)
    return (jax.nn.silu(g.astype(jnp.float32)) * o).astype(h.dtype) @ w_out


def hierarchical_moe(h, w_group, b_group, w_expert, b_expert, w_gate, w_up, w_down):
    B, S, D = h.shape
    ht = h.reshape(B * S, D)
    g_prob = jax.nn.softmax((ht @ w_group + b_group).astype(jnp.float32), axis=-1)
    g_w, g_idx = lax.top_k(g_prob, 1)
    e_logits = (ht @ w_expert + b_expert).astype(jnp.float32).reshape(-1, N_GROUPS, EXPERTS_PER_GROUP)
    sel = jnp.take_along_axis(e_logits, g_idx[:, :, None], axis=1)[:, 0]
    e_w, e_idx = lax.top_k(jax.nn.softmax(sel, axis=-1), TOP_K_IN_GROUP)
    e_w = e_w / jnp.sum(e_w, axis=-1, keepdims=True)
    weights = g_w * e_w
    flat_idx = g_idx * EXPERTS_PER_GROUP + e_idx
    combine = jnp.sum(jax.nn.one_hot(flat_idx, N_EXPERTS, dtype=jnp.float32) * weights[..., None], axis=1)
    combine = combine.astype(ht.dtype)
    out = jnp.zeros_like(ht)
    for e in range(N_EXPERTS):
        a = jax.nn.silu(ht @ w_gate[e]) * (ht @ w_up[e])
        out = out + combine[:, e:e + 1] * (a @ w_down[e])
    return out.reshape(B, S, D)


def setup_inputs(seed: int = 0) -> dict:
    key = jax.random.key(seed)
    ks = jax.random.split(key, 28)
    n_even = (DEPTH + 1) // 2
    n_odd = DEPTH // 2
    D = D_MODEL
    f32 = jnp.float32

    def nrm(k, shape, scale):
        return jax.random.normal(k, shape, f32) * scale

    x = nrm(ks[0], (BATCH, SEQ, D), 1.0)
    c = nrm(ks[1], (BATCH, D), 1.0)
    offsets = jax.random.randint(ks[2], (BATCH, 1), 0, 4096, dtype=jnp.int32)
    positions = offsets + jnp.arange(SEQ, dtype=jnp.int32)[None, :]
    return {
        "x": x,
        "c": c,
        "positions": positions,
        "w_mod_mix": nrm(ks[3], (DEPTH, D, 3 * D), 0.5 * D ** -0.5),
        "b_mod_mix": nrm(ks[4], (DEPTH, 3 * D), 0.02),
        "norm_mix": 1.0 + nrm(ks[5], (DEPTH, D), 0.05),
        "w_mod_ffn": nrm(ks[6], (DEPTH, D, 3 * D), 0.5 * D ** -0.5),
        "b_mod_ffn": nrm(ks[7], (DEPTH, 3 * D), 0.02),
        "norm_ffn": 1.0 + nrm(ks[8], (DEPTH, D), 0.05),
        "ev_w_in": nrm(ks[9], (n_even, D, EVEN_IN_WIDTH), D ** -0.5),
        "ev_q_norm": 1.0 + nrm(ks[10], (n_even, MLA_Q_LORA), 0.05),
        "ev_w_q_up": nrm(ks[11], (n_even, MLA_Q_LORA, MLA_HEADS * (MLA_NOPE + MLA_ROPE)), MLA_Q_LORA ** -0.5),
        "ev_kv_norm": 1.0 + nrm(ks[12], (n_even, MLA_KV_LORA), 0.05),
        "ev_w_kv_up": nrm(ks[13], (n_even, MLA_KV_LORA, MLA_HEADS * (MLA_NOPE + MLA_V)), MLA_KV_LORA ** -0.5),
        "ev_w_out": nrm(ks[14], (n_even, EVEN_OUT_WIDTH, D), EVEN_OUT_WIDTH ** -0.5),
        "od_w_in": nrm(ks[15], (n_odd, D, ODD_IN_WIDTH), D ** -0.5),
        "od_w_out": nrm(ks[16], (n_odd, RET_V_WIDTH, D), RET_V_WIDTH ** -0.5),
        "moe_w_group": nrm(ks[17], (DEPTH, D, N_GROUPS), D ** -0.5),
        "moe_b_group": nrm(ks[18], (DEPTH, N_GROUPS), 0.01),
        "moe_w_expert": nrm(ks[19], (DEPTH, D, N_EXPERTS), D ** -0.5),
        "moe_b_expert": nrm(ks[20], (DEPTH, N_EXPERTS), 0.01),
        "moe_w_gate": nrm(ks[21], (DEPTH, N_EXPERTS, D, EXPERT_HIDDEN), D ** -0.5),
        "moe_w_up": nrm(ks[22], (DEPTH, N_EXPERTS, D, EXPERT_HIDDEN), D ** -0.5),
        "moe_w_down": nrm(ks[23], (DEPTH, N_EXPERTS, EXPERT_HIDDEN, D), EXPERT_HIDDEN ** -0.5),
        "final_norm": 1.0 + nrm(ks[24], (D,), 0.05),
    }


def reference(x, c, positions, w_mod_mix, b_mod_mix, norm_mix, w_mod_ffn, b_mod_ffn, norm_ffn,
              ev_w_in, ev_q_norm, ev_w_q_up, ev_kv_norm, ev_w_kv_up, ev_w_out,
              od_w_in, od_w_out,
              moe_w_group, moe_b_group, moe_w_expert, moe_b_expert, moe_w_gate, moe_w_up, moe_w_down,
              final_norm):
    cond = jax.nn.silu(c)
    for layer in range(DEPTH):
        i = layer // 2
        shift, scale, gate = adaln(cond, w_mod_mix[layer], b_mod_mix[layer])
        h = rms_norm(x, norm_mix[layer]) * (1.0 + scale) + shift
        if layer % 2 == 0:
            y = hybrid_attention(h, positions, ev_w_in[i], ev_q_norm[i], ev_w_q_up[i],
                                 ev_kv_norm[i], ev_w_kv_up[i], ev_w_out[i])
        else:
            y = retention(h, positions, od_w_in[i], od_w_out[i])
        x = x + gate * y
        shift, scale, gate = adaln(cond, w_mod_ffn[layer], b_mod_ffn[layer])
        h = rms_norm(x, norm_ffn[layer]) * (1.0 + scale) + shift
        x = x + gate * hierarchical_moe(h, moe_w_group[layer], moe_b_group[layer],
                                        moe_w_expert[layer], moe_b_expert[layer],
                                        moe_w_gate[layer], moe_w_up[layer], moe_w_down[layer])
    return rms_norm(x, final_norm)
```

```python
import math
import bisect
import numpy as np
import concourse.bass as bass
import concourse.mybir as mybir
from concourse.bass_utils import run_bass_kernel_spmd

F32 = mybir.dt.float32
BF16 = mybir.dt.bfloat16
I32 = mybir.dt.int32
AF = mybir.ActivationFunctionType
ALU = mybir.AluOpType

D = 2048
KC = 16
T_OWN = 1024
T_ALL = 2048
SAME_SYNC = True


class Buf:
    def __init__(self, name, t, ap=None):
        self.name = name
        self.t = t
        self._ap = ap if ap is not None else t.ap()
        self.st = {}

    @property
    def ap(self):
        return self._ap

    def state(self, key):
        s = self.st.get(key)
        if s is None:
            s = [None, []]
            self.st[key] = s
        return s


class Eng:
    def __init__(self, nc, name, eng, ndma=0):
        self.name = name
        self.eng = eng
        self.sem = nc.alloc_semaphore("sem_" + name)
        self.insts = []
        self.snaps = []
        self.flag_seqs = []
        self.flag_vals = []
        self.count = 0
        self.clock = {}
        self.ndma = ndma
        self.dsems = [nc.alloc_semaphore(f"dsem_{name}_{i}") for i in range(ndma)]
        self.dcount = 0
        self.dsnaps = {}


class Prog:
    def __init__(self, nc):
        self.nc = nc
        self.E = {
            "pe": Eng(nc, "pe", nc.tensor),
            "act": Eng(nc, "act", nc.scalar),
            "dve": Eng(nc, "dve", nc.vector),
            "pool": Eng(nc, "pool", nc.gpsimd, ndma=8),
            "sp": Eng(nc, "sp", nc.sync, ndma=8),
        }

    def _known(self, e, ev):
        if ev[0] == "D":
            _, q, i = ev
            Q = self.E[q]
            return e.clock.get(("D", q, i % Q.ndma), -1) >= i
        p, seq = ev
        return e.clock.get(p, -1) >= seq

    def _merge(self, e, other, extra):
        c = dict(e.clock)
        if other:
            for k, v in other.items():
                if c.get(k, -1) < v:
                    c[k] = v
        for k, v in extra.items():
            if c.get(k, -1) < v:
                c[k] = v
        e.clock = c

    def wait(self, ename, ev):
        e = self.E[ename]
        if ev is None or self._known(e, ev):
            return
        if ev[0] == "D":
            _, q, i = ev
            Q = self.E[q]
            slot = i % Q.ndma
            e.eng.wait_ge(Q.dsems[slot], 16 * (i // Q.ndma + 1))
            self._merge(e, Q.dsnaps.get(i), {("D", q, slot): i})
            return
        p, seq = ev
        if p == ename and (not SAME_SYNC or p in ("pe", "sp")):
            return
        Pn = self.E[p]
        k = bisect.bisect_left(Pn.flag_seqs, seq)
        if k < len(Pn.flag_seqs):
            fseq, val = Pn.flag_seqs[k], Pn.flag_vals[k]
        else:
            Pn.count += 1
            val = Pn.count
            fseq = seq
            Pn.insts[seq].then_inc(Pn.sem, 1)
            Pn.flag_seqs.append(seq)
            Pn.flag_vals.append(val)
        e.eng.wait_ge(Pn.sem, val)
        self._merge(e, Pn.snaps[fseq], {p: fseq})

    def flag_last(self, ename):
        Pn = self.E[ename]
        seq = len(Pn.insts) - 1
        if Pn.flag_seqs and Pn.flag_seqs[-1] == seq:
            return
        Pn.count += 1
        Pn.insts[seq].then_inc(Pn.sem, 1)
        Pn.flag_seqs.append(seq)
        Pn.flag_vals.append(Pn.count)

    def _deps(self, reads, writes):
        deps = []
        for b, k in reads:
            keys = [k] if k is not None else list(b.st.keys()) + [None]
            for kk in set(keys + [None]):
                s = b.st.get(kk)
                if s and s[0] is not None:
                    deps.append(s[0])
        for b, k in writes:
            keys = [k] if k is not None else list(b.st.keys()) + [None]
            for kk in set(keys + [None]):
                s = b.st.get(kk)
                if s:
                    if s[0] is not None:
                        deps.append(s[0])
                    deps.extend(s[1])
        return deps

    def _update(self, ev, reads, writes):
        for b, k in reads:
            b.state(k)[1].append(ev)
        for b, k in writes:
            if k is None:
                b.st = {}
            s = b.state(k)
            s[0] = ev
            s[1] = []

    @staticmethod
    def _norm(lst):
        out = []
        for x in lst:
            if isinstance(x, Buf):
                out.append((x, None))
            else:
                out.append(x)
        return out

    def op(self, ename, fn, reads=(), writes=()):
        reads = self._norm(reads)
        writes = self._norm(writes)
        e = self.E[ename]
        for d in self._deps(reads, writes):
            self.wait(ename, d)
        inst = fn(e.eng)
        seq = len(e.insts)
        e.insts.append(inst)
        e.snaps.append(e.clock)
        ev = (ename, seq)
        self._update(ev, reads, writes)
        return ev

    def dma(self, qname, out, in_, reads=(), writes=()):
        reads = self._norm(reads)
        writes = self._norm(writes)
        q = self.E[qname]
        for d in self._deps(reads, writes):
            self.wait(qname, d)
        i = q.dcount
        q.dcount += 1
        if i >= q.ndma:
            self.wait(qname, ("D", qname, i - q.ndma))
        slot = i % q.ndma
        q.eng.dma_start(out=out, in_=in_).then_inc(q.dsems[slot], 16)
        q.dsnaps[i] = q.clock
        ev = ("D", qname, i)
        self._update(ev, reads, writes)
        return ev

    def barrier(self, bufs=()):
        evs = []
        for n, e in self.E.items():
            if e.insts:
                evs.append((n, len(e.insts) - 1))
            for i in range(max(0, e.dcount - e.ndma), e.dcount):
                evs.append(("D", n, i))
        for n in self.E:
            for ev in evs:
                if ev[0] != "D" and ev[0] == n:
                    continue
                self.wait(n, ev)

    def finish(self):
        evs = []
        for n, e in self.E.items():
            if e.insts:
                evs.append((n, len(e.insts) - 1))
            for i in range(max(0, e.dcount - e.ndma), e.dcount):
                evs.append(("D", n, i))
        for ev in evs:
            if ev[0] == "sp":
                continue
            self.wait("sp", ev)


NCONST = 128 * 3 + 2048 * 2
EPS = 1e-6


def host_consts():
    c = np.zeros((128, NCONST), np.float32)
    c[:, 0:128] = 1.0
    c[:, 128:256] = np.eye(128, dtype=np.float32)
    j = np.arange(128)[:, None]
    s = np.arange(128)[None, :]
    c[:, 256:384] = (j >= s).astype(np.float32)
    q = np.arange(512)[None, :]
    for jb in range(4):
        key = 128 * jb + np.arange(128)[:, None]
        c[:, 384 + jb * 512:384 + (jb + 1) * 512] = (key < q).astype(np.float32)
        c[:, 384 + 2048 + jb * 512:384 + 2048 + (jb + 1) * 512] = (key <= q).astype(np.float32)
    return c


class Ctx:
    pass


X_NEXP = 32
NO_CC = False
SKIP_L0 = False
NHEADS = 8


def build(upto=99, dbg=(), mode="F"):
    from contextlib import ExitStack
    nc = bass.Bass("TRN2", target_bir_lowering=False)
    P = Prog(nc)
    X = Ctx()
    dbg_out = {}

    X.in_names = []

    def din(name, shape, dt=F32):
        X.in_names.append(name)
        return nc.dram_tensor(name, list(shape), dt, kind="ExternalInput").ap()

    def dscratch(name, shape, dt=F32, out=False):
        t = nc.dram_tensor(name, list(shape), dt, kind="ExternalOutput" if out else "Internal")
        return Buf(name, t, t.ap())

    L0 = mode in ("A", "F") and not SKIP_L0
    xT_all = Buf("xT_all", None, din("xT_all", [128, KC, T_ALL])) if L0 else None
    pos_in = din("pos_all", [1, T_ALL], I32)
    pvalid_in = din("pvalid", [128, 1])
    cond_in = din("cond", [128, KC])
    consts_in = din("consts", [128, NCONST])
    w_mod = din("w_mod", [4, D, 3 * D])
    b_mod_in = din("b_mod", [128, 4, 48])
    norms_in = din("norms", [128, 5, KC])
    ev_w_in = din("ev_w_in", [D, 3904]) if L0 else None
    ev_qn_in = din("ev_q_norm", [128, 4]) if L0 else None
    ev_w_q_up = din("ev_w_q_up", [512, 1536]) if L0 else None
    ev_kvn_in = din("ev_kv_norm", [128, 2]) if L0 else None
    ev_w_kv_up = din("ev_w_kv_up", [256, 2048]) if L0 else None
    ev_w_out = din("ev_w_out", [D, D]) if L0 else None
    invf_in = din("invf", [128, 2])
    outT = dscratch("outT", [128, KC, T_OWN], F32, out=True)

    def dbg_tensor(name, shape, dt=F32):
        b = dscratch("dbg_" + name, shape, dt, out=True)
        dbg_out[name] = b
        return b

    gstack = ExitStack()

    def salloc(stack, name, shape, dt):
        X.uid = getattr(X, "uid", 0) + 1
        t = stack.enter_context(nc.sbuf_tensor(f"s{X.uid}_" + name, list(shape), dt))
        return Buf(name, t)

    PS = [Buf(f"ps{i}", gstack.enter_context(nc.psum_tensor(f"ps{i}", [128, 512], F32))) for i in range(8)]
    X.ps_i = 0

    X.ps_mod = 6

    def psn():
        b = PS[X.ps_i % X.ps_mod]
        X.ps_i += 1
        return b

    consts = salloc(gstack, "consts", [128, NCONST], F32)
    P.dma("sp", consts.ap, consts_in, writes=[consts])
    ones_f = consts.ap[:, 0:128]
    ident_f = consts.ap[:, 128:256]
    U_f = consts.ap[:, 256:384]

    def mstrict(j):
        return consts.ap[:, 384 + j * 512:384 + (j + 1) * 512]

    def mincl(j):
        return consts.ap[:, 384 + 2048 + j * 512:384 + 2048 + (j + 1) * 512]

    ones_b = salloc(gstack, "ones_b", [128, 128], BF16)
    P.op("dve", lambda e: e.tensor_copy(out=ones_b.ap, in_=ones_f), reads=[consts], writes=[ones_b])
    ident_b = salloc(gstack, "ident_b", [128, 128], BF16)
    P.op("dve", lambda e: e.tensor_copy(out=ident_b.ap, in_=ident_f), reads=[consts], writes=[ident_b])
    pvalid = salloc(gstack, "pvalid", [128, 1], F32)
    P.dma("sp", pvalid.ap, pvalid_in, writes=[pvalid])
    U_b = salloc(gstack, "U_b", [128, 128], BF16)
    P.op("dve", lambda e: e.tensor_copy(out=U_b.ap, in_=U_f), reads=[consts], writes=[U_b])
    ones_pv = salloc(gstack, "ones_pv", [128, 128], BF16)
    P.op("dve", lambda e: e.tensor_scalar(out=ones_pv.ap, in0=ones_f, scalar1=pvalid.ap[:, 0:1], scalar2=None, op0=ALU.mult), reads=[consts, pvalid], writes=[ones_pv])
    modA = salloc(gstack, "modA", [128, 4, KC], F32)
    modB = salloc(gstack, "modB", [128, 4, KC], F32)
    modG = salloc(gstack, "modG", [128, 4, KC], F32)
    norms = salloc(gstack, "norms", [128, 5, KC], F32)
    P.dma("sp", norms.ap, norms_in, writes=[norms])
    invf = salloc(gstack, "invf", [128, 2], F32)
    P.dma("sp", invf.ap, invf_in, writes=[invf])

    class Ring:
        def __init__(self, stack, name, n, shape, dt=BF16):
            self.b = [salloc(stack, f"{name}{i}", shape, dt) for i in range(n)]
            self.i = 0

        def next(self):
            b = self.b[self.i % len(self.b)]
            self.i += 1
            return b

    def wload(ring, view, kcn, cw):
        wb = ring.next()
        P.dma("pool", wb.ap[:, 0:kcn, 0:cw], view, writes=[wb])
        return wb

    def linT(ring, w2d, c0, ncols, act, kcn, toks, evac, piece=512):
        wv = w2d.rearrange("(kc p) n -> p kc n", p=128)
        for cc in range(c0, c0 + ncols, piece):
            cw = min(piece, c0 + ncols - cc)
            wb = wload(ring, wv[:, :, cc:cc + cw], kcn, cw)
            for oc in range(0, cw, 128):
                m = min(128, cw - oc)
                for (t0, n) in toks:
                    ps = psn()
                    for kc in range(kcn):
                        P.op("pe", lambda e, ps=ps, kc=kc, oc=oc, m=m, t0=t0, n=n, wb=wb: e.matmul(
                            ps.ap[0:m, 0:n], lhsT=wb.ap[:, kc, oc:oc + m], rhs=act.ap[:, kc, t0:t0 + n],
                            start=(kc == 0), stop=(kc == kcn - 1)),
                            reads=[wb, act], writes=[ps])
                    evac(ps, cc + oc - c0, m, t0, n)

    def linTok(ring, w2d, c0, ncols, act, kcn, ttiles, evac, piece=512):
        wv = w2d.rearrange("(kc p) n -> p kc n", p=128)
        for cc in range(c0, c0 + ncols, piece):
            cw = min(piece, c0 + ncols - cc)
            wb = wload(ring, wv[:, :, cc:cc + cw], kcn, cw)
            for t0 in ttiles:
                ps = psn()
                for kc in range(kcn):
                    P.op("pe", lambda e, ps=ps, kc=kc, cw=cw, t0=t0, wb=wb: e.matmul(
                        ps.ap[:, 0:cw], lhsT=act.ap[:, kc, t0:t0 + 128], rhs=wb.ap[:, kc, 0:cw],
                        start=(kc == 0), stop=(kc == kcn - 1)),
                        reads=[wb, act], writes=[ps])
                evac(ps, cc - c0, cw, t0)

    cond = salloc(gstack, "cond", [128, KC], F32)
    P.dma("sp", cond.ap, cond_in, writes=[cond])
    condb = salloc(gstack, "condb", [128, KC], BF16)
    P.op("act", lambda e: e.activation(out=condb.ap, in_=cond.ap, func=AF.Silu), reads=[cond], writes=[condb])
    bmod = salloc(gstack, "bmod", [128, 4, 48], F32)
    P.dma("sp", bmod.ap, b_mod_in, writes=[bmod])
    modT = salloc(gstack, "modT", [128, 4, 48], F32)

    def mod_piece(l, j, get_w):
        wv = w_mod[l].rearrange("(kc p) n -> p kc n", p=128)
        wb, wap = get_w()
        P.dma("pool", wap, wv[:, :, j * 128:(j + 1) * 128], writes=[wb])
        ps = psn()
        for kc in range(KC):
            P.op("pe", lambda e: e.matmul(ps.ap[:, 0:1], lhsT=wap[:, kc, :], rhs=condb.ap[:, kc:kc + 1], start=(kc == 0), stop=(kc == KC - 1)),
                 reads=[wb, condb], writes=[ps])
        P.op("dve", lambda e: e.tensor_tensor(out=modT.ap[:, l, j:j + 1], in0=ps.ap[:, 0:1], in1=bmod.ap[:, l, j:j + 1], op=ALU.add),
             reads=[ps, bmod], writes=[(modT, (l, j))])

    def mod_finalize(l):
        P.op("dve", lambda e: e.scalar_tensor_tensor(out=modA.ap[:, l, :], in0=modT.ap[:, l, 16:32], scalar=1.0,
                                                      in1=norms.ap[:, l, :], op0=ALU.add, op1=ALU.mult),
             reads=[modT, norms], writes=[(modA, l)])
        P.op("dve", lambda e: e.tensor_copy(out=modB.ap[:, l, :], in_=modT.ap[:, l, 0:16]), reads=[modT], writes=[(modB, l)])
        P.op("dve", lambda e: e.tensor_copy(out=modG.ap[:, l, :], in_=modT.ap[:, l, 32:48]), reads=[modT], writes=[(modG, l)])

    X.mod_todo = {}

    def mod_run(l, n, get_w):
        j0 = X.mod_todo.get(l, 0)
        if j0 >= 48:
            return
        j1 = 48 if n is None else min(48, j0 + n)
        for j in range(j0, j1):
            mod_piece(l, j, get_w)
        X.mod_todo[l] = j1
        if j1 >= 48:
            mod_finalize(l)

    with ExitStack() as st:
        ring0 = Ring(st, "w0r", 4, [128, KC, 128])

        def getw0():
            b = ring0.next()
            return b, b.ap
        first_l = [0] if mode in ("A", "F") else [2]
        for l in first_l:
            mod_run(l, None, getw0)
        if mode == "A":
            mod_run(1, None, getw0)
        if mode == "B":
            mod_run(3, None, getw0)
        if "mod" in dbg:
            d = dbg_tensor("mod", [128, 4, 48])
            P.dma("sp", d.ap, modT.ap, reads=[modT], writes=[d])
        P.barrier()

    def rmsnorm_T(st, tag, getx, nch, ntb, dim, emit, npart=128):
        sq_r = Ring(st, f"sq{tag}_", 3, [128, 512], BF16)
        tmp_r = Ring(st, f"tm{tag}_", 3, [128, 512], F32)
        rs = salloc(st, f"rs{tag}", [128, 512], F32)
        rstd = salloc(st, f"rstd{tag}", [128, 512], F32)
        for tb in range(ntb):
            ps = psn()
            xs = [getx(c, tb) for c in range(nch)]
            for c in range(nch):
                sq = sq_r.next()
                xap, xdeps = xs[c]
                P.op("act", lambda e, sq=sq, xap=xap: e.activation(out=sq.ap, in_=xap, func=AF.Square),
                     reads=xdeps, writes=[sq])
                P.op("pe", lambda e, sq=sq, ps=ps, c=c: e.matmul(ps.ap, lhsT=ones_b.ap, rhs=sq.ap, start=(c == 0), stop=(c == nch - 1)),
                     reads=[sq, ones_b], writes=[ps])
            P.op("act", lambda e, ps=ps: e.activation(out=rs.ap, in_=ps.ap, func=AF.Sqrt, scale=1.0 / dim, bias=EPS),
                 reads=[ps], writes=[rs])
            P.op("dve", lambda e: e.reciprocal(out=rstd.ap, in_=rs.ap), reads=[rs], writes=[rstd])
            for c in range(nch):
                tm = tmp_r.next()
                xap, xdeps = xs[c]
                P.op("dve", lambda e, tm=tm, xap=xap: e.tensor_tensor(out=tm.ap, in0=xap, in1=rstd.ap, op=ALU.mult),
                     reads=list(xdeps) + [rstd], writes=[tm])
                emit(tm, c, tb)

    def prologue(st, l, xsrc, T, hT, hook=None):
        xs_r = Ring(st, f"xs{l}_", 2, [128, KC, 512], F32)
        cur = {}

        def getx(c, tb):
            if c == 0:
                xs = xs_r.next()
                P.dma("sp", xs.ap, xsrc.ap[:, :, tb * 512:(tb + 1) * 512], reads=[xsrc], writes=[xs])
                cur["xs"] = xs
            return cur["xs"].ap[:, c, :], [cur["xs"]]

        def emit(tm, c, tb):
            if hook is None:
                P.op("act", lambda e: e.activation(
                    out=hT.ap[:, c, tb * 512:(tb + 1) * 512], in_=tm.ap, func=AF.Identity,
                    scale=modA.ap[:, l, c:c + 1], bias=modB.ap[:, l, c:c + 1]),
                    reads=[tm, (modA, l), (modB, l)], writes=[(hT, (c, tb))])
            else:
                hook(tm, c, tb)
        rmsnorm_T(st, f"p{l}", getx, KC, T // 512, D, emit)

    def rope_tables(st, st_tmp, tag, npart, col, T, pos_ap):
        o_sin = salloc(st, f"sin{tag}", [npart, T], F32)
        o_cos = salloc(st, f"cos{tag}", [npart, T], F32)
        posi = salloc(st_tmp, f"posi{tag}", [npart, T], I32)
        P.dma("sp", posi.ap, pos_ap.broadcast_to([npart, T]), writes=[posi])
        ang = salloc(st_tmp, f"ang{tag}", [npart, T], F32)
        P.op("dve", lambda e: e.tensor_copy(out=ang.ap, in_=posi.ap), reads=[posi], writes=[ang])
        P.op("dve", lambda e: e.tensor_scalar(out=ang.ap, in0=ang.ap, scalar1=invf.ap[0:npart, col:col + 1], scalar2=None, op0=ALU.mult),
             reads=[ang, invf], writes=[ang])
        a = salloc(st_tmp, f"a{tag}", [npart, T], F32)
        t = salloc(st_tmp, f"t{tag}", [npart, T], F32)
        ni = salloc(st_tmp, f"ni{tag}", [npart, T], I32)
        outs = []
        TWO_PI = 2.0 * math.pi
        C1 = 6.28125
        C2 = TWO_PI - C1
        for nm, off in (("sin", 0.0), ("cos", math.pi / 2)):
            o = o_sin if nm == "sin" else o_cos
            P.op("dve", lambda e, off=off: e.tensor_scalar(out=a.ap, in0=ang.ap, scalar1=off, scalar2=None, op0=ALU.add), reads=[ang], writes=[a])
            P.op("dve", lambda e: e.tensor_scalar(out=t.ap, in0=a.ap, scalar1=1.0 / TWO_PI, scalar2=None, op0=ALU.mult), reads=[a], writes=[t])
            P.op("dve", lambda e: e.tensor_copy(out=ni.ap, in_=t.ap), reads=[t], writes=[ni])
            P.op("dve", lambda e: e.tensor_copy(out=t.ap, in_=ni.ap), reads=[ni], writes=[t])
            P.op("dve", lambda e: e.scalar_tensor_tensor(out=a.ap, in0=t.ap, scalar=-C1, in1=a.ap, op0=ALU.mult, op1=ALU.add), reads=[t, a], writes=[a])
            P.op("dve", lambda e: e.scalar_tensor_tensor(out=a.ap, in0=t.ap, scalar=-C2, in1=a.ap, op0=ALU.mult, op1=ALU.add), reads=[t, a], writes=[a])
            P.op("dve", lambda e: e.tensor_scalar(out=t.ap, in0=a.ap, scalar1=math.pi, scalar2=-TWO_PI, op0=ALU.is_gt, op1=ALU.mult), reads=[a], writes=[t])
            P.op("dve", lambda e: e.tensor_tensor(out=a.ap, in0=a.ap, in1=t.ap, op=ALU.add), reads=[t, a], writes=[a])
            P.op("dve", lambda e: e.tensor_scalar(out=t.ap, in0=a.ap, scalar1=-math.pi, scalar2=TWO_PI, op0=ALU.is_lt, op1=ALU.mult), reads=[a], writes=[t])
            P.op("dve", lambda e: e.tensor_tensor(out=a.ap, in0=a.ap, in1=t.ap, op=ALU.add), reads=[t, a], writes=[a])
            P.op("dve", lambda e: e.tensor_scalar(out=a.ap, in0=a.ap, scalar1=-math.pi, scalar2=math.pi, op0=ALU.max, op1=ALU.min), reads=[a], writes=[a])
            P.op("act", lambda e, o=o: e.activation(out=o.ap, in_=a.ap, func=AF.Sin), reads=[a], writes=[o])
            outs.append(o)
        return outs[1], outs[0]

    def rope_apply(tmp_r, x1, x2, d1, cos_ap, sin_ap, o1, o2, o_w, npart, n, scale=1.0):
        for (oa, fa, fb, opx) in ((o1, cos_ap, sin_ap, ALU.subtract), (o2, sin_ap, cos_ap, ALU.add)):
            ta = tmp_r.next()
            tb_ = tmp_r.next()
            P.op("dve", lambda e, ta=ta, fa=fa: e.tensor_tensor(out=ta.ap[0:npart, 0:n], in0=x1, in1=fa, op=ALU.mult), reads=d1, writes=[ta])
            P.op("dve", lambda e, tb_=tb_, fb=fb: e.tensor_tensor(out=tb_.ap[0:npart, 0:n], in0=x2, in1=fb, op=ALU.mult), reads=d1, writes=[tb_])
            P.op("dve", lambda e, ta=ta, tb_=tb_, opx=opx: e.tensor_tensor(out=ta.ap[0:npart, 0:n], in0=ta.ap[0:npart, 0:n], in1=tb_.ap[0:npart, 0:n], op=opx),
                 reads=[ta, tb_], writes=[ta])
            P.op("dve", lambda e, ta=ta, oa=oa: e.tensor_scalar(out=oa, in0=ta.ap[0:npart, 0:n], scalar1=scale, scalar2=None, op0=ALU.mult),
                 reads=[ta], writes=o_w)

    x1T = dscratch("x1T", [128, KC, T_OWN])
    OWN0 = T_ALL - T_OWN
    if upto >= 1 and L0:
      with ExitStack() as st:
        hT = salloc(st, "hT0", [128, KC, T_ALL], BF16)
        with ExitStack() as st2:
            prologue(st2, 0, xT_all, T_ALL, hT)
            if "h0" in dbg:
                d = dbg_tensor("h0", [128, KC, T_ALL], BF16)
                P.dma("sp", d.ap, hT.ap, reads=[hT], writes=[d])
            P.barrier()
        with ExitStack() as st_t:
            cosm, sinm = rope_tables(st, st_t, "m", 32, 0, T_ALL, pos_in)
            P.barrier()
        cqn = salloc(st, "cqn", [128, 4, T_OWN], BF16)
        ckvn = salloc(st, "ckvn", [128, 2, T_ALL], BF16)
        k1 = salloc(st, "k1", [32, T_ALL], BF16)
        k2 = salloc(st, "k2", [32, T_ALL], BF16)
        ring = Ring(st, "wr0_", 4, [128, KC, 128])
        toks_all = [(t, 512) for t in range(0, T_ALL, 512)]
        toks_own = [(t, 512) for t in range(OWN0, T_ALL, 512)]
        ISQ = 128 ** -0.5
        MSC = 192 ** -0.5
        with ExitStack() as st1:
            cq = salloc(st1, "cq", [128, 4, T_OWN], F32)
            ckv = salloc(st1, "ckv", [128, 2, T_ALL], F32)
            st1a = ExitStack()
            kr = salloc(st1a, "kr", [32, 2, T_ALL], F32)

            def ev_cq(ps, col, m, t0, n):
                P.op("act", lambda e: e.activation(out=cq.ap[:, col // 128, t0 - OWN0:t0 - OWN0 + n], in_=ps.ap[:, 0:n], func=AF.Copy),
                     reads=[ps], writes=[(cq, (col // 128, t0))])
            linT(ring, ev_w_in, 3072, 512, hT, KC, toks_own, ev_cq, piece=128)

            def ev_ckv(ps, col, m, t0, n):
                P.op("act", lambda e: e.activation(out=ckv.ap[:, col // 128, t0:t0 + n], in_=ps.ap[:, 0:n], func=AF.Copy),
                     reads=[ps], writes=[(ckv, (col // 128, t0))])
            linT(ring, ev_w_in, 3584, 256, hT, KC, toks_all, ev_ckv, piece=128)
            for half in range(2):
                def ev_kr(ps, col, m, t0, n, half=half):
                    P.op("act", lambda e: e.activation(out=kr.ap[:, half, t0:t0 + n], in_=ps.ap[0:32, 0:n], func=AF.Copy),
                         reads=[ps], writes=[(kr, (half, t0))])
                linT(ring, ev_w_in, 3840 + 32 * half, 32, hT, KC, toks_all, ev_kr, piece=32)
            ropetmp = Ring(st1a, "rt_", 4, [128, 512], F32)
            for (t0, n) in toks_all:
                rope_apply(ropetmp, kr.ap[:, 0, t0:t0 + n], kr.ap[:, 1, t0:t0 + n], [kr],
                           cosm.ap[:, t0:t0 + n], sinm.ap[:, t0:t0 + n],
                           k1.ap[:, t0:t0 + n], k2.ap[:, t0:t0 + n], [(k1, t0), (k2, t0)], 32, n)
            P.barrier()
            st1a.close()
            qng = salloc(st1, "qng", [128, 4], F32)
            P.dma("sp", qng.ap, ev_qn_in, writes=[qng])
            kvng = salloc(st1, "kvng", [128, 2], F32)
            P.dma("sp", kvng.ap, ev_kvn_in, writes=[kvng])

            def emit_cq(tm, c, tb):
                P.op("act", lambda e: e.activation(out=cqn.ap[:, c, tb * 512:(tb + 1) * 512], in_=tm.ap, func=AF.Copy, scale=qng.ap[:, c:c + 1]),
                     reads=[tm, qng], writes=[(cqn, (c, tb))])
            with ExitStack() as stn:
                rmsnorm_T(stn, "cq", lambda c, tb: (cq.ap[:, c, tb * 512:(tb + 1) * 512], [cq]), 4, T_OWN // 512, 512, emit_cq)
                P.barrier()

            def emit_ckv(tm, c, tb):
                P.op("act", lambda e: e.activation(out=ckvn.ap[:, c, tb * 512:(tb + 1) * 512], in_=tm.ap, func=AF.Copy, scale=kvng.ap[:, c:c + 1]),
                     reads=[tm, kvng], writes=[(ckvn, (c, tb))])
            with ExitStack() as stn:
                rmsnorm_T(stn, "ckv", lambda c, tb: (ckv.ap[:, c, tb * 512:(tb + 1) * 512], [ckv]), 2, T_ALL // 512, 256, emit_ckv)
            P.barrier()

        oT = salloc(st, "oT", [128, KC, T_OWN], BF16)
        with ExitStack() as st2:
            qh = salloc(st2, "sbq", [128, T_OWN], BF16)
            kh = salloc(st2, "sbk", [128, T_ALL], BF16)
            vh = salloc(st2, "sbv", [128, 16, 128], BF16)
            e_r = Ring(st2, "sbe_", 3, [128, 512], F32)
            sp_r = Ring(st2, "sbs_", 2, [128, 512], F32)
            L_r = Ring(st2, "sbl_", 2, [128, 512], BF16)
            lsf_r = Ring(st2, "sblsf_", 2, [128, 512], F32)
            lsb_r = Ring(st2, "sblsb_", 2, [128, 512], BF16)
            er_r = Ring(st2, "sber_", 2, [128, 512], F32)
            a_r = Ring(st2, "sba_", 2, [128, 512], BF16)
            def getw_l0():
                b = ring.next()
                return b, b.ap
            for h in range(8):
                if mode == "F":
                    mod_run(1, 6, getw_l0)

                def ev_q(ps, col, m, t0, n):
                    P.op("act", lambda e: e.activation(out=qh.ap[:, t0 - OWN0:t0 - OWN0 + n], in_=ps.ap[:, 0:n], func=AF.Copy, scale=ISQ),
                         reads=[ps], writes=[(qh, t0)])
                linT(ring, ev_w_in, h * 128, 128, hT, KC, toks_own, ev_q, piece=128)

                def ev_k(ps, col, m, t0, n):
                    P.op("act", lambda e: e.activation(out=kh.ap[:, t0:t0 + n], in_=ps.ap[:, 0:n], func=AF.Copy),
                         reads=[ps], writes=[(kh, t0)])
                linT(ring, ev_w_in, 1024 + h * 128, 128, hT, KC, toks_all, ev_k, piece=128)

                def ev_v(ps, col, cw, t0):
                    P.op("dve", lambda e: e.tensor_copy(out=vh.ap[:, t0 // 128, :], in_=ps.ap[:, 0:128]),
                         reads=[ps], writes=[(vh, t0 // 128)])
                linTok(ring, ev_w_in, 2048 + h * 128, 128, hT, KC, list(range(0, T_ALL, 128)), ev_v, piece=128)
                P.op("dve", lambda e: e.tensor_scalar(out=vh.ap[:, 0:8, :], in0=vh.ap[:, 0:8, :], scalar1=pvalid.ap[:, 0:1], scalar2=None, op0=ALU.mult),
                     reads=[vh, pvalid], writes=[vh])
                tiles = []
                for s in range(2):
                    nkb = 8 + 4 * s + 4
                    for kb in range(nkb - 1, -1, -1):
                        tiles.append((s, kb, kb == nkb - 1, kb == 0))
                stt = [dict() for _ in tiles]

                def sb_s1(i):
                    s, kb, first, last = tiles[i]
                    q0 = s * 512
                    T = stt[i]
                    psz = psn()
                    P.op("pe", lambda e: e.matmul(psz.ap, lhsT=kh.ap[:, kb * 128:(kb + 1) * 128], rhs=qh.ap[:, q0:q0 + 512], start=True, stop=True),
                         reads=[kh, qh], writes=[psz])
                    eb = e_r.next()
                    P.op("act", lambda e: e.activation(out=eb.ap, in_=psz.ap, func=AF.Exp), reads=[psz], writes=[eb])
                    jd = kb - (8 + 4 * s)
                    Lb = L_r.next()
                    if jd >= 0:
                        spb = sp_r.next()
                        P.op("act", lambda e: e.activation(out=spb.ap, in_=eb.ap, func=AF.Ln, bias=1.0), reads=[eb], writes=[spb])
                        P.op("dve", lambda e: e.tensor_tensor(out=Lb.ap, in0=spb.ap, in1=mstrict(jd), op=ALU.mult), reads=[spb, consts], writes=[Lb])
                    else:
                        P.op("act", lambda e: e.activation(out=Lb.ap, in_=eb.ap, func=AF.Ln, bias=1.0), reads=[eb], writes=[Lb])
                    T.update(eb=eb, Lb=Lb, jd=jd)

                def sb_s2(i):
                    s, kb, first, last = tiles[i]
                    T = stt[i]
                    Lb = T["Lb"]
                    psr = psn()
                    P.op("pe", lambda e: e.matmul(psr.ap, lhsT=U_b.ap, rhs=Lb.ap, start=True, stop=first), reads=[Lb, U_b], writes=[psr])
                    if not first:
                        prev_b = stt[i - 1]["Lsb"]
                        P.op("pe", lambda e: e.matmul(psr.ap, lhsT=ones_b.ap, rhs=prev_b.ap, start=False, stop=True), reads=[prev_b, ones_b], writes=[psr])
                    erb = er_r.next()
                    P.op("act", lambda e: e.activation(out=erb.ap, in_=psr.ap, func=AF.Exp, scale=-1.0), reads=[psr], writes=[erb])
                    Lsf = lsf_r.next()
                    if first:
                        P.op("dve", lambda e: e.tensor_copy(out=Lsf.ap, in_=Lb.ap), reads=[Lb], writes=[Lsf])
                    else:
                        prev_f = stt[i - 1]["Lsf"]
                        P.op("dve", lambda e: e.tensor_tensor(out=Lsf.ap, in0=prev_f.ap, in1=Lb.ap, op=ALU.add), reads=[Lb, prev_f], writes=[Lsf])
                    Lsb = lsb_r.next()
                    P.op("dve", lambda e: e.tensor_copy(out=Lsb.ap, in_=Lsf.ap), reads=[Lsf], writes=[Lsb])
                    T.update(erb=erb, Lsf=Lsf, Lsb=Lsb)

                def sb_s3(i):
                    s, kb, first, last = tiles[i]
                    q0 = s * 512
                    T = stt[i]
                    eb, erb, jd = T["eb"], T["erb"], T["jd"]
                    pso = PS[6 + s]
                    ab = a_r.next()
                    if jd >= 0:
                        P.op("dve", lambda e: e.tensor_tensor(out=erb.ap, in0=erb.ap, in1=mstrict(jd), op=ALU.mult), reads=[erb, consts], writes=[erb])
                    P.op("dve", lambda e: e.tensor_tensor(out=ab.ap, in0=eb.ap, in1=erb.ap, op=ALU.mult), reads=[eb, erb], writes=[ab])
                    P.op("pe", lambda e: e.matmul(pso.ap[:, 0:512], lhsT=vh.ap[:, kb, :], rhs=ab.ap, start=first, stop=last),
                         reads=[vh, ab], writes=[pso])
                    if last:
                        P.op("act", lambda e: e.activation(out=oT.ap[:, h, q0:q0 + 512], in_=pso.ap, func=AF.Copy), reads=[pso], writes=[(oT, (h, s))])
                nt = len(tiles)
                for step in range(nt + 2):
                    if step < nt:
                        sb_s1(step)
                    if 0 <= step - 1 < nt:
                        sb_s2(step - 1)
                    if 0 <= step - 2 < nt:
                        sb_s3(step - 2)
            P.barrier()

        with ExitStack() as st3:
            qn = salloc(st3, "mqn", [128, T_OWN], BF16)
            q1 = salloc(st3, "mq1", [32, T_OWN], BF16)
            q2 = salloc(st3, "mq2", [32, T_OWN], BF16)
            kn = salloc(st3, "mkn", [128, T_ALL], BF16)
            vm = salloc(st3, "mv", [128, 16, 128], BF16)
            ropetmp = Ring(st3, "rt3_", 4, [128, 512], F32)
            p_r = Ring(st3, "mp_", 4, [128, 512], BF16)
            pf_r = Ring(st3, "mpf_", 3, [128, 512], F32)
            rden = salloc(st3, "mrden", [128, 512], F32)
            toks_loc = [(0, 512), (512, 512)]
            wq = ev_w_q_up.rearrange("(kc p) n -> p kc n", p=128)
            for h in range(8):
                def ev_qn(ps, col, m, t0, n):
                    P.op("act", lambda e: e.activation(out=qn.ap[:, t0:t0 + n], in_=ps.ap[:, 0:n], func=AF.Copy, scale=MSC),
                         reads=[ps], writes=[(qn, t0)])
                linT(ring, ev_w_q_up, h * 192, 128, cqn, 4, toks_loc, ev_qn, piece=128)
                wb = wload(ring, wq[:, :, h * 192 + 128:h * 192 + 192], 4, 64)
                for (t0, n) in toks_loc:
                    ps1 = psn()
                    ps2 = psn()
                    for (psx, off) in ((ps1, 0), (ps2, 32)):
                        for kc in range(4):
                            P.op("pe", lambda e: e.matmul(psx.ap[0:32, 0:n], lhsT=wb.ap[:, kc, off:off + 32], rhs=cqn.ap[:, kc, t0:t0 + n],
                                                          start=(kc == 0), stop=(kc == 3)), reads=[wb, cqn], writes=[psx])
                    rope_apply(ropetmp, ps1.ap[0:32, 0:n], ps2.ap[0:32, 0:n], [ps1, ps2],
                               cosm.ap[:, OWN0 + t0:OWN0 + t0 + n], sinm.ap[:, OWN0 + t0:OWN0 + t0 + n],
                               q1.ap[:, t0:t0 + n], q2.ap[:, t0:t0 + n], [(q1, t0), (q2, t0)], 32, n, scale=MSC)

                def ev_kn(ps, col, m, t0, n):
                    P.op("act", lambda e: e.activation(out=kn.ap[:, t0:t0 + n], in_=ps.ap[:, 0:n], func=AF.Copy),
                         reads=[ps], writes=[(kn, t0)])
                linT(ring, ev_w_kv_up, h * 256, 128, ckvn, 2, toks_all, ev_kn, piece=128)

                def ev_vm(ps, col, cw, t0):
                    P.op("dve", lambda e: e.tensor_copy(out=vm.ap[:, t0 // 128, :], in_=ps.ap[:, 0:128]),
                         reads=[ps], writes=[(vm, t0 // 128)])
                linTok(ring, ev_w_kv_up, h * 256 + 128, 128, ckvn, 2, list(range(0, T_ALL, 128)), ev_vm, piece=128)
                P.op("dve", lambda e: e.tensor_scalar(out=vm.ap[:, 0:8, :], in0=vm.ap[:, 0:8, :], scalar1=pvalid.ap[:, 0:1], scalar2=None, op0=ALU.mult),
                     reads=[vm, pvalid], writes=[vm])
                for s in range(2):
                    q0 = s * 512
                    nkb = 8 + 4 * s + 4
                    pso = PS[6]
                    psd = PS[7]
                    pbs = {}

                    def m_s1(kb):
                        psz = psn()
                        ks = slice(kb * 128, (kb + 1) * 128)
                        P.op("pe", lambda e: e.matmul(psz.ap, lhsT=kn.ap[:, ks], rhs=qn.ap[:, q0:q0 + 512], start=True, stop=False),
                             reads=[kn, qn], writes=[psz])
                        P.op("pe", lambda e: e.matmul(psz.ap, lhsT=k1.ap[:, ks], rhs=q1.ap[:, q0:q0 + 512], start=False, stop=False),
                             reads=[k1, q1], writes=[psz])
                        P.op("pe", lambda e: e.matmul(psz.ap, lhsT=k2.ap[:, ks], rhs=q2.ap[:, q0:q0 + 512], start=False, stop=True),
                             reads=[k2, q2], writes=[psz])
                        jd = kb - (8 + 4 * s)
                        pb = p_r.next()
                        if jd >= 0:
                            pf = pf_r.next()
                            P.op("act", lambda e: e.activation(out=pf.ap, in_=psz.ap, func=AF.Exp), reads=[psz], writes=[pf])
                            P.op("dve", lambda e: e.tensor_tensor(out=pb.ap, in0=pf.ap, in1=mincl(jd), op=ALU.mult), reads=[pf, consts], writes=[pb])
                        else:
                            P.op("act", lambda e: e.activation(out=pb.ap, in_=psz.ap, func=AF.Exp), reads=[psz], writes=[pb])
                        pbs[kb] = pb

                    def m_s2(kb):
                        first = (kb == 0)
                        last = (kb == nkb - 1)
                        pb = pbs[kb]
                        onesx = ones_pv if kb < 8 else ones_b
                        P.op("pe", lambda e: e.matmul(psd.ap, lhsT=onesx.ap, rhs=pb.ap, start=first, stop=last), reads=[onesx, pb], writes=[psd])
                        P.op("pe", lambda e: e.matmul(pso.ap, lhsT=vm.ap[:, kb, :], rhs=pb.ap, start=first, stop=last), reads=[vm, pb], writes=[pso])
                    for step in range(nkb + 2):
                        if step < nkb:
                            m_s1(step)
                        if 0 <= step - 2 < nkb:
                            m_s2(step - 2)
                    P.op("dve", lambda e: e.reciprocal(out=rden.ap, in_=psd.ap), reads=[psd], writes=[rden])
                    P.op("dve", lambda e: e.tensor_tensor(out=oT.ap[:, 8 + h, q0:q0 + 512], in0=pso.ap, in1=rden.ap, op=ALU.mult),
                         reads=[pso, rden], writes=[(oT, (8 + h, s))])
            P.barrier()

        with ExitStack() as st4:
            if "o0" in dbg:
                d = dbg_tensor("o0", [128, KC, T_OWN], BF16)
                P.dma("sp", d.ap, oT.ap, reads=[oT], writes=[d])
            xo_r = Ring(st4, "xo_", 3, [128, 512], F32)
            res_r = Ring(st4, "res_", 3, [128, 512], F32)

            def ev_out(ps, col, m, t0, n):
                fc = col // 128
                xo = xo_r.next()
                P.dma("sp", xo.ap[:, 0:n], xT_all.ap[:, fc, OWN0 + t0:OWN0 + t0 + n], reads=[xT_all], writes=[xo])
                res = res_r.next()
                P.op("dve", lambda e: e.scalar_tensor_tensor(out=res.ap[:, 0:n], in0=ps.ap[:, 0:n], scalar=modG.ap[:, 0, fc:fc + 1], in1=xo.ap[:, 0:n],
                                                              op0=ALU.mult, op1=ALU.add), reads=[ps, xo, (modG, 0)], writes=[res])
                P.dma("sp", x1T.ap[:, fc, t0:t0 + n], res.ap[:, 0:n], reads=[res], writes=[(x1T, (fc, t0))])
            linT(ring, ev_w_out, 0, D, oT, KC, [(0, 512), (512, 512)], ev_out, piece=128)
            P.barrier()
    X.final_src = x1T
    if "x1" in dbg and L0:
        d = dbg_tensor("x1", [128, KC, T_OWN])
        P.dma("sp", d.ap, x1T.ap, reads=[x1T], writes=[d])


    need_moe = (upto >= 2 and L0) or (upto >= 5 and mode in ("B", "F"))
    if need_moe:
        moe_wr_in = din("moe_wr", [2, D, 36])
        moe_br_in = din("moe_br", [2, 1, 36])
        moe_w_gate = din("moe_w_gate", [2, 32, D, 512])
        moe_w_up = din("moe_w_up", [2, 32, D, 512])
        moe_w_down = din("moe_w_down", [2, 32, 512, D])
    BIG = 1.0e30
    AX = mybir.AxisListType.X

    def moe_layer(l, xin, xout, n_exp=32):
        li = 2 * l + 1
        with ExitStack() as st:
            hT = salloc(st, f"hTm{l}", [128, KC, T_OWN], BF16)
            comb = salloc(st, f"comb{l}", [128, 8, 32], F32)
            wr = salloc(st, f"wr{l}", [128, KC, 36], F32)
            P.dma("sp", wr.ap, moe_wr_in[l].rearrange("(kc p) n -> p kc n", p=128), writes=[wr])
            br = salloc(st, f"br{l}", [128, 36], F32)
            P.dma("sp", br.ap, moe_br_in[l].broadcast_to([128, 36]), writes=[br])
            with ExitStack() as stp:
                hf_r = Ring(stp, f"hf{l}_", 2, [128, 512], F32)
                sm = {k: salloc(stp, f"rt{l}_{k}", [128, n], F32) for k, n in
                      (("lg", 36), ("gmax", 1), ("ngmax", 1), ("ge", 4), ("gsum", 1), ("gw", 1), ("ohg", 4), ("pen", 4), ("ml", 32),
                       ("m1", 1), ("oh1", 32), ("ml2", 32), ("m2", 1), ("oh2", 32), ("d", 1), ("ed", 1), ("den", 1), ("rden", 1),
                       ("w1", 1), ("w2", 1), ("tmp", 32))}
                X.ps_mod = 4

                def hook(tm, c, tb):
                    hf = hf_r.next()
                    P.op("act", lambda e: e.activation(out=hf.ap, in_=tm.ap, func=AF.Identity,
                                                       scale=modA.ap[:, li, c:c + 1], bias=modB.ap[:, li, c:c + 1]),
                         reads=[tm, (modA, li), (modB, li)], writes=[hf])
                    P.op("dve", lambda e: e.tensor_copy(out=hT.ap[:, c, tb * 512:(tb + 1) * 512], in_=hf.ap), reads=[hf], writes=[(hT, (c, tb))])
                    for tt in range(4):
                        P.op("pe", lambda e: e.matmul(PS[4 + tt].ap[:, 0:36], lhsT=hf.ap[:, tt * 128:(tt + 1) * 128], rhs=wr.ap[:, c, :],
                                                      start=(c == 0), stop=(c == KC - 1)), reads=[hf, wr], writes=[PS[4 + tt]])
                    if c == KC - 1:
                        for tt in range(4):
                            gt = tb * 4 + tt
                            S = sm

                            def dv(fn, r, w):
                                P.op("dve", fn, reads=r, writes=w)
                            dv(lambda e: e.tensor_tensor(out=S["lg"].ap, in0=PS[4 + tt].ap[:, 0:36], in1=br.ap, op=ALU.add), [PS[4 + tt], br], [S["lg"]])
                            gl = S["lg"].ap[:, 0:4]
                            el = S["lg"].ap[:, 4:36]
                            dv(lambda e: e.reduce_max(out=S["gmax"].ap, in_=gl, axis=AX), [S["lg"]], [S["gmax"]])
                            dv(lambda e: e.tensor_scalar(out=S["ngmax"].ap, in0=S["gmax"].ap, scalar1=-1.0, scalar2=None, op0=ALU.mult), [S["gmax"]], [S["ngmax"]])
                            P.op("act", lambda e: e.activation(out=S["ge"].ap, in_=gl, func=AF.Exp, bias=S["ngmax"].ap[:, 0:1], accum_out=S["gsum"].ap),
                                 reads=[S["lg"], S["ngmax"]], writes=[S["ge"], S["gsum"]])
                            dv(lambda e: e.reciprocal(out=S["gw"].ap, in_=S["gsum"].ap), [S["gsum"]], [S["gw"]])
                            dv(lambda e: e.tensor_scalar(out=S["ohg"].ap, in0=gl, scalar1=S["gmax"].ap[:, 0:1], scalar2=None, op0=ALU.is_equal), [S["lg"], S["gmax"]], [S["ohg"]])
                            dv(lambda e: e.tensor_scalar(out=S["pen"].ap, in0=S["ohg"].ap, scalar1=BIG, scalar2=-BIG, op0=ALU.mult, op1=ALU.add), [S["ohg"]], [S["pen"]])
                            for g in range(4):
                                dv(lambda e: e.tensor_scalar(out=S["ml"].ap[:, g * 8:(g + 1) * 8], in0=S["lg"].ap[:, 4 + g * 8:4 + (g + 1) * 8],
                                                             scalar1=S["pen"].ap[:, g:g + 1], scalar2=None, op0=ALU.add), [S["lg"], S["pen"]], [S["ml"]])
                            dv(lambda e: e.reduce_max(out=S["m1"].ap, in_=S["ml"].ap, axis=AX), [S["ml"]], [S["m1"]])
                            dv(lambda e: e.tensor_scalar(out=S["oh1"].ap, in0=S["ml"].ap, scalar1=S["m1"].ap[:, 0:1], scalar2=None, op0=ALU.is_equal), [S["ml"], S["m1"]], [S["oh1"]])
                            dv(lambda e: e.scalar_tensor_tensor(out=S["ml2"].ap, in0=S["oh1"].ap, scalar=-BIG, in1=S["ml"].ap, op0=ALU.mult, op1=ALU.add), [S["oh1"], S["ml"]], [S["ml2"]])
                            dv(lambda e: e.reduce_max(out=S["m2"].ap, in_=S["ml2"].ap, axis=AX), [S["ml2"]], [S["m2"]])
                            dv(lambda e: e.tensor_scalar(out=S["oh2"].ap, in0=S["ml2"].ap, scalar1=S["m2"].ap[:, 0:1], scalar2=None, op0=ALU.is_equal), [S["ml2"], S["m2"]], [S["oh2"]])
                            dv(lambda e: e.tensor_tensor(out=S["d"].ap, in0=S["m2"].ap, in1=S["m1"].ap, op=ALU.subtract), [S["m1"], S["m2"]], [S["d"]])
                            P.op("act", lambda e: e.activation(out=S["ed"].ap, in_=S["d"].ap, func=AF.Exp), reads=[S["d"]], writes=[S["ed"]])
                            dv(lambda e: e.tensor_scalar(out=S["den"].ap, in0=S["ed"].ap, scalar1=1.0, scalar2=None, op0=ALU.add), [S["ed"]], [S["den"]])
                            dv(lambda e: e.reciprocal(out=S["rden"].ap, in_=S["den"].ap), [S["den"]], [S["rden"]])
                            dv(lambda e: e.tensor_tensor(out=S["w1"].ap, in0=S["rden"].ap, in1=S["gw"].ap, op=ALU.mult), [S["rden"], S["gw"]], [S["w1"]])
                            dv(lambda e: e.tensor_tensor(out=S["w2"].ap, in0=S["w1"].ap, in1=S["ed"].ap, op=ALU.mult), [S["w1"], S["ed"]], [S["w2"]])
                            dv(lambda e: e.tensor_scalar(out=S["tmp"].ap, in0=S["oh1"].ap, scalar1=S["w1"].ap[:, 0:1], scalar2=None, op0=ALU.mult), [S["oh1"], S["w1"]], [S["tmp"]])
                            dv(lambda e: e.scalar_tensor_tensor(out=comb.ap[:, gt, :], in0=S["oh2"].ap, scalar=S["w2"].ap[:, 0:1], in1=S["tmp"].ap, op0=ALU.mult, op1=ALU.add),
                               [S["oh2"], S["w2"], S["tmp"]], [(comb, gt)])
                prologue(stp, li, xin, T_OWN, hT, hook=hook)
                X.ps_mod = 6
                P.barrier()
            if f"comb{l}" in dbg:
                d = dbg_tensor(f"comb{l}", [128, 8, 32])
                P.dma("sp", d.ap, comb.ap, reads=[comb], writes=[d])
            if f"hm{l}" in dbg:
                d = dbg_tensor(f"hm{l}", [128, KC, T_OWN], BF16)
                P.dma("sp", d.ap, hT.ap, reads=[hT], writes=[d])
            yacc = salloc(st, f"yacc{l}", [128, KC, T_OWN], F32)
            with ExitStack() as ste:
                mring = Ring(ste, f"mw{l}_", 12, [128, 2048])
                aT = salloc(ste, f"aT{l}", [128, 4, T_OWN], BF16)
                sg_r = Ring(ste, f"sg{l}_", 2, [128, 512], F32)
                t1_r = Ring(ste, f"t1{l}_", 2, [128, 512], F32)
                cs_r = Ring(ste, f"cs{l}_", 2, [128, T_OWN], F32)
                dg_r = Ring(ste, f"dg{l}_", 2, [128, 128], F32)
                def getw_m():
                    b = mring.next()
                    return b, b.ap.rearrange("p (k n) -> p k n", n=128)
                for ex in range(n_exp):
                    if mode == "F" and l == 0:
                        mod_run(2, 3, getw_m)
                        if X.mod_todo.get(2, 0) >= 48:
                            mod_run(3, 3, getw_m)
                    cs = cs_r.next()
                    for half in range(2):
                        psc = psn()
                        for q in range(4):
                            gt = half * 4 + q
                            dg = dg_r.next()
                            P.op("dve", lambda e: e.tensor_scalar(out=dg.ap, in0=ident_f, scalar1=comb.ap[:, gt, ex:ex + 1], scalar2=None, op0=ALU.mult),
                                 reads=[consts, (comb, gt)], writes=[dg])
                            P.op("pe", lambda e: e.matmul(psc.ap[:, q * 128:(q + 1) * 128], lhsT=ones_f, rhs=dg.ap, start=True, stop=True),
                                 reads=[consts, dg], writes=[psc])
                        P.op("act", lambda e: e.activation(out=cs.ap[:, half * 512:(half + 1) * 512], in_=psc.ap, func=AF.Copy), reads=[psc], writes=[(cs, half)])
                    wgv = moe_w_gate[l, ex].rearrange("(kc p) n -> p kc n", p=128)
                    wuv = moe_w_up[l, ex].rearrange("(kc p) n -> p kc n", p=128)
                    for hc in range(4):
                        wg = mring.next()
                        P.dma("pool", wg.ap.rearrange("p (k n) -> p k n", n=128), wgv[:, :, hc * 128:(hc + 1) * 128], writes=[wg])
                        wu = mring.next()
                        P.dma("pool", wu.ap.rearrange("p (k n) -> p k n", n=128), wuv[:, :, hc * 128:(hc + 1) * 128], writes=[wu])
                        for th in range(2):
                            psg = psn()
                            psu = psn()
                            for (psx, wx) in ((psg, wg), (psu, wu)):
                                for kc in range(KC):
                                    P.op("pe", lambda e: e.matmul(psx.ap, lhsT=wx.ap[:, kc * 128:(kc + 1) * 128], rhs=hT.ap[:, kc, th * 512:(th + 1) * 512],
                                                                  start=(kc == 0), stop=(kc == KC - 1)), reads=[wx, hT], writes=[psx])
                            sg = sg_r.next()
                            P.op("act", lambda e: e.activation(out=sg.ap, in_=psg.ap, func=AF.Silu), reads=[psg], writes=[sg])
                            t1 = t1_r.next()
                            P.op("dve", lambda e: e.tensor_tensor(out=t1.ap, in0=psu.ap, in1=sg.ap, op=ALU.mult), reads=[psu, sg], writes=[t1])
                            P.op("dve", lambda e: e.tensor_tensor(out=aT.ap[:, hc, th * 512:(th + 1) * 512], in0=t1.ap, in1=cs.ap[:, th * 512:(th + 1) * 512], op=ALU.mult),
                                 reads=[t1, (cs, th)], writes=[(aT, (hc, th))])
                    wds = []
                    for hc in range(4):
                        wd = mring.next()
                        P.dma("pool", wd.ap, moe_w_down[l, ex, hc * 128:(hc + 1) * 128, :], writes=[wd])
                        wds.append(wd)
                    for fc in range(KC):
                        for th in range(2):
                            ps = psn()
                            for hc in range(4):
                                P.op("pe", lambda e: e.matmul(ps.ap, lhsT=wds[hc].ap[:, fc * 128:(fc + 1) * 128], rhs=aT.ap[:, hc, th * 512:(th + 1) * 512],
                                                              start=(hc == 0), stop=(hc == 3)), reads=[wds[hc], (aT, (hc, th))], writes=[ps])
                            ysl = yacc.ap[:, fc, th * 512:(th + 1) * 512]
                            if ex == 0:
                                P.op("dve", lambda e: e.tensor_copy(out=ysl, in_=ps.ap), reads=[ps], writes=[(yacc, (fc, th))])
                            else:
                                P.op("dve", lambda e: e.tensor_tensor(out=ysl, in0=ps.ap, in1=ysl, op=ALU.add), reads=[ps, (yacc, (fc, th))], writes=[(yacc, (fc, th))])
                            if ex == n_exp - 1:
                                xo = sg_r.next()
                                P.dma("sp", xo.ap, xin.ap[:, fc, th * 512:(th + 1) * 512], reads=[xin], writes=[xo])
                                res = t1_r.next()
                                P.op("dve", lambda e: e.scalar_tensor_tensor(out=res.ap, in0=ysl, scalar=modG.ap[:, li, fc:fc + 1],
                                                                              in1=xo.ap, op0=ALU.mult, op1=ALU.add), reads=[(yacc, (fc, th)), xo, (modG, li)], writes=[res])
                                P.dma("sp", xout.ap[:, fc, th * 512:(th + 1) * 512], res.ap, reads=[res], writes=[(xout, (fc, th))])
                P.barrier()
            return
            with ExitStack() as sto:
                xo_r = Ring(sto, f"mxo{l}_", 3, [128, 512], F32)
                res_r = Ring(sto, f"mres{l}_", 3, [128, 512], F32)
                for fc in range(KC):
                    for th in range(2):
                        xo = xo_r.next()
                        P.dma("sp", xo.ap, xin.ap[:, fc, th * 512:(th + 1) * 512], reads=[xin], writes=[xo])
                        res = res_r.next()
                        P.op("dve", lambda e: e.scalar_tensor_tensor(out=res.ap, in0=yacc.ap[:, fc, th * 512:(th + 1) * 512], scalar=modG.ap[:, li, fc:fc + 1],
                                                                      in1=xo.ap, op0=ALU.mult, op1=ALU.add), reads=[(yacc, (fc, th)), xo, (modG, li)], writes=[res])
                        P.dma("sp", xout.ap[:, fc, th * 512:(th + 1) * 512], res.ap, reads=[res], writes=[(xout, (fc, th))])
                P.barrier()

    if mode == "B" or SKIP_L0:
        x2T = Buf("x2T", None, din("x2T_in", [128, KC, T_OWN]))
    else:
        x2T = dscratch("x2T", [128, KC, T_OWN], out=(mode == "A"))
    if upto >= 2 and L0:
        if mode == "F":
            with ExitStack() as stq:
                rq = Ring(stq, "wq1r", 2, [128, KC, 128])
                mod_run(1, None, lambda: (lambda b: (b, b.ap))(rq.next()))
        moe_layer(0, x1T, x2T, n_exp=X_NEXP)
        if mode == "F":
            with ExitStack() as stq:
                rq = Ring(stq, "wq2r", 2, [128, KC, 128])
                mod_run(2, None, lambda: (lambda b: (b, b.ap))(rq.next()))
                mod_run(3, None, lambda: (lambda b: (b, b.ap))(rq.next()))
                P.barrier()
        X.final_src = x2T
    if "x2" in dbg and L0:
        d = dbg_tensor("x2", [128, KC, T_OWN])
        P.dma("sp", d.ap, x2T.ap, reads=[x2T], writes=[d])


    od_w_in = din("od_w_in", [D, 12288])
    od_w_out = din("od_w_out", [4096, D])
    ret_c_in = din("ret_consts", [128, 8 + 2048])
    GAM = [1.0 - 2.0 ** (-5.0 - h) for h in range(8)]
    KSC = 256 ** -0.5
    pos_own = pos_in[:, OWN0:T_ALL]
    toks_loc = [(0, 512), (512, 512)]

    def ret_common(st, cached=False):
        R = Ctx()
        R.hT = salloc(st, "hT1", [128, KC, T_OWN], BF16)
        R.rc = salloc(st, "retc", [128, 8 + 2048], F32)
        P.dma("sp", R.rc.ap, ret_c_in, writes=[R.rc])
        if cached:
            P.dma("sp", R.hT.ap, c_hT.ap, reads=[c_hT], writes=[R.hT])
            R.sin = salloc(st, "sinr2", [128, T_OWN], F32)
            R.cos = salloc(st, "cosr2", [128, T_OWN], F32)
            P.dma("sp", R.cos.ap, c_cs.ap[0], reads=[(c_cs, 0)], writes=[R.cos])
            P.dma("sp", R.sin.ap, c_cs.ap[1], reads=[(c_cs, 1)], writes=[R.sin])
        else:
            with ExitStack() as stp:
                prologue(stp, 2, x2T, T_OWN, R.hT)
                P.barrier()
            with ExitStack() as st_t:
                R.cos, R.sin = rope_tables(st, st_t, "r", 128, 1, T_OWN, pos_own)
                P.barrier()
        R.ring = Ring(st, "wr1_", 4, [128, KC, 128])
        R.ring512 = Ring(st, "wr1b_", 2, [128, KC, 512])
        R.kT = salloc(st, "rkT", [128, 2, T_OWN], BF16)
        R.kd = salloc(st, "rkd", [128, 8, 256], BF16)
        R.v = salloc(st, "rv", [128, 8, 512], BF16)
        R.S = salloc(st, "rS", [128, 2, 512], F32)
        R.ropetmp = Ring(st, "rt1_", 4, [128, 512], F32)
        return R

    def ret_proj_rope(R, col0, outT, scale):
        wv = od_w_in.rearrange("(kc p) n -> p kc n", p=128)
        w1 = wload(R.ring, wv[:, :, col0:col0 + 128], KC, 128)
        w2 = wload(R.ring, wv[:, :, col0 + 128:col0 + 256], KC, 128)
        for (t0, n) in toks_loc:
            ps1 = psn()
            ps2 = psn()
            for (psx, wx) in ((ps1, w1), (ps2, w2)):
                for kc in range(KC):
                    P.op("pe", lambda e: e.matmul(psx.ap[:, 0:n], lhsT=wx.ap[:, kc, :], rhs=R.hT.ap[:, kc, t0:t0 + n], start=(kc == 0), stop=(kc == KC - 1)),
                         reads=[wx, R.hT], writes=[psx])
            rope_apply(R.ropetmp, ps1.ap[:, 0:n], ps2.ap[:, 0:n], [ps1, ps2], R.cos.ap[:, t0:t0 + n], R.sin.ap[:, t0:t0 + n],
                       outT.ap[:, 0, t0:t0 + n], outT.ap[:, 1, t0:t0 + n], [(outT, t0)], 128, n, scale=scale)

    def ret_kv(R, h):
        ret_proj_rope(R, 2048 + h * 256, R.kT, KSC)

        def ev_v(ps, col, cw, t0):
            P.op("act", lambda e: e.activation(out=R.v.ap[:, t0 // 128, col:col + cw], in_=ps.ap[:, 0:cw], func=AF.Copy),
                 reads=[ps], writes=[(R.v, t0 // 128)])
        linTok(R.ring512, od_w_in, 4096 + h * 512, 512, R.hT, KC, list(range(0, T_OWN, 128)), ev_v, piece=512)
        for gt in range(8):
            for dc in range(2):
                ps = psn()
                P.op("pe", lambda e: e.matmul(ps.ap[:, 0:128], lhsT=R.kT.ap[:, dc, gt * 128:(gt + 1) * 128], rhs=ident_b.ap, start=True, stop=True),
                     reads=[R.kT, ident_b], writes=[ps])
                P.op("dve", lambda e: e.tensor_scalar(out=R.kd.ap[:, gt, dc * 128:(dc + 1) * 128], in0=ps.ap[:, 0:128], scalar1=R.rc.ap[:, h:h + 1], scalar2=None, op0=ALU.mult),
                     reads=[ps, R.rc], writes=[(R.kd, gt)])

    def ret_state_update(R, h, gt, first):
        for dc in range(2):
            ps = psn()
            P.op("pe", lambda e: e.matmul(ps.ap, lhsT=R.kd.ap[:, gt, dc * 128:(dc + 1) * 128], rhs=R.v.ap[:, gt, :], start=True, stop=True),
                 reads=[(R.kd, gt), (R.v, gt)], writes=[ps])
            if first:
                P.op("dve", lambda e: e.tensor_copy(out=R.S.ap[:, dc, :], in_=ps.ap), reads=[ps], writes=[(R.S, dc)])
            else:
                P.op("dve", lambda e: e.scalar_tensor_tensor(out=R.S.ap[:, dc, :], in0=R.S.ap[:, dc, :], scalar=GAM[h] ** 128, in1=ps.ap, op0=ALU.mult, op1=ALU.add),
                     reads=[ps, (R.S, dc)], writes=[(R.S, dc)])

    state_out = dscratch("state_out", [8, 128, 2, 512], F32, out=(mode == "A"))
    x3T = dscratch("x3T", [128, KC, T_OWN])
    x4T = dscratch("x4T", [128, KC, T_OWN])

    CACHE = (mode == "F")
    if CACHE:
        c_kT = dscratch("c_kT", [8, 128, 2, T_OWN], BF16)
        c_kd = dscratch("c_kd", [8, 128, 8, 256], BF16)
        c_v = dscratch("c_v", [8, 128, 8, 512], BF16)
        c_hT = dscratch("c_hT", [128, KC, T_OWN], BF16)
        c_cs = dscratch("c_cs", [2, 128, T_OWN], F32)
    if upto >= 3 and mode in ("A", "F"):
        with ExitStack() as st:
            R = ret_common(st)
            if CACHE:
                P.dma("sp", c_hT.ap, R.hT.ap, reads=[R.hT], writes=[c_hT])
                P.dma("sp", c_cs.ap[0], R.cos.ap, reads=[R.cos], writes=[(c_cs, 0)])
                P.dma("sp", c_cs.ap[1], R.sin.ap, reads=[R.sin], writes=[(c_cs, 1)])
            for h in range(NHEADS):
                ret_kv(R, h)
                if CACHE:
                    P.dma("sp", c_kT.ap[h], R.kT.ap, reads=[R.kT], writes=[(c_kT, h)])
                    P.dma("sp", c_kd.ap[h], R.kd.ap, reads=[R.kd], writes=[(c_kd, h)])
                    P.dma("sp", c_v.ap[h], R.v.ap, reads=[R.v], writes=[(c_v, h)])
                for gt in range(8):
                    ret_state_update(R, h, gt, gt == 0)
                P.dma("sp", state_out.ap[h], R.S.ap, reads=[R.S], writes=[(state_out, h)])
            P.barrier()

    if mode == "B":
        state_in = Buf("state_in", None, din("state_in", [8, 128, 2, 512]))
    elif mode == "F":
        state_in_h = []
        for h in range(NHEADS):
            gt_ = nc.dram_tensor(f"state_gath{h}", [256, 1024], F32, kind="Internal")
            gath = Buf(f"state_gath{h}", gt_, gt_.ap())
            if NO_CC:
                P.dma("sp", gath.ap[0:128, :], state_out.ap[h].rearrange("p d e -> p (d e)"), reads=[(state_out, h)], writes=[gath])
            else:
                P.op("pool", lambda e: e.collective_compute("AllGather", ALU.bypass, replica_groups=[[0, 1], [2, 3], [4, 5], [6, 7]],
                                                            ins=[state_out.ap[h].rearrange("p d e -> p (d e)")], outs=[gath.ap]),
                     reads=[(state_out, h)], writes=[gath])
                P.flag_last("pool")
            state_in_h.append(gath)
    og_s = dscratch("og_s", [128, 32, T_OWN], BF16)
    if upto >= 4 and mode in ("B", "F"):
        with ExitStack() as st:
            R = ret_common(st, cached=CACHE)
            ogh_r = Ring(st, "rogh_", 2, [128, 4, T_OWN], BF16)
            qT = salloc(st, "rqT", [128, 2, T_OWN], BF16)
            qdT = salloc(st, "rqdT", [128, 2, T_OWN], BF16)
            gT = salloc(st, "rgT", [128, 4, T_OWN], BF16)
            Sb = salloc(st, "rSb", [128, 8, 1024], BF16)
            sc_r = Ring(st, "rsc_", 3, [128, 128], BF16)
            sq_r = Ring(st, "rsq_", 3, [128, 512], F32)
            rs_r = Ring(st, "rrs_", 2, [128, 128], F32)
            rstd_r = Ring(st, "rrstd_", 2, [128, 128], F32)
            tmp_r = Ring(st, "rtmp_", 3, [128, 128], F32)
            for h in range(NHEADS):
                if CACHE:
                    P.dma("sp", R.kT.ap, c_kT.ap[h], reads=[(c_kT, h)], writes=[R.kT])
                    P.dma("sp", R.kd.ap, c_kd.ap[h], reads=[(c_kd, h)], writes=[R.kd])
                    P.dma("sp", R.v.ap, c_v.ap[h], reads=[(c_v, h)], writes=[R.v])
                else:
                    ret_kv(R, h)
                ret_proj_rope(R, h * 256, qT, 1.0)
                for gt in range(8):
                    for dc in range(2):
                        P.op("dve", lambda e: e.tensor_tensor(out=qdT.ap[:, dc, gt * 128:(gt + 1) * 128], in0=qT.ap[:, dc, gt * 128:(gt + 1) * 128],
                                                               in1=R.rc.ap[:, 8 + h * 128:8 + (h + 1) * 128], op=ALU.mult), reads=[qT, R.rc], writes=[(qdT, gt)])

                def ev_g(ps, col, m, t0, n):
                    P.op("act", lambda e: e.activation(out=gT.ap[:, col // 128, t0:t0 + n], in_=ps.ap[:, 0:n], func=AF.Silu),
                         reads=[ps], writes=[(gT, (col // 128, t0))])
                linT(R.ring, od_w_in, 8192 + h * 512, 512, R.hT, KC, toks_loc, ev_g, piece=128)
                if mode == "F":
                    P.dma("sp", R.S.ap, state_in_h[h].ap[0:128, :].rearrange("p (d e) -> p d e", e=512), reads=[state_in_h[h]], writes=[R.S])
                else:
                    P.dma("sp", R.S.ap, state_in.ap[h], reads=[(state_in, h)], writes=[R.S])
                P.op("dve", lambda e: e.tensor_scalar(out=R.S.ap, in0=R.S.ap, scalar1=pvalid.ap[:, 0:1], scalar2=None, op0=ALU.mult), reads=[R.S, pvalid], writes=[R.S])
                for gt in range(8):
                    P.op("act", lambda e: e.activation(out=Sb.ap[:, gt, :], in_=R.S.ap.rearrange("p d e -> p (d e)"), func=AF.Copy), reads=[R.S], writes=[(Sb, gt)])
                    if gt < 7:
                        ret_state_update(R, h, gt, False)
                ogh = ogh_r.next()
                cst = [dict() for _ in range(8)]

                def r_t1(gt):
                    ts = slice(gt * 128, (gt + 1) * 128)
                    pss = psn()
                    for dc in range(2):
                        P.op("pe", lambda e: e.matmul(pss.ap[:, 0:128], lhsT=R.kT.ap[:, dc, ts], rhs=qT.ap[:, dc, ts], start=(dc == 0), stop=(dc == 1)),
                             reads=[R.kT, qT], writes=[pss])
                    sc = sc_r.next()
                    P.op("dve", lambda e: e.tensor_tensor(out=sc.ap, in0=pss.ap[:, 0:128], in1=R.rc.ap[:, 8 + 1024 + h * 128:8 + 1024 + (h + 1) * 128], op=ALU.mult),
                         reads=[pss, R.rc], writes=[sc])
                    cst[gt]["sc"] = sc

                def r_t2(gt):
                    ts = slice(gt * 128, (gt + 1) * 128)
                    sc = cst[gt]["sc"]
                    pso = psn()
                    for ec in range(4):
                        es = slice(ec * 128, (ec + 1) * 128)
                        P.op("pe", lambda e: e.matmul(pso.ap[:, es], lhsT=R.v.ap[:, gt, es], rhs=sc.ap, start=True, stop=False), reads=[(R.v, gt), sc], writes=[pso])
                        P.op("pe", lambda e: e.matmul(pso.ap[:, es], lhsT=Sb.ap[:, gt, ec * 128:(ec + 1) * 128], rhs=qdT.ap[:, 0, ts], start=False, stop=False),
                             reads=[(Sb, gt), (qdT, gt)], writes=[pso])
                        P.op("pe", lambda e: e.matmul(pso.ap[:, es], lhsT=Sb.ap[:, gt, 512 + ec * 128:512 + (ec + 1) * 128], rhs=qdT.ap[:, 1, ts], start=False, stop=True),
                             reads=[(Sb, gt), (qdT, gt)], writes=[pso])
                    sq = sq_r.next()
                    P.op("act", lambda e: e.activation(out=sq.ap, in_=pso.ap, func=AF.Square), reads=[pso], writes=[sq])
                    cst[gt].update(pso=pso, sq=sq)

                def r_t3(gt):
                    ts = slice(gt * 128, (gt + 1) * 128)
                    pso, sq = cst[gt]["pso"], cst[gt]["sq"]
                    psq = psn()
                    for ec in range(4):
                        P.op("pe", lambda e: e.matmul(psq.ap[:, 0:128], lhsT=ones_f, rhs=sq.ap[:, ec * 128:(ec + 1) * 128], start=(ec == 0), stop=(ec == 3)),
                             reads=[sq, consts], writes=[psq])
                    rs = rs_r.next()
                    P.op("act", lambda e: e.activation(out=rs.ap, in_=psq.ap[:, 0:128], func=AF.Sqrt, scale=1.0 / 512, bias=EPS), reads=[psq], writes=[rs])
                    rstd = rstd_r.next()
                    P.op("dve", lambda e: e.reciprocal(out=rstd.ap, in_=rs.ap), reads=[rs], writes=[rstd])
                    for ec in range(4):
                        tm = tmp_r.next()
                        P.op("dve", lambda e: e.tensor_tensor(out=tm.ap, in0=pso.ap[:, ec * 128:(ec + 1) * 128], in1=rstd.ap, op=ALU.mult), reads=[pso, rstd], writes=[tm])
                        P.op("dve", lambda e: e.tensor_tensor(out=ogh.ap[:, ec, ts], in0=tm.ap, in1=gT.ap[:, ec, ts], op=ALU.mult),
                             reads=[tm, gT], writes=[(ogh, (ec, gt))])
                for step in range(8 + 2):
                    if step < 8:
                        r_t1(step)
                    if 0 <= step - 1 < 8:
                        r_t2(step - 1)
                    if 0 <= step - 2 < 8:
                        r_t3(step - 2)
                P.dma("sp", og_s.ap[:, h * 4:(h + 1) * 4, :], ogh.ap, reads=[ogh], writes=[(og_s, h)])
            P.barrier()
        with ExitStack() as st:
            ogT = salloc(st, "ogT", [128, 32, T_OWN], BF16)
            P.dma("sp", ogT.ap, og_s.ap, reads=[og_s], writes=[ogT])
            ring32 = Ring(st, "wr32_", 3, [128, 32, 128])
            xo_r = Ring(st, "rxo_", 3, [128, 512], F32)
            res_r = Ring(st, "rres_", 3, [128, 512], F32)

            def ev_out1(ps, col, m, t0, n):
                fc = col // 128
                xo = xo_r.next()
                P.dma("sp", xo.ap[:, 0:n], x2T.ap[:, fc, t0:t0 + n], reads=[x2T], writes=[xo])
                res = res_r.next()
                P.op("dve", lambda e: e.scalar_tensor_tensor(out=res.ap[:, 0:n], in0=ps.ap[:, 0:n], scalar=modG.ap[:, 2, fc:fc + 1], in1=xo.ap[:, 0:n],
                                                              op0=ALU.mult, op1=ALU.add), reads=[ps, xo, (modG, 2)], writes=[res])
                P.dma("sp", x3T.ap[:, fc, t0:t0 + n], res.ap[:, 0:n], reads=[res], writes=[(x3T, (fc, t0))])
            linT(ring32, od_w_out, 0, D, ogT, 32, toks_loc, ev_out1, piece=128)
            P.barrier()
    if "x3" in dbg:
        d = dbg_tensor("x3", [128, KC, T_OWN])
        P.dma("sp", d.ap, x3T.ap, reads=[x3T], writes=[d])
    if upto >= 5 and mode in ("B", "F"):
        moe_layer(1, x3T, x4T, n_exp=X_NEXP)
        X.final_src = x4T
    if "x4" in dbg:
        d = dbg_tensor("x4", [128, KC, T_OWN])
        P.dma("sp", d.ap, x4T.ap, reads=[x4T], writes=[d])
    if upto >= 6 and mode in ("B", "F"):
        with ExitStack() as st:
            xs_r = Ring(st, "fxs_", 2, [128, KC, 512], F32)
            fo_r = Ring(st, "fo_", 3, [128, 512], F32)
            cur = {}

            def getx(c, tb):
                if c == 0:
                    xs = xs_r.next()
                    P.dma("sp", xs.ap, x4T.ap[:, :, tb * 512:(tb + 1) * 512], reads=[x4T], writes=[xs])
                    cur["xs"] = xs
                return cur["xs"].ap[:, c, :], [cur["xs"]]

            def emit(tm, c, tb):
                fo = fo_r.next()
                P.op("act", lambda e: e.activation(out=fo.ap, in_=tm.ap, func=AF.Copy, scale=norms.ap[:, 4, c:c + 1]), reads=[tm, norms], writes=[fo])
                P.dma("sp", outT.ap[:, c, tb * 512:(tb + 1) * 512], fo.ap, reads=[fo], writes=[(outT, (c, tb))])
            rmsnorm_T(st, "fin", getx, KC, T_OWN // 512, D, emit)
            P.barrier()
        X.final_src = None
    X.P = P
    X.nc = nc
    X.dbg_out = dbg_out
    return X, locals()


def _fm(v):
    v = np.asarray(v, np.float32)
    n = v.shape[-1] // 128
    return np.ascontiguousarray(v.reshape(n, 128).T)


def _xT(xb):
    T = xb.shape[0]
    return np.ascontiguousarray(xb.T.reshape(KC, 128, T).transpose(1, 0, 2))


def shared_inputs(I):
    S = {}
    S["consts"] = host_consts()
    S["w_mod"] = np.ascontiguousarray(np.stack([I["w_mod_mix"][0], I["w_mod_ffn"][0], I["w_mod_mix"][1], I["w_mod_ffn"][1]]))
    bm = [I["b_mod_mix"][0], I["b_mod_ffn"][0], I["b_mod_mix"][1], I["b_mod_ffn"][1]]
    S["b_mod"] = np.ascontiguousarray(np.stack([_fm(b) for b in bm], axis=1))
    nm = [I["norm_mix"][0], I["norm_ffn"][0], I["norm_mix"][1], I["norm_ffn"][1], I["final_norm"]]
    S["norms"] = np.ascontiguousarray(np.stack([_fm(b) for b in nm], axis=1))
    S["ev_w_in"] = np.ascontiguousarray(I["ev_w_in"][0])
    S["ev_q_norm"] = _fm(I["ev_q_norm"][0])
    S["ev_w_q_up"] = np.ascontiguousarray(I["ev_w_q_up"][0])
    S["ev_kv_norm"] = _fm(I["ev_kv_norm"][0])
    S["ev_w_kv_up"] = np.ascontiguousarray(I["ev_w_kv_up"][0])
    S["ev_w_out"] = np.ascontiguousarray(I["ev_w_out"][0])
    invf = np.zeros((128, 2), np.float32)
    p = np.arange(128)
    invf[:, 0] = np.exp(-math.log(10000.0) * (p % 32).astype(np.float32) / np.float32(32)).astype(np.float32)
    invf[:, 1] = np.exp(-math.log(10000.0) * p.astype(np.float32) / np.float32(128)).astype(np.float32)
    S["invf"] = invf
    rc = np.zeros((128, 8 + 2048), np.float64)
    idx = np.arange(128, dtype=np.float64)
    for h in range(8):
        lg = np.log1p(-(2.0 ** (-5.0 - h)))
        rc[:, h] = np.exp((127.0 - idx) * lg)
        rc[:, 8 + h * 128:8 + (h + 1) * 128] = np.exp((idx + 1.0) * lg)[None, :]
        rel = idx[None, :] - idx[:, None]
        rc[:, 8 + 1024 + h * 128:8 + 1024 + (h + 1) * 128] = np.where(rel >= 0, np.exp(np.maximum(rel, 0.0) * lg), 0.0)
    S["ret_consts"] = rc.astype(np.float32)
    S["od_w_in"] = np.ascontiguousarray(I["od_w_in"][0])
    S["od_w_out"] = np.ascontiguousarray(I["od_w_out"][0])
    S["moe_wr"] = np.ascontiguousarray(np.concatenate([I["moe_w_group"], I["moe_w_expert"]], axis=-1))
    S["moe_br"] = np.ascontiguousarray(np.concatenate([I["moe_b_group"], I["moe_b_expert"]], axis=-1)[:, None, :])
    S["moe_w_gate"] = np.asarray(I["moe_w_gate"])
    S["moe_w_up"] = np.asarray(I["moe_w_up"])
    S["moe_w_down"] = np.asarray(I["moe_w_down"])
    return S


def core_inputs(I, core):
    b, hf = core // 2, core % 2
    x = np.asarray(I["x"][b], np.float32)
    pos = np.asarray(I["positions"][b], np.int32)
    M = {}
    if hf == 0:
        xa = np.concatenate([np.zeros((T_OWN, D), np.float32), x[:T_OWN]], axis=0)
        pa = np.concatenate([np.zeros((T_OWN,), np.int32), pos[:T_OWN]])
    else:
        xa = x
        pa = pos
    M["xT_all"] = _xT(xa)
    M["pos_all"] = np.ascontiguousarray(pa.reshape(1, T_ALL))
    M["pvalid"] = np.full((128, 1), float(hf), np.float32)
    M["cond"] = _fm(I["c"][b])
    return M


def _tok(a):
    a = np.asarray(a)
    p, c, t = a.shape
    return np.ascontiguousarray(a.transpose(2, 1, 0).reshape(t, c * p))


FUSED = True


def kernel(**inputs):
    I = {k: np.asarray(v) for k, v in inputs.items()}
    n = 8
    S = shared_inputs(I)
    cores = [core_inputs(I, c) for c in range(n)]
    if FUSED:
        X, _ = build(mode="F")
        X.P.finish()
        maps = [{k: (cores[c][k] if k in cores[c] else S[k]) for k in X.in_names} for c in range(n)]
        res = run_bass_kernel_spmd(X.nc, maps, core_ids=list(range(n)))
        outs = [res.results[c]["outT"] for c in range(n)]
    else:
        XA, _ = build(upto=3, mode="A")
        XA.P.finish()
        maps = [{k: (cores[c][k] if k in cores[c] else S[k]) for k in XA.in_names} for c in range(n)]
        resA = run_bass_kernel_spmd(XA.nc, maps, core_ids=list(range(n)))
        XB, _ = build(mode="B")
        XB.P.finish()
        maps = []
        for c in range(n):
            M = dict(cores[c])
            M["x2T_in"] = np.asarray(resA.results[c]["x2T"])
            M["state_in"] = (np.asarray(resA.results[c - 1]["state_out"]) if c % 2 == 1
                             else np.zeros((8, 128, 2, 512), np.float32))
            maps.append({k: (M[k] if k in M else S[k]) for k in XB.in_names})
        resB = run_bass_kernel_spmd(XB.nc, maps, core_ids=list(range(n)))
        outs = [resB.results[c]["outT"] for c in range(n)]
    out = np.zeros((4, 2048, 2048), np.float32)
    for c in range(n):
        b, hf = c // 2, c % 2
        out[b, hf * T_OWN:(hf + 1) * T_OWN, :] = _tok(outs[c])
    return out
```

```python
import math
import bisect
import numpy as np
import concourse.bass as bass
import concourse.mybir as mybir
from concourse.bass_utils import run_bass_kernel_spmd

F32 = mybir.dt.float32
BF16 = mybir.dt.bfloat16
I32 = mybir.dt.int32
AF = mybir.ActivationFunctionType
ALU = mybir.AluOpType

D = 2048
KC = 16
T_OWN = 1024
T_ALL = 2048
SAME_SYNC = True


class Buf:
    def __init__(self, name, t, ap=None):
        self.name = name
        self.t = t
        self._ap = ap if ap is not None else t.ap()
        self.st = {}

    @property
    def ap(self):
        return self._ap

    def state(self, key):
        s = self.st.get(key)
        if s is None:
            s = [None, []]
            self.st[key] = s
        return s


class Eng:
    def __init__(self, nc, name, eng, ndma=0):
        self.name = name
        self.eng = eng
        self.sem = nc.alloc_semaphore("sem_" + name)
        self.insts = []
        self.snaps = []
        self.flag_seqs = []
        self.flag_vals = []
        self.count = 0
        self.clock = {}
        self.ndma = ndma
        self.dsems = [nc.alloc_semaphore(f"dsem_{name}_{i}") for i in range(ndma)]
        self.dcount = 0
        self.dsnaps = {}


class Prog:
    def __init__(self, nc):
        self.nc = nc
        self.E = {
            "pe": Eng(nc, "pe", nc.tensor),
            "act": Eng(nc, "act", nc.scalar),
            "dve": Eng(nc, "dve", nc.vector),
            "pool": Eng(nc, "pool", nc.gpsimd, ndma=8),
            "sp": Eng(nc, "sp", nc.sync, ndma=8),
        }

    def _known(self, e, ev):
        if ev[0] == "D":
            _, q, i = ev
            Q = self.E[q]
            return e.clock.get(("D", q, i % Q.ndma), -1) >= i
        p, seq = ev
        return e.clock.get(p, -1) >= seq

    def _merge(self, e, other, extra):
        c = dict(e.clock)
        if other:
            for k, v in other.items():
                if c.get(k, -1) < v:
                    c[k] = v
        for k, v in extra.items():
            if c.get(k, -1) < v:
                c[k] = v
        e.clock = c

    def wait(self, ename, ev):
        e = self.E[ename]
        if ev is None or self._known(e, ev):
            return
        if ev[0] == "D":
            _, q, i = ev
            Q = self.E[q]
            slot = i % Q.ndma
            e.eng.wait_ge(Q.dsems[slot], 16 * (i // Q.ndma + 1))
            self._merge(e, Q.dsnaps.get(i), {("D", q, slot): i})
            return
        p, seq = ev
        if p == ename and (not SAME_SYNC or p in ("pe", "sp")):
            return
        Pn = self.E[p]
        k = bisect.bisect_left(Pn.flag_seqs, seq)
        if k < len(Pn.flag_seqs):
            fseq, val = Pn.flag_seqs[k], Pn.flag_vals[k]
        else:
            Pn.count += 1
            val = Pn.count
            fseq = seq
            Pn.insts[seq].then_inc(Pn.sem, 1)
            Pn.flag_seqs.append(seq)
            Pn.flag_vals.append(val)
        e.eng.wait_ge(Pn.sem, val)
        self._merge(e, Pn.snaps[fseq], {p: fseq})

    def flag_last(self, ename):
        Pn = self.E[ename]
        seq = len(Pn.insts) - 1
        if Pn.flag_seqs and Pn.flag_seqs[-1] == seq:
            return
        Pn.count += 1
        Pn.insts[seq].then_inc(Pn.sem, 1)
        Pn.flag_seqs.append(seq)
        Pn.flag_vals.append(Pn.count)

    def _deps(self, reads, writes):
        deps = []
        for b, k in reads:
            keys = [k] if k is not None else list(b.st.keys()) + [None]
            for kk in set(keys + [None]):
                s = b.st.get(kk)
                if s and s[0] is not None:
                    deps.append(s[0])
        for b, k in writes:
            keys = [k] if k is not None else list(b.st.keys()) + [None]
            for kk in set(keys + [None]):
                s = b.st.get(kk)
                if s:
                    if s[0] is not None:
                        deps.append(s[0])
                    deps.extend(s[1])
        return deps

    def _update(self, ev, reads, writes):
        for b, k in reads:
            b.state(k)[1].append(ev)
        for b, k in writes:
            if k is None:
                b.st = {}
            s = b.state(k)
            s[0] = ev
            s[1] = []

    @staticmethod
    def _norm(lst):
        out = []
        for x in lst:
            if isinstance(x, Buf):
                out.append((x, None))
            else:
                out.append(x)
        return out

    def op(self, ename, fn, reads=(), writes=()):
        reads = self._norm(reads)
        writes = self._norm(writes)
        e = self.E[ename]
        for d in self._deps(reads, writes):
            self.wait(ename, d)
        inst = fn(e.eng)
        seq = len(e.insts)
        e.insts.append(inst)
        e.snaps.append(e.clock)
        ev = (ename, seq)
        self._update(ev, reads, writes)
        return ev

    def dma(self, qname, out, in_, reads=(), writes=()):
        reads = self._norm(reads)
        writes = self._norm(writes)
        q = self.E[qname]
        for d in self._deps(reads, writes):
            self.wait(qname, d)
        i = q.dcount
        q.dcount += 1
        if i >= q.ndma:
            self.wait(qname, ("D", qname, i - q.ndma))
        slot = i % q.ndma
        q.eng.dma_start(out=out, in_=in_).then_inc(q.dsems[slot], 16)
        q.dsnaps[i] = q.clock
        ev = ("D", qname, i)
        self._update(ev, reads, writes)
        return ev

    def barrier(self, bufs=()):
        evs = []
        for n, e in self.E.items():
            if e.insts:
                evs.append((n, len(e.insts) - 1))
            for i in range(max(0, e.dcount - e.ndma), e.dcount):
                evs.append(("D", n, i))
        for n in self.E:
            for ev in evs:
                if ev[0] != "D" and ev[0] == n:
                    continue
                self.wait(n, ev)

    def finish(self):
        evs = []
        for n, e in self.E.items():
            if e.insts:
                evs.append((n, len(e.insts) - 1))
            for i in range(max(0, e.dcount - e.ndma), e.dcount):
                evs.append(("D", n, i))
        for ev in evs:
            if ev[0] == "sp":
                continue
            self.wait("sp", ev)


NCONST = 128 * 3 + 2048 * 2
EPS = 1e-6


def host_consts():
    c = np.zeros((128, NCONST), np.float32)
    c[:, 0:128] = 1.0
    c[:, 128:256] = np.eye(128, dtype=np.float32)
    j = np.arange(128)[:, None]
    s = np.arange(128)[None, :]
    c[:, 256:384] = (j >= s).astype(np.float32)
    q = np.arange(512)[None, :]
    for jb in range(4):
        key = 128 * jb + np.arange(128)[:, None]
        c[:, 384 + jb * 512:384 + (jb + 1) * 512] = (key < q).astype(np.float32)
        c[:, 384 + 2048 + jb * 512:384 + 2048 + (jb + 1) * 512] = (key <= q).astype(np.float32)
    return c


class Ctx:
    pass


X_NEXP = 32
NO_CC = False
SKIP_L0 = False
NHEADS = 8


def build(upto=99, dbg=(), mode="F"):
    from contextlib import ExitStack
    nc = bass.Bass("TRN2", target_bir_lowering=False)
    P = Prog(nc)
    X = Ctx()
    dbg_out = {}

    X.in_names = []

    def din(name, shape, dt=F32):
        X.in_names.append(name)
        return nc.dram_tensor(name, list(shape), dt, kind="ExternalInput").ap()

    def dscratch(name, shape, dt=F32, out=False):
        t = nc.dram_tensor(name, list(shape), dt, kind="ExternalOutput" if out else "Internal")
        return Buf(name, t, t.ap())

    L0 = mode in ("A", "F") and not SKIP_L0
    xT_all = Buf("xT_all", None, din("xT_all", [128, KC, T_ALL])) if L0 else None
    pos_in = din("pos_all", [1, T_ALL], I32)
    pvalid_in = din("pvalid", [128, 1])
    cond_in = din("cond", [128, KC])
    consts_in = din("consts", [128, NCONST])
    w_mod = din("w_mod", [4, D, 3 * D])
    b_mod_in = din("b_mod", [128, 4, 48])
    norms_in = din("norms", [128, 5, KC])
    ev_w_in = din("ev_w_in", [D, 3904]) if L0 else None
    ev_qn_in = din("ev_q_norm", [128, 4]) if L0 else None
    ev_w_q_up = din("ev_w_q_up", [512, 1536]) if L0 else None
    ev_kvn_in = din("ev_kv_norm", [128, 2]) if L0 else None
    ev_w_kv_up = din("ev_w_kv_up", [256, 2048]) if L0 else None
    ev_w_out = din("ev_w_out", [D, D]) if L0 else None
    invf_in = din("invf", [128, 2])
    outT = dscratch("outT", [128, KC, T_OWN], F32, out=True)

    def dbg_tensor(name, shape, dt=F32):
        b = dscratch("dbg_" + name, shape, dt, out=True)
        dbg_out[name] = b
        return b

    gstack = ExitStack()

    def salloc(stack, name, shape, dt):
        X.uid = getattr(X, "uid", 0) + 1
        t = stack.enter_context(nc.sbuf_tensor(f"s{X.uid}_" + name, list(shape), dt))
        return Buf(name, t)

    PS = [Buf(f"ps{i}", gstack.enter_context(nc.psum_tensor(f"ps{i}", [128, 512], F32))) for i in range(8)]
    X.ps_i = 0

    X.ps_mod = 6

    def psn():
        b = PS[X.ps_i % X.ps_mod]
        X.ps_i += 1
        return b

    consts = salloc(gstack, "consts", [128, NCONST], F32)
    P.dma("sp", consts.ap, consts_in, writes=[consts])
    ones_f = consts.ap[:, 0:128]
    ident_f = consts.ap[:, 128:256]
    U_f = consts.ap[:, 256:384]

    def mstrict(j):
        return consts.ap[:, 384 + j * 512:384 + (j + 1) * 512]

    def mincl(j):
        return consts.ap[:, 384 + 2048 + j * 512:384 + 2048 + (j + 1) * 512]

    ones_b = salloc(gstack, "ones_b", [128, 128], BF16)
    P.op("dve", lambda e: e.tensor_copy(out=ones_b.ap, in_=ones_f), reads=[consts], writes=[ones_b])
    ident_b = salloc(gstack, "ident_b", [128, 128], BF16)
    P.op("dve", lambda e: e.tensor_copy(out=ident_b.ap, in_=ident_f), reads=[consts], writes=[ident_b])
    pvalid = salloc(gstack, "pvalid", [128, 1], F32)
    P.dma("sp", pvalid.ap, pvalid_in, writes=[pvalid])
    U_b = salloc(gstack, "U_b", [128, 128], BF16)
    P.op("dve", lambda e: e.tensor_copy(out=U_b.ap, in_=U_f), reads=[consts], writes=[U_b])
    ones_pv = salloc(gstack, "ones_pv", [128, 128], BF16)
    P.op("dve", lambda e: e.tensor_scalar(out=ones_pv.ap, in0=ones_f, scalar1=pvalid.ap[:, 0:1], scalar2=None, op0=ALU.mult), reads=[consts, pvalid], writes=[ones_pv])
    modA = salloc(gstack, "modA", [128, 4, KC], F32)
    modB = salloc(gstack, "modB", [128, 4, KC], F32)
    modG = salloc(gstack, "modG", [128, 4, KC], F32)
    norms = salloc(gstack, "norms", [128, 5, KC], F32)
    P.dma("sp", norms.ap, norms_in, writes=[norms])
    invf = salloc(gstack, "invf", [128, 2], F32)
    P.dma("sp", invf.ap, invf_in, writes=[invf])

    class Ring:
        def __init__(self, stack, name, n, shape, dt=BF16):
            self.b = [salloc(stack, f"{name}{i}", shape, dt) for i in range(n)]
            self.i = 0

        def next(self):
            b = self.b[self.i % len(self.b)]
            self.i += 1
            return b

    def wload(ring, view, kcn, cw):
        wb = ring.next()
        P.dma("pool", wb.ap[:, 0:kcn, 0:cw], view, writes=[wb])
        return wb

    def linT(ring, w2d, c0, ncols, act, kcn, toks, evac, piece=512):
        wv = w2d.rearrange("(kc p) n -> p kc n", p=128)
        for cc in range(c0, c0 + ncols, piece):
            cw = min(piece, c0 + ncols - cc)
            wb = wload(ring, wv[:, :, cc:cc + cw], kcn, cw)
            for oc in range(0, cw, 128):
                m = min(128, cw - oc)
                for (t0, n) in toks:
                    ps = psn()
                    for kc in range(kcn):
                        P.op("pe", lambda e, ps=ps, kc=kc, oc=oc, m=m, t0=t0, n=n, wb=wb: e.matmul(
                            ps.ap[0:m, 0:n], lhsT=wb.ap[:, kc, oc:oc + m], rhs=act.ap[:, kc, t0:t0 + n],
                            start=(kc == 0), stop=(kc == kcn - 1)),
                            reads=[wb, act], writes=[ps])
                    evac(ps, cc + oc - c0, m, t0, n)

    def linTok(ring, w2d, c0, ncols, act, kcn, ttiles, evac, piece=512):
        wv = w2d.rearrange("(kc p) n -> p kc n", p=128)
        for cc in range(c0, c0 + ncols, piece):
            cw = min(piece, c0 + ncols - cc)
            wb = wload(ring, wv[:, :, cc:cc + cw], kcn, cw)
            for t0 in ttiles:
                ps = psn()
                for kc in range(kcn):
                    P.op("pe", lambda e, ps=ps, kc=kc, cw=cw, t0=t0, wb=wb: e.matmul(
                        ps.ap[:, 0:cw], lhsT=act.ap[:, kc, t0:t0 + 128], rhs=wb.ap[:, kc, 0:cw],
                        start=(kc == 0), stop=(kc == kcn - 1)),
                        reads=[wb, act], writes=[ps])
                evac(ps, cc - c0, cw, t0)

    cond = salloc(gstack, "cond", [128, KC], F32)
    P.dma("sp", cond.ap, cond_in, writes=[cond])
    condb = salloc(gstack, "condb", [128, KC], BF16)
    P.op("act", lambda e: e.activation(out=condb.ap, in_=cond.ap, func=AF.Silu), reads=[cond], writes=[condb])
    bmod = salloc(gstack, "bmod", [128, 4, 48], F32)
    P.dma("sp", bmod.ap, b_mod_in, writes=[bmod])
    modT = salloc(gstack, "modT", [128, 4, 48], F32)

    def mod_piece(l, j, get_w):
        wv = w_mod[l].rearrange("(kc p) n -> p kc n", p=128)
        wb, wap = get_w()
        P.dma("pool", wap, wv[:, :, j * 128:(j + 1) * 128], writes=[wb])
        ps = psn()
        for kc in range(KC):
            P.op("pe", lambda e: e.matmul(ps.ap[:, 0:1], lhsT=wap[:, kc, :], rhs=condb.ap[:, kc:kc + 1], start=(kc == 0), stop=(kc == KC - 1)),
                 reads=[wb, condb], writes=[ps])
        P.op("dve", lambda e: e.tensor_tensor(out=modT.ap[:, l, j:j + 1], in0=ps.ap[:, 0:1], in1=bmod.ap[:, l, j:j + 1], op=ALU.add),
             reads=[ps, bmod], writes=[(modT, (l, j))])

    def mod_finalize(l):
        P.op("dve", lambda e: e.scalar_tensor_tensor(out=modA.ap[:, l, :], in0=modT.ap[:, l, 16:32], scalar=1.0,
                                                      in1=norms.ap[:, l, :], op0=ALU.add, op1=ALU.mult),
             reads=[modT, norms], writes=[(modA, l)])
        P.op("dve", lambda e: e.tensor_copy(out=modB.ap[:, l, :], in_=modT.ap[:, l, 0:16]), reads=[modT], writes=[(modB, l)])
        P.op("dve", lambda e: e.tensor_copy(out=modG.ap[:, l, :], in_=modT.ap[:, l, 32:48]), reads=[modT], writes=[(modG, l)])

    X.mod_todo = {}

    def mod_run(l, n, get_w):
        j0 = X.mod_todo.get(l, 0)
        if j0 >= 48:
            return
        j1 = 48 if n is None else min(48, j0 + n)
        for j in range(j0, j1):
            mod_piece(l, j, get_w)
        X.mod_todo[l] = j1
        if j1 >= 48:
            mod_finalize(l)

    with ExitStack() as st:
        ring0 = Ring(st, "w0r", 4, [128, KC, 128])

        def getw0():
            b = ring0.next()
            return b, b.ap
        first_l = [0] if mode in ("A", "F") else [2]
        for l in first_l:
            mod_run(l, None, getw0)
        if mode == "A":
            mod_run(1, None, getw0)
        if mode == "B":
            mod_run(3, None, getw0)
        if "mod" in dbg:
            d = dbg_tensor("mod", [128, 4, 48])
            P.dma("sp", d.ap, modT.ap, reads=[modT], writes=[d])
        P.barrier()

    def rmsnorm_T(st, tag, getx, nch, ntb, dim, emit, npart=128):
        sq_r = Ring(st, f"sq{tag}_", 3, [128, 512], BF16)
        tmp_r = Ring(st, f"tm{tag}_", 3, [128, 512], F32)
        rs = salloc(st, f"rs{tag}", [128, 512], F32)
        rstd = salloc(st, f"rstd{tag}", [128, 512], F32)
        for tb in range(ntb):
            ps = psn()
            xs = [getx(c, tb) for c in range(nch)]
            for c in range(nch):
                sq = sq_r.next()
                xap, xdeps = xs[c]
                P.op("act", lambda e, sq=sq, xap=xap: e.activation(out=sq.ap, in_=xap, func=AF.Square),
                     reads=xdeps, writes=[sq])
                P.op("pe", lambda e, sq=sq, ps=ps, c=c: e.matmul(ps.ap, lhsT=ones_b.ap, rhs=sq.ap, start=(c == 0), stop=(c == nch - 1)),
                     reads=[sq, ones_b], writes=[ps])
            P.op("act", lambda e, ps=ps: e.activation(out=rs.ap, in_=ps.ap, func=AF.Sqrt, scale=1.0 / dim, bias=EPS),
                 reads=[ps], writes=[rs])
            P.op("dve", lambda e: e.reciprocal(out=rstd.ap, in_=rs.ap), reads=[rs], writes=[rstd])
            for c in range(nch):
                tm = tmp_r.next()
                xap, xdeps = xs[c]
                P.op("dve", lambda e, tm=tm, xap=xap: e.tensor_tensor(out=tm.ap, in0=xap, in1=rstd.ap, op=ALU.mult),
                     reads=list(xdeps) + [rstd], writes=[tm])
                emit(tm, c, tb)

    def prologue(st, l, xsrc, T, hT, hook=None):
        xs_r = Ring(st, f"xs{l}_", 2, [128, KC, 512], F32)
        cur = {}

        def getx(c, tb):
            if c == 0:
                xs = xs_r.next()
                P.dma("sp", xs.ap, xsrc.ap[:, :, tb * 512:(tb + 1) * 512], reads=[xsrc], writes=[xs])
                cur["xs"] = xs
            return cur["xs"].ap[:, c, :], [cur["xs"]]

        def emit(tm, c, tb):
            if hook is None:
                P.op("act", lambda e: e.activation(
                    out=hT.ap[:, c, tb * 512:(tb + 1) * 512], in_=tm.ap, func=AF.Identity,
                    scale=modA.ap[:, l, c:c + 1], bias=modB.ap[:, l, c:c + 1]),
                    reads=[tm, (modA, l), (modB, l)], writes=[(hT, (c, tb))])
            else:
                hook(tm, c, tb)
        rmsnorm_T(st, f"p{l}", getx, KC, T // 512, D, emit)

    def rope_tables(st, st_tmp, tag, npart, col, T, pos_ap):
        o_sin = salloc(st, f"sin{tag}", [npart, T], F32)
        o_cos = salloc(st, f"cos{tag}", [npart, T], F32)
        posi = salloc(st_tmp, f"posi{tag}", [npart, T], I32)
        P.dma("sp", posi.ap, pos_ap.broadcast_to([npart, T]), writes=[posi])
        ang = salloc(st_tmp, f"ang{tag}", [npart, T], F32)
        P.op("dve", lambda e: e.tensor_copy(out=ang.ap, in_=posi.ap), reads=[posi], writes=[ang])
        P.op("dve", lambda e: e.tensor_scalar(out=ang.ap, in0=ang.ap, scalar1=invf.ap[0:npart, col:col + 1], scalar2=None, op0=ALU.mult),
             reads=[ang, invf], writes=[ang])
        a = salloc(st_tmp, f"a{tag}", [npart, T], F32)
        t = salloc(st_tmp, f"t{tag}", [npart, T], F32)
        ni = salloc(st_tmp, f"ni{tag}", [npart, T], I32)
        outs = []
        TWO_PI = 2.0 * math.pi
        C1 = 6.28125
        C2 = TWO_PI - C1
        for nm, off in (("sin", 0.0), ("cos", math.pi / 2)):
            o = o_sin if nm == "sin" else o_cos
            P.op("dve", lambda e, off=off: e.tensor_scalar(out=a.ap, in0=ang.ap, scalar1=off, scalar2=None, op0=ALU.add), reads=[ang], writes=[a])
            P.op("dve", lambda e: e.tensor_scalar(out=t.ap, in0=a.ap, scalar1=1.0 / TWO_PI, scalar2=None, op0=ALU.mult), reads=[a], writes=[t])
            P.op("dve", lambda e: e.tensor_copy(out=ni.ap, in_=t.ap), reads=[t], writes=[ni])
            P.op("dve", lambda e: e.tensor_copy(out=t.ap, in_=ni.ap), reads=[ni], writes=[t])
            P.op("dve", lambda e: e.scalar_tensor_tensor(out=a.ap, in0=t.ap, scalar=-C1, in1=a.ap, op0=ALU.mult, op1=ALU.add), reads=[t, a], writes=[a])
            P.op("dve", lambda e: e.scalar_tensor_tensor(out=a.ap, in0=t.ap, scalar=-C2, in1=a.ap, op0=ALU.mult, op1=ALU.add), reads=[t, a], writes=[a])
            P.op("dve", lambda e: e.tensor_scalar(out=t.ap, in0=a.ap, scalar1=math.pi, scalar2=-TWO_PI, op0=ALU.is_gt, op1=ALU.mult), reads=[a], writes=[t])
            P.op("dve", lambda e: e.tensor_tensor(out=a.ap, in0=a.ap, in1=t.ap, op=ALU.add), reads=[t, a], writes=[a])
            P.op("dve", lambda e: e.tensor_scalar(out=t.ap, in0=a.ap, scalar1=-math.pi, scalar2=TWO_PI, op0=ALU.is_lt, op1=ALU.mult), reads=[a], writes=[t])
            P.op("dve", lambda e: e.tensor_tensor(out=a.ap, in0=a.ap, in1=t.ap, op=ALU.add), reads=[t, a], writes=[a])
            P.op("dve", lambda e: e.tensor_scalar(out=a.ap, in0=a.ap, scalar1=-math.pi, scalar2=math.pi, op0=ALU.max, op1=ALU.min), reads=[a], writes=[a])
            P.op("act", lambda e, o=o: e.activation(out=o.ap, in_=a.ap, func=AF.Sin), reads=[a], writes=[o])
            outs.append(o)
        return outs[1], outs[0]

    def rope_apply(tmp_r, x1, x2, d1, cos_ap, sin_ap, o1, o2, o_w, npart, n, scale=1.0):
        for (oa, fa, fb, opx) in ((o1, cos_ap, sin_ap, ALU.subtract), (o2, sin_ap, cos_ap, ALU.add)):
            ta = tmp_r.next()
            tb_ = tmp_r.next()
            P.op("dve", lambda e, ta=ta, fa=fa: e.tensor_tensor(out=ta.ap[0:npart, 0:n], in0=x1, in1=fa, op=ALU.mult), reads=d1, writes=[ta])
            P.op("dve", lambda e, tb_=tb_, fb=fb: e.tensor_tensor(out=tb_.ap[0:npart, 0:n], in0=x2, in1=fb, op=ALU.mult), reads=d1, writes=[tb_])
            P.op("dve", lambda e, ta=ta, tb_=tb_, opx=opx: e.tensor_tensor(out=ta.ap[0:npart, 0:n], in0=ta.ap[0:npart, 0:n], in1=tb_.ap[0:npart, 0:n], op=opx),
                 reads=[ta, tb_], writes=[ta])
            P.op("dve", lambda e, ta=ta, oa=oa: e.tensor_scalar(out=oa, in0=ta.ap[0:npart, 0:n], scalar1=scale, scalar2=None, op0=ALU.mult),
                 reads=[ta], writes=o_w)

    x1T = dscratch("x1T", [128, KC, T_OWN])
    OWN0 = T_ALL - T_OWN
    if upto >= 1 and L0:
      with ExitStack() as st:
        hT = salloc(st, "hT0", [128, KC, T_ALL], BF16)
        with ExitStack() as st2:
            prologue(st2, 0, xT_all, T_ALL, hT)
            if "h0" in dbg:
                d = dbg_tensor("h0", [128, KC, T_ALL], BF16)
                P.dma("sp", d.ap, hT.ap, reads=[hT], writes=[d])
            P.barrier()
        with ExitStack() as st_t:
            cosm, sinm = rope_tables(st, st_t, "m", 32, 0, T_ALL, pos_in)
            P.barrier()
        cqn = salloc(st, "cqn", [128, 4, T_OWN], BF16)
        ckvn = salloc(st, "ckvn", [128, 2, T_ALL], BF16)
        k1 = salloc(st, "k1", [32, T_ALL], BF16)
        k2 = salloc(st, "k2", [32, T_ALL], BF16)
        ring = Ring(st, "wr0_", 4, [128, KC, 128])
        toks_all = [(t, 512) for t in range(0, T_ALL, 512)]
        toks_own = [(t, 512) for t in range(OWN0, T_ALL, 512)]
        ISQ = 128 ** -0.5
        MSC = 192 ** -0.5
        with ExitStack() as st1:
            cq = salloc(st1, "cq", [128, 4, T_OWN], F32)
            ckv = salloc(st1, "ckv", [128, 2, T_ALL], F32)
            st1a = ExitStack()
            kr = salloc(st1a, "kr", [32, 2, T_ALL], F32)

            def ev_cq(ps, col, m, t0, n):
                P.op("act", lambda e: e.activation(out=cq.ap[:, col // 128, t0 - OWN0:t0 - OWN0 + n], in_=ps.ap[:, 0:n], func=AF.Copy),
                     reads=[ps], writes=[(cq, (col // 128, t0))])
            linT(ring, ev_w_in, 3072, 512, hT, KC, toks_own, ev_cq, piece=128)

            def ev_ckv(ps, col, m, t0, n):
                P.op("act", lambda e: e.activation(out=ckv.ap[:, col // 128, t0:t0 + n], in_=ps.ap[:, 0:n], func=AF.Copy),
                     reads=[ps], writes=[(ckv, (col // 128, t0))])
            linT(ring, ev_w_in, 3584, 256, hT, KC, toks_all, ev_ckv, piece=128)
            for half in range(2):
                def ev_kr(ps, col, m, t0, n, half=half):
                    P.op("act", lambda e: e.activation(out=kr.ap[:, half, t0:t0 + n], in_=ps.ap[0:32, 0:n], func=AF.Copy),
                         reads=[ps], writes=[(kr, (half, t0))])
                linT(ring, ev_w_in, 3840 + 32 * half, 32, hT, KC, toks_all, ev_kr, piece=32)
            ropetmp = Ring(st1a, "rt_", 4, [128, 512], F32)
            for (t0, n) in toks_all:
                rope_apply(ropetmp, kr.ap[:, 0, t0:t0 + n], kr.ap[:, 1, t0:t0 + n], [kr],
                           cosm.ap[:, t0:t0 + n], sinm.ap[:, t0:t0 + n],
                           k1.ap[:, t0:t0 + n], k2.ap[:, t0:t0 + n], [(k1, t0), (k2, t0)], 32, n)
            P.barrier()
            st1a.close()
            qng = salloc(st1, "qng", [128, 4], F32)
            P.dma("sp", qng.ap, ev_qn_in, writes=[qng])
            kvng = salloc(st1, "kvng", [128, 2], F32)
            P.dma("sp", kvng.ap, ev_kvn_in, writes=[kvng])

            def emit_cq(tm, c, tb):
                P.op("act", lambda e: e.activation(out=cqn.ap[:, c, tb * 512:(tb + 1) * 512], in_=tm.ap, func=AF.Copy, scale=qng.ap[:, c:c + 1]),
                     reads=[tm, qng], writes=[(cqn, (c, tb))])
            with ExitStack() as stn:
                rmsnorm_T(stn, "cq", lambda c, tb: (cq.ap[:, c, tb * 512:(tb + 1) * 512], [cq]), 4, T_OWN // 512, 512, emit_cq)
                P.barrier()

            def emit_ckv(tm, c, tb):
                P.op("act", lambda e: e.activation(out=ckvn.ap[:, c, tb * 512:(tb + 1) * 512], in_=tm.ap, func=AF.Copy, scale=kvng.ap[:, c:c + 1]),
                     reads=[tm, kvng], writes=[(ckvn, (c, tb))])
            with ExitStack() as stn:
                rmsnorm_T(stn, "ckv", lambda c, tb: (ckv.ap[:, c, tb * 512:(tb + 1) * 512], [ckv]), 2, T_ALL // 512, 256, emit_ckv)
            P.barrier()

        oT = salloc(st, "oT", [128, KC, T_OWN], BF16)
        with ExitStack() as st2:
            qh = salloc(st2, "sbq", [128, T_OWN], BF16)
            kh = salloc(st2, "sbk", [128, T_ALL], BF16)
            vh = salloc(st2, "sbv", [128, 16, 128], BF16)
            e_r = Ring(st2, "sbe_", 3, [128, 512], F32)
            sp_r = Ring(st2, "sbs_", 2, [128, 512], F32)
            L_r = Ring(st2, "sbl_", 2, [128, 512], BF16)
            lsf_r = Ring(st2, "sblsf_", 2, [128, 512], F32)
            lsb_r = Ring(st2, "sblsb_", 2, [128, 512], BF16)
            er_r = Ring(st2, "sber_", 2, [128, 512], F32)
            a_r = Ring(st2, "sba_", 2, [128, 512], BF16)
            def getw_l0():
                b = ring.next()
                return b, b.ap
            for h in range(8):
                if mode == "F":
                    mod_run(1, 6, getw_l0)

                def ev_q(ps, col, m, t0, n):
                    P.op("act", lambda e: e.activation(out=qh.ap[:, t0 - OWN0:t0 - OWN0 + n], in_=ps.ap[:, 0:n], func=AF.Copy, scale=ISQ),
                         reads=[ps], writes=[(qh, t0)])
                linT(ring, ev_w_in, h * 128, 128, hT, KC, toks_own, ev_q, piece=128)

                def ev_k(ps, col, m, t0, n):
                    P.op("act", lambda e: e.activation(out=kh.ap[:, t0:t0 + n], in_=ps.ap[:, 0:n], func=AF.Copy),
                         reads=[ps], writes=[(kh, t0)])
                linT(ring, ev_w_in, 1024 + h * 128, 128, hT, KC, toks_all, ev_k, piece=128)

                def ev_v(ps, col, cw, t0):
                    P.op("dve", lambda e: e.tensor_copy(out=vh.ap[:, t0 // 128, :], in_=ps.ap[:, 0:128]),
                         reads=[ps], writes=[(vh, t0 // 128)])
                linTok(ring, ev_w_in, 2048 + h * 128, 128, hT, KC, list(range(0, T_ALL, 128)), ev_v, piece=128)
                P.op("dve", lambda e: e.tensor_scalar(out=vh.ap[:, 0:8, :], in0=vh.ap[:, 0:8, :], scalar1=pvalid.ap[:, 0:1], scalar2=None, op0=ALU.mult),
                     reads=[vh, pvalid], writes=[vh])
                tiles = []
                for s in range(2):
                    nkb = 8 + 4 * s + 4
                    for kb in range(nkb - 1, -1, -1):
                        tiles.append((s, kb, kb == nkb - 1, kb == 0))
                stt = [dict() for _ in tiles]

                def sb_s1(i):
                    s, kb, first, last = tiles[i]
                    q0 = s * 512
                    T = stt[i]
                    psz = psn()
                    P.op("pe", lambda e: e.matmul(psz.ap, lhsT=kh.ap[:, kb * 128:(kb + 1) * 128], rhs=qh.ap[:, q0:q0 + 512], start=True, stop=True),
                         reads=[kh, qh], writes=[psz])
                    eb = e_r.next()
                    P.op("act", lambda e: e.activation(out=eb.ap, in_=psz.ap, func=AF.Exp), reads=[psz], writes=[eb])
                    jd = kb - (8 + 4 * s)
                    Lb = L_r.next()
                    if jd >= 0:
                        spb = sp_r.next()
                        P.op("act", lambda e: e.activation(out=spb.ap, in_=eb.ap, func=AF.Ln, bias=1.0), reads=[eb], writes=[spb])
                        P.op("dve", lambda e: e.tensor_tensor(out=Lb.ap, in0=spb.ap, in1=mstrict(jd), op=ALU.mult), reads=[spb, consts], writes=[Lb])
                        P.flag_last("dve")
                    else:
                        P.op("act", lambda e: e.activation(out=Lb.ap, in_=eb.ap, func=AF.Ln, bias=1.0), reads=[eb], writes=[Lb])
                        P.flag_last("act")
                    T.update(eb=eb, Lb=Lb, jd=jd)

                def sb_s2(i):
                    s, kb, first, last = tiles[i]
                    T = stt[i]
                    Lb = T["Lb"]
                    psr = psn()
                    P.op("pe", lambda e: e.matmul(psr.ap, lhsT=U_b.ap, rhs=Lb.ap, start=True, stop=first), reads=[Lb, U_b], writes=[psr])
                    if not first:
                        prev_b = stt[i - 1]["Lsb"]
                        P.op("pe", lambda e: e.matmul(psr.ap, lhsT=ones_b.ap, rhs=prev_b.ap, start=False, stop=True), reads=[prev_b, ones_b], writes=[psr])
                    erb = er_r.next()
                    P.op("act", lambda e: e.activation(out=erb.ap, in_=psr.ap, func=AF.Exp, scale=-1.0), reads=[psr], writes=[erb])
                    P.flag_last("act")
                    Lsf = lsf_r.next()
                    if first:
                        P.op("dve", lambda e: e.tensor_copy(out=Lsf.ap, in_=Lb.ap), reads=[Lb], writes=[Lsf])
                    else:
                        prev_f = stt[i - 1]["Lsf"]
                        P.op("dve", lambda e: e.tensor_tensor(out=Lsf.ap, in0=prev_f.ap, in1=Lb.ap, op=ALU.add), reads=[Lb, prev_f], writes=[Lsf])
                    Lsb = lsb_r.next()
                    P.op("dve", lambda e: e.tensor_copy(out=Lsb.ap, in_=Lsf.ap), reads=[Lsf], writes=[Lsb])
                    P.flag_last("dve")
                    T.update(erb=erb, Lsf=Lsf, Lsb=Lsb)

                def sb_s3(i):
                    s, kb, first, last = tiles[i]
                    q0 = s * 512
                    T = stt[i]
                    eb, erb, jd = T["eb"], T["erb"], T["jd"]
                    pso = PS[6 + s]
                    ab = a_r.next()
                    if jd >= 0:
                        P.op("dve", lambda e: e.tensor_tensor(out=erb.ap, in0=erb.ap, in1=mstrict(jd), op=ALU.mult), reads=[erb, consts], writes=[erb])
                    P.op("dve", lambda e: e.tensor_tensor(out=ab.ap, in0=eb.ap, in1=erb.ap, op=ALU.mult), reads=[eb, erb], writes=[ab])
                    P.op("pe", lambda e: e.matmul(pso.ap[:, 0:512], lhsT=vh.ap[:, kb, :], rhs=ab.ap, start=first, stop=last),
                         reads=[vh, ab], writes=[pso])
                    if last:
                        P.op("act", lambda e: e.activation(out=oT.ap[:, h, q0:q0 + 512], in_=pso.ap, func=AF.Copy), reads=[pso], writes=[(oT, (h, s))])
                nt = len(tiles)
                for step in range(nt + 2):
                    if step < nt:
                        sb_s1(step)
                    if 0 <= step - 1 < nt:
                        sb_s2(step - 1)
                    if 0 <= step - 2 < nt:
                        sb_s3(step - 2)
            P.barrier()

        with ExitStack() as st3:
            qn = salloc(st3, "mqn", [128, T_OWN], BF16)
            q1 = salloc(st3, "mq1", [32, T_OWN], BF16)
            q2 = salloc(st3, "mq2", [32, T_OWN], BF16)
            kn = salloc(st3, "mkn", [128, T_ALL], BF16)
            vm = salloc(st3, "mv", [128, 16, 128], BF16)
            ropetmp = Ring(st3, "rt3_", 4, [128, 512], F32)
            p_r = Ring(st3, "mp_", 4, [128, 512], BF16)
            pf_r = Ring(st3, "mpf_", 3, [128, 512], F32)
            rden = salloc(st3, "mrden", [128, 512], F32)
            toks_loc = [(0, 512), (512, 512)]
            wq = ev_w_q_up.rearrange("(kc p) n -> p kc n", p=128)
            for h in range(8):
                def ev_qn(ps, col, m, t0, n):
                    P.op("act", lambda e: e.activation(out=qn.ap[:, t0:t0 + n], in_=ps.ap[:, 0:n], func=AF.Copy, scale=MSC),
                         reads=[ps], writes=[(qn, t0)])
                linT(ring, ev_w_q_up, h * 192, 128, cqn, 4, toks_loc, ev_qn, piece=128)
                wb = wload(ring, wq[:, :, h * 192 + 128:h * 192 + 192], 4, 64)
                for (t0, n) in toks_loc:
                    ps1 = psn()
                    ps2 = psn()
                    for (psx, off) in ((ps1, 0), (ps2, 32)):
                        for kc in range(4):
                            P.op("pe", lambda e: e.matmul(psx.ap[0:32, 0:n], lhsT=wb.ap[:, kc, off:off + 32], rhs=cqn.ap[:, kc, t0:t0 + n],
                                                          start=(kc == 0), stop=(kc == 3)), reads=[wb, cqn], writes=[psx])
                    rope_apply(ropetmp, ps1.ap[0:32, 0:n], ps2.ap[0:32, 0:n], [ps1, ps2],
                               cosm.ap[:, OWN0 + t0:OWN0 + t0 + n], sinm.ap[:, OWN0 + t0:OWN0 + t0 + n],
                               q1.ap[:, t0:t0 + n], q2.ap[:, t0:t0 + n], [(q1, t0), (q2, t0)], 32, n, scale=MSC)

                def ev_kn(ps, col, m, t0, n):
                    P.op("act", lambda e: e.activation(out=kn.ap[:, t0:t0 + n], in_=ps.ap[:, 0:n], func=AF.Copy),
                         reads=[ps], writes=[(kn, t0)])
                linT(ring, ev_w_kv_up, h * 256, 128, ckvn, 2, toks_all, ev_kn, piece=128)

                def ev_vm(ps, col, cw, t0):
                    P.op("dve", lambda e: e.tensor_copy(out=vm.ap[:, t0 // 128, :], in_=ps.ap[:, 0:128]),
                         reads=[ps], writes=[(vm, t0 // 128)])
                linTok(ring, ev_w_kv_up, h * 256 + 128, 128, ckvn, 2, list(range(0, T_ALL, 128)), ev_vm, piece=128)
                P.op("dve", lambda e: e.tensor_scalar(out=vm.ap[:, 0:8, :], in0=vm.ap[:, 0:8, :], scalar1=pvalid.ap[:, 0:1], scalar2=None, op0=ALU.mult),
                     reads=[vm, pvalid], writes=[vm])
                for s in range(2):
                    q0 = s * 512
                    nkb = 8 + 4 * s + 4
                    pso = PS[6]
                    psd = PS[7]
                    pbs = {}

                    def m_s1(kb):
                        psz = psn()
                        ks = slice(kb * 128, (kb + 1) * 128)
                        P.op("pe", lambda e: e.matmul(psz.ap, lhsT=kn.ap[:, ks], rhs=qn.ap[:, q0:q0 + 512], start=True, stop=False),
                             reads=[kn, qn], writes=[psz])
                        P.op("pe", lambda e: e.matmul(psz.ap, lhsT=k1.ap[:, ks], rhs=q1.ap[:, q0:q0 + 512], start=False, stop=False),
                             reads=[k1, q1], writes=[psz])
                        P.op("pe", lambda e: e.matmul(psz.ap, lhsT=k2.ap[:, ks], rhs=q2.ap[:, q0:q0 + 512], start=False, stop=True),
                             reads=[k2, q2], writes=[psz])
                        jd = kb - (8 + 4 * s)
                        pb = p_r.next()
                        if jd >= 0:
                            pf = pf_r.next()
                            P.op("act", lambda e: e.activation(out=pf.ap, in_=psz.ap, func=AF.Exp), reads=[psz], writes=[pf])
                            P.op("dve", lambda e: e.tensor_tensor(out=pb.ap, in0=pf.ap, in1=mincl(jd), op=ALU.mult), reads=[pf, consts], writes=[pb])
                        else:
                            P.op("act", lambda e: e.activation(out=pb.ap, in_=psz.ap, func=AF.Exp), reads=[psz], writes=[pb])
                        pbs[kb] = pb

                    def m_s2(kb):
                        first = (kb == 0)
                        last = (kb == nkb - 1)
                        pb = pbs[kb]
                        onesx = ones_pv if kb < 8 else ones_b
                        P.op("pe", lambda e: e.matmul(psd.ap, lhsT=onesx.ap, rhs=pb.ap, start=first, stop=last), reads=[onesx, pb], writes=[psd])
                        P.op("pe", lambda e: e.matmul(pso.ap, lhsT=vm.ap[:, kb, :], rhs=pb.ap, start=first, stop=last), reads=[vm, pb], writes=[pso])
                    for step in range(nkb + 2):
                        if step < nkb:
                            m_s1(step)
                        if 0 <= step - 2 < nkb:
                            m_s2(step - 2)
                    P.op("dve", lambda e: e.reciprocal(out=rden.ap, in_=psd.ap), reads=[psd], writes=[rden])
                    P.op("dve", lambda e: e.tensor_tensor(out=oT.ap[:, 8 + h, q0:q0 + 512], in0=pso.ap, in1=rden.ap, op=ALU.mult),
                         reads=[pso, rden], writes=[(oT, (8 + h, s))])
            P.barrier()

        with ExitStack() as st4:
            if "o0" in dbg:
                d = dbg_tensor("o0", [128, KC, T_OWN], BF16)
                P.dma("sp", d.ap, oT.ap, reads=[oT], writes=[d])
            xo_r = Ring(st4, "xo_", 3, [128, 512], F32)
            res_r = Ring(st4, "res_", 3, [128, 512], F32)

            def ev_out(ps, col, m, t0, n):
                fc = col // 128
                xo = xo_r.next()
                P.dma("sp", xo.ap[:, 0:n], xT_all.ap[:, fc, OWN0 + t0:OWN0 + t0 + n], reads=[xT_all], writes=[xo])
                res = res_r.next()
                P.op("dve", lambda e: e.scalar_tensor_tensor(out=res.ap[:, 0:n], in0=ps.ap[:, 0:n], scalar=modG.ap[:, 0, fc:fc + 1], in1=xo.ap[:, 0:n],
                                                              op0=ALU.mult, op1=ALU.add), reads=[ps, xo, (modG, 0)], writes=[res])
                P.dma("sp", x1T.ap[:, fc, t0:t0 + n], res.ap[:, 0:n], reads=[res], writes=[(x1T, (fc, t0))])
            linT(ring, ev_w_out, 0, D, oT, KC, [(0, 512), (512, 512)], ev_out, piece=128)
            P.barrier()
    X.final_src = x1T
    if "x1" in dbg and L0:
        d = dbg_tensor("x1", [128, KC, T_OWN])
        P.dma("sp", d.ap, x1T.ap, reads=[x1T], writes=[d])


    need_moe = (upto >= 2 and L0) or (upto >= 5 and mode in ("B", "F"))
    if need_moe:
        moe_wr_in = din("moe_wr", [2, D, 36])
        moe_br_in = din("moe_br", [2, 1, 36])
        moe_w_gate = din("moe_w_gate", [2, 32, D, 512])
        moe_w_up = din("moe_w_up", [2, 32, D, 512])
        moe_w_down = din("moe_w_down", [2, 32, 512, D])
    BIG = 1.0e30
    AX = mybir.AxisListType.X

    def moe_layer(l, xin, xout, n_exp=32):
        li = 2 * l + 1
        with ExitStack() as st:
            hT = salloc(st, f"hTm{l}", [128, KC, T_OWN], BF16)
            comb = salloc(st, f"comb{l}", [128, 8, 32], F32)
            wr = salloc(st, f"wr{l}", [128, KC, 36], F32)
            P.dma("sp", wr.ap, moe_wr_in[l].rearrange("(kc p) n -> p kc n", p=128), writes=[wr])
            br = salloc(st, f"br{l}", [128, 36], F32)
            P.dma("sp", br.ap, moe_br_in[l].broadcast_to([128, 36]), writes=[br])
            with ExitStack() as stp:
                hf_r = Ring(stp, f"hf{l}_", 2, [128, 512], F32)
                sm = {k: salloc(stp, f"rt{l}_{k}", [128, n], F32) for k, n in
                      (("lg", 36), ("gmax", 1), ("ngmax", 1), ("ge", 4), ("gsum", 1), ("gw", 1), ("ohg", 4), ("pen", 4), ("ml", 32),
                       ("m1", 1), ("oh1", 32), ("ml2", 32), ("m2", 1), ("oh2", 32), ("d", 1), ("ed", 1), ("den", 1), ("rden", 1),
                       ("w1", 1), ("w2", 1), ("tmp", 32))}
                X.ps_mod = 4

                def hook(tm, c, tb):
                    hf = hf_r.next()
                    P.op("act", lambda e: e.activation(out=hf.ap, in_=tm.ap, func=AF.Identity,
                                                       scale=modA.ap[:, li, c:c + 1], bias=modB.ap[:, li, c:c + 1]),
                         reads=[tm, (modA, li), (modB, li)], writes=[hf])
                    P.op("dve", lambda e: e.tensor_copy(out=hT.ap[:, c, tb * 512:(tb + 1) * 512], in_=hf.ap), reads=[hf], writes=[(hT, (c, tb))])
                    for tt in range(4):
                        P.op("pe", lambda e: e.matmul(PS[4 + tt].ap[:, 0:36], lhsT=hf.ap[:, tt * 128:(tt + 1) * 128], rhs=wr.ap[:, c, :],
                                                      start=(c == 0), stop=(c == KC - 1)), reads=[hf, wr], writes=[PS[4 + tt]])
                    if c == KC - 1:
                        for tt in range(4):
                            gt = tb * 4 + tt
                            S = sm

                            def dv(fn, r, w):
                                P.op("dve", fn, reads=r, writes=w)
                            dv(lambda e: e.tensor_tensor(out=S["lg"].ap, in0=PS[4 + tt].ap[:, 0:36], in1=br.ap, op=ALU.add), [PS[4 + tt], br], [S["lg"]])
                            gl = S["lg"].ap[:, 0:4]
                            el = S["lg"].ap[:, 4:36]
                            dv(lambda e: e.reduce_max(out=S["gmax"].ap, in_=gl, axis=AX), [S["lg"]], [S["gmax"]])
                            dv(lambda e: e.tensor_scalar(out=S["ngmax"].ap, in0=S["gmax"].ap, scalar1=-1.0, scalar2=None, op0=ALU.mult), [S["gmax"]], [S["ngmax"]])
                            P.op("act", lambda e: e.activation(out=S["ge"].ap, in_=gl, func=AF.Exp, bias=S["ngmax"].ap[:, 0:1], accum_out=S["gsum"].ap),
                                 reads=[S["lg"], S["ngmax"]], writes=[S["ge"], S["gsum"]])
                            dv(lambda e: e.reciprocal(out=S["gw"].ap, in_=S["gsum"].ap), [S["gsum"]], [S["gw"]])
                            dv(lambda e: e.tensor_scalar(out=S["ohg"].ap, in0=gl, scalar1=S["gmax"].ap[:, 0:1], scalar2=None, op0=ALU.is_equal), [S["lg"], S["gmax"]], [S["ohg"]])
                            dv(lambda e: e.tensor_scalar(out=S["pen"].ap, in0=S["ohg"].ap, scalar1=BIG, scalar2=-BIG, op0=ALU.mult, op1=ALU.add), [S["ohg"]], [S["pen"]])
                            for g in range(4):
                                dv(lambda e: e.tensor_scalar(out=S["ml"].ap[:, g * 8:(g + 1) * 8], in0=S["lg"].ap[:, 4 + g * 8:4 + (g + 1) * 8],
                                                             scalar1=S["pen"].ap[:, g:g + 1], scalar2=None, op0=ALU.add), [S["lg"], S["pen"]], [S["ml"]])
                            dv(lambda e: e.reduce_max(out=S["m1"].ap, in_=S["ml"].ap, axis=AX), [S["ml"]], [S["m1"]])
                            dv(lambda e: e.tensor_scalar(out=S["oh1"].ap, in0=S["ml"].ap, scalar1=S["m1"].ap[:, 0:1], scalar2=None, op0=ALU.is_equal), [S["ml"], S["m1"]], [S["oh1"]])
                            dv(lambda e: e.scalar_tensor_tensor(out=S["ml2"].ap, in0=S["oh1"].ap, scalar=-BIG, in1=S["ml"].ap, op0=ALU.mult, op1=ALU.add), [S["oh1"], S["ml"]], [S["ml2"]])
                            dv(lambda e: e.reduce_max(out=S["m2"].ap, in_=S["ml2"].ap, axis=AX), [S["ml2"]], [S["m2"]])
                            dv(lambda e: e.tensor_scalar(out=S["oh2"].ap, in0=S["ml2"].ap, scalar1=S["m2"].ap[:, 0:1], scalar2=None, op0=ALU.is_equal), [S["ml2"], S["m2"]], [S["oh2"]])
                            dv(lambda e: e.tensor_tensor(out=S["d"].ap, in0=S["m2"].ap, in1=S["m1"].ap, op=ALU.subtract), [S["m1"], S["m2"]], [S["d"]])
                            P.op("act", lambda e: e.activation(out=S["ed"].ap, in_=S["d"].ap, func=AF.Exp), reads=[S["d"]], writes=[S["ed"]])
                            dv(lambda e: e.tensor_scalar(out=S["den"].ap, in0=S["ed"].ap, scalar1=1.0, scalar2=None, op0=ALU.add), [S["ed"]], [S["den"]])
                            dv(lambda e: e.reciprocal(out=S["rden"].ap, in_=S["den"].ap), [S["den"]], [S["rden"]])
                            dv(lambda e: e.tensor_tensor(out=S["w1"].ap, in0=S["rden"].ap, in1=S["gw"].ap, op=ALU.mult), [S["rden"], S["gw"]], [S["w1"]])
                            dv(lambda e: e.tensor_tensor(out=S["w2"].ap, in0=S["w1"].ap, in1=S["ed"].ap, op=ALU.mult), [S["w1"], S["ed"]], [S["w2"]])
                            dv(lambda e: e.tensor_scalar(out=S["tmp"].ap, in0=S["oh1"].ap, scalar1=S["w1"].ap[:, 0:1], scalar2=None, op0=ALU.mult), [S["oh1"], S["w1"]], [S["tmp"]])
                            dv(lambda e: e.scalar_tensor_tensor(out=comb.ap[:, gt, :], in0=S["oh2"].ap, scalar=S["w2"].ap[:, 0:1], in1=S["tmp"].ap, op0=ALU.mult, op1=ALU.add),
                               [S["oh2"], S["w2"], S["tmp"]], [(comb, gt)])
                prologue(stp, li, xin, T_OWN, hT, hook=hook)
                X.ps_mod = 6
                P.barrier()
            if f"comb{l}" in dbg:
                d = dbg_tensor(f"comb{l}", [128, 8, 32])
                P.dma("sp", d.ap, comb.ap, reads=[comb], writes=[d])
            if f"hm{l}" in dbg:
                d = dbg_tensor(f"hm{l}", [128, KC, T_OWN], BF16)
                P.dma("sp", d.ap, hT.ap, reads=[hT], writes=[d])
            yacc = salloc(st, f"yacc{l}", [128, KC, T_OWN], F32)
            with ExitStack() as ste:
                mring = Ring(ste, f"mw{l}_", 12, [128, 2048])
                aT = salloc(ste, f"aT{l}", [128, 4, T_OWN], BF16)
                sg_r = Ring(ste, f"sg{l}_", 2, [128, 512], F32)
                t1_r = Ring(ste, f"t1{l}_", 2, [128, 512], F32)
                cs_r = Ring(ste, f"cs{l}_", 2, [128, T_OWN], F32)
                dg_r = Ring(ste, f"dg{l}_", 2, [128, 128], F32)
                def getw_m():
                    b = mring.next()
                    return b, b.ap.rearrange("p (k n) -> p k n", n=128)
                for ex in range(n_exp):
                    if mode == "F" and l == 0:
                        mod_run(2, 3, getw_m)
                        if X.mod_todo.get(2, 0) >= 48:
                            mod_run(3, 3, getw_m)
                    cs = cs_r.next()
                    for half in range(2):
                        psc = psn()
                        for q in range(4):
                            gt = half * 4 + q
                            dg = dg_r.next()
                            P.op("dve", lambda e: e.tensor_scalar(out=dg.ap, in0=ident_f, scalar1=comb.ap[:, gt, ex:ex + 1], scalar2=None, op0=ALU.mult),
                                 reads=[consts, (comb, gt)], writes=[dg])
                            P.op("pe", lambda e: e.matmul(psc.ap[:, q * 128:(q + 1) * 128], lhsT=ones_f, rhs=dg.ap, start=True, stop=True),
                                 reads=[consts, dg], writes=[psc])
                        P.op("act", lambda e: e.activation(out=cs.ap[:, half * 512:(half + 1) * 512], in_=psc.ap, func=AF.Copy), reads=[psc], writes=[(cs, half)])
                    wgv = moe_w_gate[l, ex].rearrange("(kc p) n -> p kc n", p=128)
                    wuv = moe_w_up[l, ex].rearrange("(kc p) n -> p kc n", p=128)
                    for hc in range(4):
                        wg = mring.next()
                        P.dma("pool", wg.ap.rearrange("p (k n) -> p k n", n=128), wgv[:, :, hc * 128:(hc + 1) * 128], writes=[wg])
                        wu = mring.next()
                        P.dma("pool", wu.ap.rearrange("p (k n) -> p k n", n=128), wuv[:, :, hc * 128:(hc + 1) * 128], writes=[wu])
                        for th in range(2):
                            psg = psn()
                            psu = psn()
                            for (psx, wx) in ((psg, wg), (psu, wu)):
                                for kc in range(KC):
                                    P.op("pe", lambda e: e.matmul(psx.ap, lhsT=wx.ap[:, kc * 128:(kc + 1) * 128], rhs=hT.ap[:, kc, th * 512:(th + 1) * 512],
                                                                  start=(kc == 0), stop=(kc == KC - 1)), reads=[wx, hT], writes=[psx])
                            sg = sg_r.next()
                            P.op("act", lambda e: e.activation(out=sg.ap, in_=psg.ap, func=AF.Silu), reads=[psg], writes=[sg])
                            t1 = t1_r.next()
                            P.op("dve", lambda e: e.tensor_tensor(out=t1.ap, in0=psu.ap, in1=sg.ap, op=ALU.mult), reads=[psu, sg], writes=[t1])
                            P.op("dve", lambda e: e.tensor_tensor(out=aT.ap[:, hc, th * 512:(th + 1) * 512], in0=t1.ap, in1=cs.ap[:, th * 512:(th + 1) * 512], op=ALU.mult),
                                 reads=[t1, (cs, th)], writes=[(aT, (hc, th))])
                    wds = []
                    for hc in range(4):
                        wd = mring.next()
                        P.dma("pool", wd.ap, moe_w_down[l, ex, hc * 128:(hc + 1) * 128, :], writes=[wd])
                        wds.append(wd)
                    for fc in range(KC):
                        for th in range(2):
                            ps = psn()
                            for hc in range(4):
                                P.op("pe", lambda e: e.matmul(ps.ap, lhsT=wds[hc].ap[:, fc * 128:(fc + 1) * 128], rhs=aT.ap[:, hc, th * 512:(th + 1) * 512],
                                                              start=(hc == 0), stop=(hc == 3)), reads=[wds[hc], (aT, (hc, th))], writes=[ps])
                            ysl = yacc.ap[:, fc, th * 512:(th + 1) * 512]
                            if ex == 0:
                                P.op("dve", lambda e: e.tensor_copy(out=ysl, in_=ps.ap), reads=[ps], writes=[(yacc, (fc, th))])
                            else:
                                P.op("dve", lambda e: e.tensor_tensor(out=ysl, in0=ps.ap, in1=ysl, op=ALU.add), reads=[ps, (yacc, (fc, th))], writes=[(yacc, (fc, th))])
                P.barrier()
            with ExitStack() as sto:
                xo_r = Ring(sto, f"mxo{l}_", 3, [128, 512], F32)
                res_r = Ring(sto, f"mres{l}_", 3, [128, 512], F32)
                for fc in range(KC):
                    for th in range(2):
                        xo = xo_r.next()
                        P.dma("sp", xo.ap, xin.ap[:, fc, th * 512:(th + 1) * 512], reads=[xin], writes=[xo])
                        res = res_r.next()
                        P.op("dve", lambda e: e.scalar_tensor_tensor(out=res.ap, in0=yacc.ap[:, fc, th * 512:(th + 1) * 512], scalar=modG.ap[:, li, fc:fc + 1],
                                                                      in1=xo.ap, op0=ALU.mult, op1=ALU.add), reads=[(yacc, (fc, th)), xo, (modG, li)], writes=[res])
                        P.dma("sp", xout.ap[:, fc, th * 512:(th + 1) * 512], res.ap, reads=[res], writes=[(xout, (fc, th))])
                P.barrier()

    if mode == "B" or SKIP_L0:
        x2T = Buf("x2T", None, din("x2T_in", [128, KC, T_OWN]))
    else:
        x2T = dscratch("x2T", [128, KC, T_OWN], out=(mode == "A"))
    if upto >= 2 and L0:
        if mode == "F":
            with ExitStack() as stq:
                rq = Ring(stq, "wq1r", 2, [128, KC, 128])
                mod_run(1, None, lambda: (lambda b: (b, b.ap))(rq.next()))
        moe_layer(0, x1T, x2T, n_exp=X_NEXP)
        if mode == "F":
            with ExitStack() as stq:
                rq = Ring(stq, "wq2r", 2, [128, KC, 128])
                mod_run(2, None, lambda: (lambda b: (b, b.ap))(rq.next()))
                mod_run(3, None, lambda: (lambda b: (b, b.ap))(rq.next()))
                P.barrier()
        X.final_src = x2T
    if "x2" in dbg and L0:
        d = dbg_tensor("x2", [128, KC, T_OWN])
        P.dma("sp", d.ap, x2T.ap, reads=[x2T], writes=[d])


    od_w_in = din("od_w_in", [D, 12288])
    od_w_out = din("od_w_out", [4096, D])
    ret_c_in = din("ret_consts", [128, 8 + 2048])
    GAM = [1.0 - 2.0 ** (-5.0 - h) for h in range(8)]
    KSC = 256 ** -0.5
    pos_own = pos_in[:, OWN0:T_ALL]
    toks_loc = [(0, 512), (512, 512)]

    def ret_common(st, cached=False):
        R = Ctx()
        R.hT = salloc(st, "hT1", [128, KC, T_OWN], BF16)
        R.rc = salloc(st, "retc", [128, 8 + 2048], F32)
        P.dma("sp", R.rc.ap, ret_c_in, writes=[R.rc])
        if cached:
            P.dma("sp", R.hT.ap, c_hT.ap, reads=[c_hT], writes=[R.hT])
            R.sin = salloc(st, "sinr2", [128, T_OWN], F32)
            R.cos = salloc(st, "cosr2", [128, T_OWN], F32)
            P.dma("sp", R.cos.ap, c_cs.ap[0], reads=[(c_cs, 0)], writes=[R.cos])
            P.dma("sp", R.sin.ap, c_cs.ap[1], reads=[(c_cs, 1)], writes=[R.sin])
        else:
            with ExitStack() as stp:
                prologue(stp, 2, x2T, T_OWN, R.hT)
                P.barrier()
            with ExitStack() as st_t:
                R.cos, R.sin = rope_tables(st, st_t, "r", 128, 1, T_OWN, pos_own)
                P.barrier()
        R.ring = Ring(st, "wr1_", 4, [128, KC, 128])
        R.ring512 = Ring(st, "wr1b_", 2, [128, KC, 512])
        R.kT = salloc(st, "rkT", [128, 2, T_OWN], BF16)
        R.kd = salloc(st, "rkd", [128, 8, 256], BF16)
        R.v = salloc(st, "rv", [128, 8, 512], BF16)
        R.S = salloc(st, "rS", [128, 2, 512], F32)
        R.ropetmp = Ring(st, "rt1_", 4, [128, 512], F32)
        return R

    def ret_proj_rope(R, col0, outT, scale):
        wv = od_w_in.rearrange("(kc p) n -> p kc n", p=128)
        w1 = wload(R.ring, wv[:, :, col0:col0 + 128], KC, 128)
        w2 = wload(R.ring, wv[:, :, col0 + 128:col0 + 256], KC, 128)
        for (t0, n) in toks_loc:
            ps1 = psn()
            ps2 = psn()
            for (psx, wx) in ((ps1, w1), (ps2, w2)):
                for kc in range(KC):
                    P.op("pe", lambda e: e.matmul(psx.ap[:, 0:n], lhsT=wx.ap[:, kc, :], rhs=R.hT.ap[:, kc, t0:t0 + n], start=(kc == 0), stop=(kc == KC - 1)),
                         reads=[wx, R.hT], writes=[psx])
            rope_apply(R.ropetmp, ps1.ap[:, 0:n], ps2.ap[:, 0:n], [ps1, ps2], R.cos.ap[:, t0:t0 + n], R.sin.ap[:, t0:t0 + n],
                       outT.ap[:, 0, t0:t0 + n], outT.ap[:, 1, t0:t0 + n], [(outT, t0)], 128, n, scale=scale)

    def ret_kv(R, h):
        ret_proj_rope(R, 2048 + h * 256, R.kT, KSC)

        def ev_v(ps, col, cw, t0):
            P.op("act", lambda e: e.activation(out=R.v.ap[:, t0 // 128, col:col + cw], in_=ps.ap[:, 0:cw], func=AF.Copy),
                 reads=[ps], writes=[(R.v, t0 // 128)])
        linTok(R.ring512, od_w_in, 4096 + h * 512, 512, R.hT, KC, list(range(0, T_OWN, 128)), ev_v, piece=512)
        for gt in range(8):
            for dc in range(2):
                ps = psn()
                P.op("pe", lambda e: e.matmul(ps.ap[:, 0:128], lhsT=R.kT.ap[:, dc, gt * 128:(gt + 1) * 128], rhs=ident_b.ap, start=True, stop=True),
                     reads=[R.kT, ident_b], writes=[ps])
                P.op("dve", lambda e: e.tensor_scalar(out=R.kd.ap[:, gt, dc * 128:(dc + 1) * 128], in0=ps.ap[:, 0:128], scalar1=R.rc.ap[:, h:h + 1], scalar2=None, op0=ALU.mult),
                     reads=[ps, R.rc], writes=[(R.kd, gt)])

    def ret_state_update(R, h, gt, first):
        for dc in range(2):
            ps = psn()
            P.op("pe", lambda e: e.matmul(ps.ap, lhsT=R.kd.ap[:, gt, dc * 128:(dc + 1) * 128], rhs=R.v.ap[:, gt, :], start=True, stop=True),
                 reads=[(R.kd, gt), (R.v, gt)], writes=[ps])
            if first:
                P.op("dve", lambda e: e.tensor_copy(out=R.S.ap[:, dc, :], in_=ps.ap), reads=[ps], writes=[(R.S, dc)])
            else:
                P.op("dve", lambda e: e.scalar_tensor_tensor(out=R.S.ap[:, dc, :], in0=R.S.ap[:, dc, :], scalar=GAM[h] ** 128, in1=ps.ap, op0=ALU.mult, op1=ALU.add),
                     reads=[ps, (R.S, dc)], writes=[(R.S, dc)])

    state_out = dscratch("state_out", [8, 128, 2, 512], F32, out=(mode == "A"))
    x3T = dscratch("x3T", [128, KC, T_OWN])
    x4T = dscratch("x4T", [128, KC, T_OWN])

    CACHE = (mode == "F")
    if CACHE:
        c_kT = dscratch("c_kT", [8, 128, 2, T_OWN], BF16)
        c_kd = dscratch("c_kd", [8, 128, 8, 256], BF16)
        c_v = dscratch("c_v", [8, 128, 8, 512], BF16)
        c_hT = dscratch("c_hT", [128, KC, T_OWN], BF16)
        c_cs = dscratch("c_cs", [2, 128, T_OWN], F32)
    if upto >= 3 and mode in ("A", "F"):
        with ExitStack() as st:
            R = ret_common(st)
            if CACHE:
                P.dma("sp", c_hT.ap, R.hT.ap, reads=[R.hT], writes=[c_hT])
                P.dma("sp", c_cs.ap[0], R.cos.ap, reads=[R.cos], writes=[(c_cs, 0)])
                P.dma("sp", c_cs.ap[1], R.sin.ap, reads=[R.sin], writes=[(c_cs, 1)])
            for h in range(NHEADS):
                ret_kv(R, h)
                if CACHE:
                    P.dma("sp", c_kT.ap[h], R.kT.ap, reads=[R.kT], writes=[(c_kT, h)])
                    P.dma("sp", c_kd.ap[h], R.kd.ap, reads=[R.kd], writes=[(c_kd, h)])
                    P.dma("sp", c_v.ap[h], R.v.ap, reads=[R.v], writes=[(c_v, h)])
                for gt in range(8):
                    ret_state_update(R, h, gt, gt == 0)
                P.dma("sp", state_out.ap[h], R.S.ap, reads=[R.S], writes=[(state_out, h)])
            P.barrier()

    if mode == "B":
        state_in = Buf("state_in", None, din("state_in", [8, 128, 2, 512]))
    elif mode == "F":
        state_in_h = []
        for h in range(NHEADS):
            gt_ = nc.dram_tensor(f"state_gath{h}", [256, 1024], F32, kind="Internal")
            gath = Buf(f"state_gath{h}", gt_, gt_.ap())
            if NO_CC:
                P.dma("sp", gath.ap[0:128, :], state_out.ap[h].rearrange("p d e -> p (d e)"), reads=[(state_out, h)], writes=[gath])
            else:
                P.op("pool", lambda e: e.collective_compute("AllGather", ALU.bypass, replica_groups=[[0, 1], [2, 3], [4, 5], [6, 7]],
                                                            ins=[state_out.ap[h].rearrange("p d e -> p (d e)")], outs=[gath.ap]),
                     reads=[(state_out, h)], writes=[gath])
                P.flag_last("pool")
            state_in_h.append(gath)
    og_s = dscratch("og_s", [128, 32, T_OWN], BF16)
    if upto >= 4 and mode in ("B", "F"):
        with ExitStack() as st:
            R = ret_common(st, cached=CACHE)
            ogh_r = Ring(st, "rogh_", 2, [128, 4, T_OWN], BF16)
            qT = salloc(st, "rqT", [128, 2, T_OWN], BF16)
            qdT = salloc(st, "rqdT", [128, 2, T_OWN], BF16)
            gT = salloc(st, "rgT", [128, 4, T_OWN], BF16)
            Sb = salloc(st, "rSb", [128, 8, 1024], BF16)
            sc_r = Ring(st, "rsc_", 3, [128, 128], BF16)
            sq_r = Ring(st, "rsq_", 3, [128, 512], F32)
            rs_r = Ring(st, "rrs_", 2, [128, 128], F32)
            rstd_r = Ring(st, "rrstd_", 2, [128, 128], F32)
            tmp_r = Ring(st, "rtmp_", 3, [128, 128], F32)
            for h in range(NHEADS):
                if CACHE:
                    P.dma("sp", R.kT.ap, c_kT.ap[h], reads=[(c_kT, h)], writes=[R.kT])
                    P.dma("sp", R.kd.ap, c_kd.ap[h], reads=[(c_kd, h)], writes=[R.kd])
                    P.dma("sp", R.v.ap, c_v.ap[h], reads=[(c_v, h)], writes=[R.v])
                else:
                    ret_kv(R, h)
                ret_proj_rope(R, h * 256, qT, 1.0)
                for gt in range(8):
                    for dc in range(2):
                        P.op("dve", lambda e: e.tensor_tensor(out=qdT.ap[:, dc, gt * 128:(gt + 1) * 128], in0=qT.ap[:, dc, gt * 128:(gt + 1) * 128],
                                                               in1=R.rc.ap[:, 8 + h * 128:8 + (h + 1) * 128], op=ALU.mult), reads=[qT, R.rc], writes=[(qdT, gt)])

                def ev_g(ps, col, m, t0, n):
                    P.op("act", lambda e: e.activation(out=gT.ap[:, col // 128, t0:t0 + n], in_=ps.ap[:, 0:n], func=AF.Silu),
                         reads=[ps], writes=[(gT, (col // 128, t0))])
                linT(R.ring, od_w_in, 8192 + h * 512, 512, R.hT, KC, toks_loc, ev_g, piece=128)
                if mode == "F":
                    P.dma("sp", R.S.ap, state_in_h[h].ap[0:128, :].rearrange("p (d e) -> p d e", e=512), reads=[state_in_h[h]], writes=[R.S])
                else:
                    P.dma("sp", R.S.ap, state_in.ap[h], reads=[(state_in, h)], writes=[R.S])
                P.op("dve", lambda e: e.tensor_scalar(out=R.S.ap, in0=R.S.ap, scalar1=pvalid.ap[:, 0:1], scalar2=None, op0=ALU.mult), reads=[R.S, pvalid], writes=[R.S])
                for gt in range(8):
                    P.op("act", lambda e: e.activation(out=Sb.ap[:, gt, :], in_=R.S.ap.rearrange("p d e -> p (d e)"), func=AF.Copy), reads=[R.S], writes=[(Sb, gt)])
                    if gt < 7:
                        ret_state_update(R, h, gt, False)
                ogh = ogh_r.next()
                cst = [dict() for _ in range(8)]

                def r_t1(gt):
                    ts = slice(gt * 128, (gt + 1) * 128)
                    pss = psn()
                    for dc in range(2):
                        P.op("pe", lambda e: e.matmul(pss.ap[:, 0:128], lhsT=R.kT.ap[:, dc, ts], rhs=qT.ap[:, dc, ts], start=(dc == 0), stop=(dc == 1)),
                             reads=[R.kT, qT], writes=[pss])
                    sc = sc_r.next()
                    P.op("dve", lambda e: e.tensor_tensor(out=sc.ap, in0=pss.ap[:, 0:128], in1=R.rc.ap[:, 8 + 1024 + h * 128:8 + 1024 + (h + 1) * 128], op=ALU.mult),
                         reads=[pss, R.rc], writes=[sc])
                    cst[gt]["sc"] = sc

                def r_t2(gt):
                    ts = slice(gt * 128, (gt + 1) * 128)
                    sc = cst[gt]["sc"]
                    pso = psn()
                    for ec in range(4):
                        es = slice(ec * 128, (ec + 1) * 128)
                        P.op("pe", lambda e: e.matmul(pso.ap[:, es], lhsT=R.v.ap[:, gt, es], rhs=sc.ap, start=True, stop=False), reads=[(R.v, gt), sc], writes=[pso])
                        P.op("pe", lambda e: e.matmul(pso.ap[:, es], lhsT=Sb.ap[:, gt, ec * 128:(ec + 1) * 128], rhs=qdT.ap[:, 0, ts], start=False, stop=False),
                             reads=[(Sb, gt), (qdT, gt)], writes=[pso])
                        P.op("pe", lambda e: e.matmul(pso.ap[:, es], lhsT=Sb.ap[:, gt, 512 + ec * 128:512 + (ec + 1) * 128], rhs=qdT.ap[:, 1, ts], start=False, stop=True),
                             reads=[(Sb, gt), (qdT, gt)], writes=[pso])
                    sq = sq_r.next()
                    P.op("act", lambda e: e.activation(out=sq.ap, in_=pso.ap, func=AF.Square), reads=[pso], writes=[sq])
                    cst[gt].update(pso=pso, sq=sq)

                def r_t3(gt):
                    ts = slice(gt * 128, (gt + 1) * 128)
                    pso, sq = cst[gt]["pso"], cst[gt]["sq"]
                    psq = psn()
                    for ec in range(4):
                        P.op("pe", lambda e: e.matmul(psq.ap[:, 0:128], lhsT=ones_f, rhs=sq.ap[:, ec * 128:(ec + 1) * 128], start=(ec == 0), stop=(ec == 3)),
                             reads=[sq, consts], writes=[psq])
                    rs = rs_r.next()
                    P.op("act", lambda e: e.activation(out=rs.ap, in_=psq.ap[:, 0:128], func=AF.Sqrt, scale=1.0 / 512, bias=EPS), reads=[psq], writes=[rs])
                    rstd = rstd_r.next()
                    P.op("dve", lambda e: e.reciprocal(out=rstd.ap, in_=rs.ap), reads=[rs], writes=[rstd])
                    for ec in range(4):
                        tm = tmp_r.next()
                        P.op("dve", lambda e: e.tensor_tensor(out=tm.ap, in0=pso.ap[:, ec * 128:(ec + 1) * 128], in1=rstd.ap, op=ALU.mult), reads=[pso, rstd], writes=[tm])
                        P.op("dve", lambda e: e.tensor_tensor(out=ogh.ap[:, ec, ts], in0=tm.ap, in1=gT.ap[:, ec, ts], op=ALU.mult),
                             reads=[tm, gT], writes=[(ogh, (ec, gt))])
                for step in range(8 + 2):
                    if step < 8:
                        r_t1(step)
                    if 0 <= step - 1 < 8:
                        r_t2(step - 1)
                    if 0 <= step - 2 < 8:
                        r_t3(step - 2)
                P.dma("sp", og_s.ap[:, h * 4:(h + 1) * 4, :], ogh.ap, reads=[ogh], writes=[(og_s, h)])
            P.barrier()
        with ExitStack() as st:
            ogT = salloc(st, "ogT", [128, 32, T_OWN], BF16)
            P.dma("sp", ogT.ap, og_s.ap, reads=[og_s], writes=[ogT])
            ring32 = Ring(st, "wr32_", 3, [128, 32, 128])
            xo_r = Ring(st, "rxo_", 3, [128, 512], F32)
            res_r = Ring(st, "rres_", 3, [128, 512], F32)

            def ev_out1(ps, col, m, t0, n):
                fc = col // 128
                xo = xo_r.next()
                P.dma("sp", xo.ap[:, 0:n], x2T.ap[:, fc, t0:t0 + n], reads=[x2T], writes=[xo])
                res = res_r.next()
                P.op("dve", lambda e: e.scalar_tensor_tensor(out=res.ap[:, 0:n], in0=ps.ap[:, 0:n], scalar=modG.ap[:, 2, fc:fc + 1], in1=xo.ap[:, 0:n],
                                                              op0=ALU.mult, op1=ALU.add), reads=[ps, xo, (modG, 2)], writes=[res])
                P.dma("sp", x3T.ap[:, fc, t0:t0 + n], res.ap[:, 0:n], reads=[res], writes=[(x3T, (fc, t0))])
            linT(ring32, od_w_out, 0, D, ogT, 32, toks_loc, ev_out1, piece=128)
            P.barrier()
    if "x3" in dbg:
        d = dbg_tensor("x3", [128, KC, T_OWN])
        P.dma("sp", d.ap, x3T.ap, reads=[x3T], writes=[d])
    if upto >= 5 and mode in ("B", "F"):
        moe_layer(1, x3T, x4T, n_exp=X_NEXP)
        X.final_src = x4T
    if "x4" in dbg:
        d = dbg_tensor("x4", [128, KC, T_OWN])
        P.dma("sp", d.ap, x4T.ap, reads=[x4T], writes=[d])
    if upto >= 6 and mode in ("B", "F"):
        with ExitStack() as st:
            xs_r = Ring(st, "fxs_", 2, [128, KC, 512], F32)
            fo_r = Ring(st, "fo_", 3, [128, 512], F32)
            cur = {}

            def getx(c, tb):
                if c == 0:
                    xs = xs_r.next()
                    P.dma("sp", xs.ap, x4T.ap[:, :, tb * 512:(tb + 1) * 512], reads=[x4T], writes=[xs])
                    cur["xs"] = xs
                return cur["xs"].ap[:, c, :], [cur["xs"]]

            def emit(tm, c, tb):
                fo = fo_r.next()
                P.op("act", lambda e: e.activation(out=fo.ap, in_=tm.ap, func=AF.Copy, scale=norms.ap[:, 4, c:c + 1]), reads=[tm, norms], writes=[fo])
                P.dma("sp", outT.ap[:, c, tb * 512:(tb + 1) * 512], fo.ap, reads=[fo], writes=[(outT, (c, tb))])
            rmsnorm_T(st, "fin", getx, KC, T_OWN // 512, D, emit)
            P.barrier()
        X.final_src = None
    X.P = P
    X.nc = nc
    X.dbg_out = dbg_out
    return X, locals()


def _fm(v):
    v = np.asarray(v, np.float32)
    n = v.shape[-1] // 128
    return np.ascontiguousarray(v.reshape(n, 128).T)


def _xT(xb):
    T = xb.shape[0]
    return np.ascontiguousarray(xb.T.reshape(KC, 128, T).transpose(1, 0, 2))


def shared_inputs(I):
    S = {}
    S["consts"] = host_consts()
    S["w_mod"] = np.ascontiguousarray(np.stack([I["w_mod_mix"][0], I["w_mod_ffn"][0], I["w_mod_mix"][1], I["w_mod_ffn"][1]]))
    bm = [I["b_mod_mix"][0], I["b_mod_ffn"][0], I["b_mod_mix"][1], I["b_mod_ffn"][1]]
    S["b_mod"] = np.ascontiguousarray(np.stack([_fm(b) for b in bm], axis=1))
    nm = [I["norm_mix"][0], I["norm_ffn"][0], I["norm_mix"][1], I["norm_ffn"][1], I["final_norm"]]
    S["norms"] = np.ascontiguousarray(np.stack([_fm(b) for b in nm], axis=1))
    S["ev_w_in"] = np.ascontiguousarray(I["ev_w_in"][0])
    S["ev_q_norm"] = _fm(I["ev_q_norm"][0])
    S["ev_w_q_up"] = np.ascontiguousarray(I["ev_w_q_up"][0])
    S["ev_kv_norm"] = _fm(I["ev_kv_norm"][0])
    S["ev_w_kv_up"] = np.ascontiguousarray(I["ev_w_kv_up"][0])
    S["ev_w_out"] = np.ascontiguousarray(I["ev_w_out"][0])
    invf = np.zeros((128, 2), np.float32)
    p = np.arange(128)
    invf[:, 0] = np.exp(-math.log(10000.0) * (p % 32).astype(np.float32) / np.float32(32)).astype(np.float32)
    invf[:, 1] = np.exp(-math.log(10000.0) * p.astype(np.float32) / np.float32(128)).astype(np.float32)
    S["invf"] = invf
    rc = np.zeros((128, 8 + 2048), np.float64)
    idx = np.arange(128, dtype=np.float64)
    for h in range(8):
        lg = np.log1p(-(2.0 ** (-5.0 - h)))
        rc[:, h] = np.exp((127.0 - idx) * lg)
        rc[:, 8 + h * 128:8 + (h + 1) * 128] = np.exp((idx + 1.0) * lg)[None, :]
        rel = idx[None, :] - idx[:, None]
        rc[:, 8 + 1024 + h * 128:8 + 1024 + (h + 1) * 128] = np.where(rel >= 0, np.exp(np.maximum(rel, 0.0) * lg), 0.0)
    S["ret_consts"] = rc.astype(np.float32)
    S["od_w_in"] = np.ascontiguousarray(I["od_w_in"][0])
    S["od_w_out"] = np.ascontiguousarray(I["od_w_out"][0])
    S["moe_wr"] = np.ascontiguousarray(np.concatenate([I["moe_w_group"], I["moe_w_expert"]], axis=-1))
    S["moe_br"] = np.ascontiguousarray(np.concatenate([I["moe_b_group"], I["moe_b_expert"]], axis=-1)[:, None, :])
    S["moe_w_gate"] = np.asarray(I["moe_w_gate"])
    S["moe_w_up"] = np.asarray(I["moe_w_up"])
    S["moe_w_down"] = np.asarray(I["moe_w_down"])
    return S


def core_inputs(I, core):
    b, hf = core // 2, core % 2
    x = np.asarray(I["x"][b], np.float32)
    pos = np.asarray(I["positions"][b], np.int32)
    M = {}
    if hf == 0:
        xa = np.concatenate([np.zeros((T_OWN, D), np.float32), x[:T_OWN]], axis=0)
        pa = np.concatenate([np.zeros((T_OWN,), np.int32), pos[:T_OWN]])
    else:
        xa = x
        pa = pos
    M["xT_all"] = _xT(xa)
    M["pos_all"] = np.ascontiguousarray(pa.reshape(1, T_ALL))
    M["pvalid"] = np.full((128, 1), float(hf), np.float32)
    M["cond"] = _fm(I["c"][b])
    return M


def _tok(a):
    a = np.asarray(a)
    p, c, t = a.shape
    return np.ascontiguousarray(a.transpose(2, 1, 0).reshape(t, c * p))


FUSED = True


def kernel(**inputs):
    I = {k: np.asarray(v) for k, v in inputs.items()}
    n = 8
    S = shared_inputs(I)
    cores = [core_inputs(I, c) for c in range(n)]
    if FUSED:
        X, _ = build(mode="F")
        X.P.finish()
        maps = [{k: (cores[c][k] if k in cores[c] else S[k]) for k in X.in_names} for c in range(n)]
        res = run_bass_kernel_spmd(X.nc, maps, core_ids=list(range(n)))
        outs = [res.results[c]["outT"] for c in range(n)]
    else:
        XA, _ = build(upto=3, mode="A")
        XA.P.finish()
        maps = [{k: (cores[c][k] if k in cores[c] else S[k]) for k in XA.in_names} for c in range(n)]
        resA = run_bass_kernel_spmd(XA.nc, maps, core_ids=list(range(n)))
        XB, _ = build(mode="B")
        XB.P.finish()
        maps = []
        for c in range(n):
            M = dict(cores[c])
            M["x2T_in"] = np.asarray(resA.results[c]["x2T"])
            M["state_in"] = (np.asarray(resA.results[c - 1]["state_out"]) if c % 2 == 1
                             else np.zeros((8, 128, 2, 512), np.float32))
            maps.append({k: (M[k] if k in M else S[k]) for k in XB.in_names})
        resB = run_bass_kernel_spmd(XB.nc, maps, core_ids=list(range(n)))
        outs = [resB.results[c]["outT"] for c in range(n)]
    out = np.zeros((4, 2048, 2048), np.float32)
    for c in range(n):
        b, hf = c // 2, c % 2
        out[b, hf * T_OWN:(hf + 1) * T_OWN, :] = _tok(outs[c])
    return out
```

```python
import math
import bisect
import numpy as np
import concourse.bass as bass
import concourse.mybir as mybir
from concourse.bass_utils import run_bass_kernel_spmd

F32 = mybir.dt.float32
BF16 = mybir.dt.bfloat16
I32 = mybir.dt.int32
AF = mybir.ActivationFunctionType
ALU = mybir.AluOpType

D = 2048
KC = 16
T_OWN = 1024
T_ALL = 2048
SAME_SYNC = True


class Buf:
    def __init__(self, name, t, ap=None):
        self.name = name
        self.t = t
        self._ap = ap if ap is not None else t.ap()
        self.st = {}

    @property
    def ap(self):
        return self._ap

    def state(self, key):
        s = self.st.get(key)
        if s is None:
            s = [None, []]
            self.st[key] = s
        return s


class Eng:
    def __init__(self, nc, name, eng, ndma=0):
        self.name = name
        self.eng = eng
        self.sem = nc.alloc_semaphore("sem_" + name)
        self.insts = []
        self.snaps = []
        self.flag_seqs = []
        self.flag_vals = []
        self.count = 0
        self.clock = {}
        self.ndma = ndma
        self.dsems = [nc.alloc_semaphore(f"dsem_{name}_{i}") for i in range(ndma)]
        self.dcount = 0
        self.dsnaps = {}


class Prog:
    def __init__(self, nc):
        self.nc = nc
        self.E = {
            "pe": Eng(nc, "pe", nc.tensor),
            "act": Eng(nc, "act", nc.scalar),
            "dve": Eng(nc, "dve", nc.vector),
            "pool": Eng(nc, "pool", nc.gpsimd, ndma=8),
            "sp": Eng(nc, "sp", nc.sync, ndma=8),
        }

    def _known(self, e, ev):
        if ev[0] == "D":
            _, q, i = ev
            Q = self.E[q]
            return e.clock.get(("D", q, i % Q.ndma), -1) >= i
        p, seq = ev
        return e.clock.get(p, -1) >= seq

    def _merge(self, e, other, extra):
        c = dict(e.clock)
        if other:
            for k, v in other.items():
                if c.get(k, -1) < v:
                    c[k] = v
        for k, v in extra.items():
            if c.get(k, -1) < v:
                c[k] = v
        e.clock = c

    def wait(self, ename, ev):
        e = self.E[ename]
        if ev is None or self._known(e, ev):
            return
        if ev[0] == "D":
            _, q, i = ev
            Q = self.E[q]
            slot = i % Q.ndma
            e.eng.wait_ge(Q.dsems[slot], 16 * (i // Q.ndma + 1))
            self._merge(e, Q.dsnaps.get(i), {("D", q, slot): i})
            return
        p, seq = ev
        if p == ename and (not SAME_SYNC or p in ("pe", "sp")):
            return
        Pn = self.E[p]
        k = bisect.bisect_left(Pn.flag_seqs, seq)
        if k < len(Pn.flag_seqs):
            fseq, val = Pn.flag_seqs[k], Pn.flag_vals[k]
        else:
            Pn.count += 1
            val = Pn.count
            fseq = seq
            Pn.insts[seq].then_inc(Pn.sem, 1)
            Pn.flag_seqs.append(seq)
            Pn.flag_vals.append(val)
        e.eng.wait_ge(Pn.sem, val)
        self._merge(e, Pn.snaps[fseq], {p: fseq})

    def flag_last(self, ename):
        Pn = self.E[ename]
        seq = len(Pn.insts) - 1
        if Pn.flag_seqs and Pn.flag_seqs[-1] == seq:
            return
        Pn.count += 1
        Pn.insts[seq].then_inc(Pn.sem, 1)
        Pn.flag_seqs.append(seq)
        Pn.flag_vals.append(Pn.count)

    def _deps(self, reads, writes):
        deps = []
        for b, k in reads:
            keys = [k] if k is not None else list(b.st.keys()) + [None]
            for kk in set(keys + [None]):
                s = b.st.get(kk)
                if s and s[0] is not None:
                    deps.append(s[0])
        for b, k in writes:
            keys = [k] if k is not None else list(b.st.keys()) + [None]
            for kk in set(keys + [None]):
                s = b.st.get(kk)
                if s:
                    if s[0] is not None:
                        deps.append(s[0])
                    deps.extend(s[1])
        return deps

    def _update(self, ev, reads, writes):
        for b, k in reads:
            b.state(k)[1].append(ev)
        for b, k in writes:
            if k is None:
                b.st = {}
            s = b.state(k)
            s[0] = ev
            s[1] = []

    @staticmethod
    def _norm(lst):
        out = []
        for x in lst:
            if isinstance(x, Buf):
                out.append((x, None))
            else:
                out.append(x)
        return out

    def op(self, ename, fn, reads=(), writes=()):
        reads = self._norm(reads)
        writes = self._norm(writes)
        e = self.E[ename]
        for d in self._deps(reads, writes):
            self.wait(ename, d)
        inst = fn(e.eng)
        seq = len(e.insts)
        e.insts.append(inst)
        e.snaps.append(e.clock)
        ev = (ename, seq)
        self._update(ev, reads, writes)
        return ev

    def dma(self, qname, out, in_, reads=(), writes=()):
        reads = self._norm(reads)
        writes = self._norm(writes)
        q = self.E[qname]
        for d in self._deps(reads, writes):
            self.wait(qname, d)
        i = q.dcount
        q.dcount += 1
        if i >= q.ndma:
            self.wait(qname, ("D", qname, i - q.ndma))
        slot = i % q.ndma
        q.eng.dma_start(out=out, in_=in_).then_inc(q.dsems[slot], 16)
        q.dsnaps[i] = q.clock
        ev = ("D", qname, i)
        self._update(ev, reads, writes)
        return ev

    def barrier(self, bufs=()):
        evs = []
        for n, e in self.E.items():
            if e.insts:
                evs.append((n, len(e.insts) - 1))
            for i in range(max(0, e.dcount - e.ndma), e.dcount):
                evs.append(("D", n, i))
        for n in self.E:
            for ev in evs:
                if ev[0] != "D" and ev[0] == n:
                    continue
                self.wait(n, ev)

    def finish(self):
        evs = []
        for n, e in self.E.items():
            if e.insts:
                evs.append((n, len(e.insts) - 1))
            for i in range(max(0, e.dcount - e.ndma), e.dcount):
                evs.append(("D", n, i))
        for ev in evs:
            if ev[0] == "sp":
                continue
            self.wait("sp", ev)


NCONST = 128 * 3 + 2048 * 2
EPS = 1e-6


def host_consts():
    c = np.zeros((128, NCONST), np.float32)
    c[:, 0:128] = 1.0
    c[:, 128:256] = np.eye(128, dtype=np.float32)
    j = np.arange(128)[:, None]
    s = np.arange(128)[None, :]
    c[:, 256:384] = (j >= s).astype(np.float32)
    q = np.arange(512)[None, :]
    for jb in range(4):
        key = 128 * jb + np.arange(128)[:, None]
        c[:, 384 + jb * 512:384 + (jb + 1) * 512] = (key < q).astype(np.float32)
        c[:, 384 + 2048 + jb * 512:384 + 2048 + (jb + 1) * 512] = (key <= q).astype(np.float32)
    return c


class Ctx:
    pass


X_NEXP = 32
NO_CC = False
SKIP_L0 = False
NHEADS = 8


def build(upto=99, dbg=(), mode="F"):
    from contextlib import ExitStack
    nc = bass.Bass("TRN2", target_bir_lowering=False)
    P = Prog(nc)
    X = Ctx()
    dbg_out = {}

    X.in_names = []

    def din(name, shape, dt=F32):
        X.in_names.append(name)
        return nc.dram_tensor(name, list(shape), dt, kind="ExternalInput").ap()

    def dscratch(name, shape, dt=F32, out=False):
        t = nc.dram_tensor(name, list(shape), dt, kind="ExternalOutput" if out else "Internal")
        return Buf(name, t, t.ap())

    L0 = mode in ("A", "F") and not SKIP_L0
    xT_all = Buf("xT_all", None, din("xT_all", [128, KC, T_ALL])) if L0 else None
    pos_in = din("pos_all", [1, T_ALL], I32)
    pvalid_in = din("pvalid", [128, 1])
    cond_in = din("cond", [128, KC])
    consts_in = din("consts", [128, NCONST])
    w_mod = din("w_mod", [4, D, 3 * D])
    b_mod_in = din("b_mod", [128, 4, 48])
    norms_in = din("norms", [128, 5, KC])
    ev_w_in = din("ev_w_in", [D, 3904]) if L0 else None
    ev_qn_in = din("ev_q_norm", [128, 4]) if L0 else None
    ev_w_q_up = din("ev_w_q_up", [512, 1536]) if L0 else None
    ev_kvn_in = din("ev_kv_norm", [128, 2]) if L0 else None
    ev_w_kv_up = din("ev_w_kv_up", [256, 2048]) if L0 else None
    ev_w_out = din("ev_w_out", [D, D]) if L0 else None
    invf_in = din("invf", [128, 2])
    outT = dscratch("outT", [128, KC, T_OWN], F32, out=True)

    def dbg_tensor(name, shape, dt=F32):
        b = dscratch("dbg_" + name, shape, dt, out=True)
        dbg_out[name] = b
        return b

    gstack = ExitStack()

    def salloc(stack, name, shape, dt):
        X.uid = getattr(X, "uid", 0) + 1
        t = stack.enter_context(nc.sbuf_tensor(f"s{X.uid}_" + name, list(shape), dt))
        return Buf(name, t)

    PS = [Buf(f"ps{i}", gstack.enter_context(nc.psum_tensor(f"ps{i}", [128, 512], F32))) for i in range(8)]
    X.ps_i = 0

    X.ps_mod = 6

    def psn():
        b = PS[X.ps_i % X.ps_mod]
        X.ps_i += 1
        return b

    consts = salloc(gstack, "consts", [128, NCONST], F32)
    P.dma("sp", consts.ap, consts_in, writes=[consts])
    ones_f = consts.ap[:, 0:128]
    ident_f = consts.ap[:, 128:256]
    U_f = consts.ap[:, 256:384]

    def mstrict(j):
        return consts.ap[:, 384 + j * 512:384 + (j + 1) * 512]

    def mincl(j):
        return consts.ap[:, 384 + 2048 + j * 512:384 + 2048 + (j + 1) * 512]

    ones_b = salloc(gstack, "ones_b", [128, 128], BF16)
    P.op("dve", lambda e: e.tensor_copy(out=ones_b.ap, in_=ones_f), reads=[consts], writes=[ones_b])
    ident_b = salloc(gstack, "ident_b", [128, 128], BF16)
    P.op("dve", lambda e: e.tensor_copy(out=ident_b.ap, in_=ident_f), reads=[consts], writes=[ident_b])
    pvalid = salloc(gstack, "pvalid", [128, 1], F32)
    P.dma("sp", pvalid.ap, pvalid_in, writes=[pvalid])
    U_b = salloc(gstack, "U_b", [128, 128], BF16)
    P.op("dve", lambda e: e.tensor_copy(out=U_b.ap, in_=U_f), reads=[consts], writes=[U_b])
    ones_pv = salloc(gstack, "ones_pv", [128, 128], BF16)
    P.op("dve", lambda e: e.tensor_scalar(out=ones_pv.ap, in0=ones_f, scalar1=pvalid.ap[:, 0:1], scalar2=None, op0=ALU.mult), reads=[consts, pvalid], writes=[ones_pv])
    modA = salloc(gstack, "modA", [128, 4, KC], F32)
    modB = salloc(gstack, "modB", [128, 4, KC], F32)
    modG = salloc(gstack, "modG", [128, 4, KC], F32)
    norms = salloc(gstack, "norms", [128, 5, KC], F32)
    P.dma("sp", norms.ap, norms_in, writes=[norms])
    invf = salloc(gstack, "invf", [128, 2], F32)
    P.dma("sp", invf.ap, invf_in, writes=[invf])

    class Ring:
        def __init__(self, stack, name, n, shape, dt=BF16):
            self.b = [salloc(stack, f"{name}{i}", shape, dt) for i in range(n)]
            self.i = 0

        def next(self):
            b = self.b[self.i % len(self.b)]
            self.i += 1
            return b

    def wload(ring, view, kcn, cw):
        wb = ring.next()
        P.dma("pool", wb.ap[:, 0:kcn, 0:cw], view, writes=[wb])
        return wb

    def linT(ring, w2d, c0, ncols, act, kcn, toks, evac, piece=512):
        wv = w2d.rearrange("(kc p) n -> p kc n", p=128)
        for cc in range(c0, c0 + ncols, piece):
            cw = min(piece, c0 + ncols - cc)
            wb = wload(ring, wv[:, :, cc:cc + cw], kcn, cw)
            for oc in range(0, cw, 128):
                m = min(128, cw - oc)
                for (t0, n) in toks:
                    ps = psn()
                    for kc in range(kcn):
                        P.op("pe", lambda e, ps=ps, kc=kc, oc=oc, m=m, t0=t0, n=n, wb=wb: e.matmul(
                            ps.ap[0:m, 0:n], lhsT=wb.ap[:, kc, oc:oc + m], rhs=act.ap[:, kc, t0:t0 + n],
                            start=(kc == 0), stop=(kc == kcn - 1)),
                            reads=[wb, act], writes=[ps])
                    evac(ps, cc + oc - c0, m, t0, n)

    def linTok(ring, w2d, c0, ncols, act, kcn, ttiles, evac, piece=512):
        wv = w2d.rearrange("(kc p) n -> p kc n", p=128)
        for cc in range(c0, c0 + ncols, piece):
            cw = min(piece, c0 + ncols - cc)
            wb = wload(ring, wv[:, :, cc:cc + cw], kcn, cw)
            for t0 in ttiles:
                ps = psn()
                for kc in range(kcn):
                    P.op("pe", lambda e, ps=ps, kc=kc, cw=cw, t0=t0, wb=wb: e.matmul(
                        ps.ap[:, 0:cw], lhsT=act.ap[:, kc, t0:t0 + 128], rhs=wb.ap[:, kc, 0:cw],
                        start=(kc == 0), stop=(kc == kcn - 1)),
                        reads=[wb, act], writes=[ps])
                evac(ps, cc - c0, cw, t0)

    cond = salloc(gstack, "cond", [128, KC], F32)
    P.dma("sp", cond.ap, cond_in, writes=[cond])
    condb = salloc(gstack, "condb", [128, KC], BF16)
    P.op("act", lambda e: e.activation(out=condb.ap, in_=cond.ap, func=AF.Silu), reads=[cond], writes=[condb])
    bmod = salloc(gstack, "bmod", [128, 4, 48], F32)
    P.dma("sp", bmod.ap, b_mod_in, writes=[bmod])
    modT = salloc(gstack, "modT", [128, 4, 48], F32)

    def mod_piece(l, j, get_w):
        wv = w_mod[l].rearrange("(kc p) n -> p kc n", p=128)
        wb, wap = get_w()
        P.dma("pool", wap, wv[:, :, j * 128:(j + 1) * 128], writes=[wb])
        ps = psn()
        for kc in range(KC):
            P.op("pe", lambda e: e.matmul(ps.ap[:, 0:1], lhsT=wap[:, kc, :], rhs=condb.ap[:, kc:kc + 1], start=(kc == 0), stop=(kc == KC - 1)),
                 reads=[wb, condb], writes=[ps])
        P.op("dve", lambda e: e.tensor_tensor(out=modT.ap[:, l, j:j + 1], in0=ps.ap[:, 0:1], in1=bmod.ap[:, l, j:j + 1], op=ALU.add),
             reads=[ps, bmod], writes=[(modT, (l, j))])

    def mod_finalize(l):
        P.op("dve", lambda e: e.scalar_tensor_tensor(out=modA.ap[:, l, :], in0=modT.ap[:, l, 16:32], scalar=1.0,
                                                      in1=norms.ap[:, l, :], op0=ALU.add, op1=ALU.mult),
             reads=[modT, norms], writes=[(modA, l)])
        P.op("dve", lambda e: e.tensor_copy(out=modB.ap[:, l, :], in_=modT.ap[:, l, 0:16]), reads=[modT], writes=[(modB, l)])
        P.op("dve", lambda e: e.tensor_copy(out=modG.ap[:, l, :], in_=modT.ap[:, l, 32:48]), reads=[modT], writes=[(modG, l)])

    X.mod_todo = {}

    def mod_run(l, n, get_w):
        j0 = X.mod_todo.get(l, 0)
        if j0 >= 48:
            return
        j1 = 48 if n is None else min(48, j0 + n)
        for j in range(j0, j1):
            mod_piece(l, j, get_w)
        X.mod_todo[l] = j1
        if j1 >= 48:
            mod_finalize(l)

    with ExitStack() as st:
        ring0 = Ring(st, "w0r", 4, [128, KC, 128])

        def getw0():
            b = ring0.next()
            return b, b.ap
        first_l = [0] if mode in ("A", "F") else [2]
        for l in first_l:
            mod_run(l, None, getw0)
        if mode == "A":
            mod_run(1, None, getw0)
        if mode == "B":
            mod_run(3, None, getw0)
        if "mod" in dbg:
            d = dbg_tensor("mod", [128, 4, 48])
            P.dma("sp", d.ap, modT.ap, reads=[modT], writes=[d])
        P.barrier()

    def rmsnorm_T(st, tag, getx, nch, ntb, dim, emit, npart=128):
        sq_r = Ring(st, f"sq{tag}_", 3, [128, 512], BF16)
        tmp_r = Ring(st, f"tm{tag}_", 3, [128, 512], F32)
        rs = salloc(st, f"rs{tag}", [128, 512], F32)
        rstd = salloc(st, f"rstd{tag}", [128, 512], F32)
        for tb in range(ntb):
            ps = psn()
            xs = [getx(c, tb) for c in range(nch)]
            for c in range(nch):
                sq = sq_r.next()
                xap, xdeps = xs[c]
                P.op("act", lambda e, sq=sq, xap=xap: e.activation(out=sq.ap, in_=xap, func=AF.Square),
                     reads=xdeps, writes=[sq])
                P.op("pe", lambda e, sq=sq, ps=ps, c=c: e.matmul(ps.ap, lhsT=ones_b.ap, rhs=sq.ap, start=(c == 0), stop=(c == nch - 1)),
                     reads=[sq, ones_b], writes=[ps])
            P.op("act", lambda e, ps=ps: e.activation(out=rs.ap, in_=ps.ap, func=AF.Sqrt, scale=1.0 / dim, bias=EPS),
                 reads=[ps], writes=[rs])
            P.op("dve", lambda e: e.reciprocal(out=rstd.ap, in_=rs.ap), reads=[rs], writes=[rstd])
            for c in range(nch):
                tm = tmp_r.next()
                xap, xdeps = xs[c]
                P.op("dve", lambda e, tm=tm, xap=xap: e.tensor_tensor(out=tm.ap, in0=xap, in1=rstd.ap, op=ALU.mult),
                     reads=list(xdeps) + [rstd], writes=[tm])
                emit(tm, c, tb)

    def prologue(st, l, xsrc, T, hT, hook=None):
        xs_r = Ring(st, f"xs{l}_", 2, [128, KC, 512], F32)
        cur = {}

        def getx(c, tb):
            if c == 0:
                xs = xs_r.next()
                P.dma("sp", xs.ap, xsrc.ap[:, :, tb * 512:(tb + 1) * 512], reads=[xsrc], writes=[xs])
                cur["xs"] = xs
            return cur["xs"].ap[:, c, :], [cur["xs"]]

        def emit(tm, c, tb):
            if hook is None:
                P.op("act", lambda e: e.activation(
                    out=hT.ap[:, c, tb * 512:(tb + 1) * 512], in_=tm.ap, func=AF.Identity,
                    scale=modA.ap[:, l, c:c + 1], bias=modB.ap[:, l, c:c + 1]),
                    reads=[tm, (modA, l), (modB, l)], writes=[(hT, (c, tb))])
            else:
                hook(tm, c, tb)
        rmsnorm_T(st, f"p{l}", getx, KC, T // 512, D, emit)

    def rope_tables(st, st_tmp, tag, npart, col, T, pos_ap):
        o_sin = salloc(st, f"sin{tag}", [npart, T], F32)
        o_cos = salloc(st, f"cos{tag}", [npart, T], F32)
        posi = salloc(st_tmp, f"posi{tag}", [npart, T], I32)
        P.dma("sp", posi.ap, pos_ap.broadcast_to([npart, T]), writes=[posi])
        ang = salloc(st_tmp, f"ang{tag}", [npart, T], F32)
        P.op("dve", lambda e: e.tensor_copy(out=ang.ap, in_=posi.ap), reads=[posi], writes=[ang])
        P.op("dve", lambda e: e.tensor_scalar(out=ang.ap, in0=ang.ap, scalar1=invf.ap[0:npart, col:col + 1], scalar2=None, op0=ALU.mult),
             reads=[ang, invf], writes=[ang])
        a = salloc(st_tmp, f"a{tag}", [npart, T], F32)
        t = salloc(st_tmp, f"t{tag}", [npart, T], F32)
        ni = salloc(st_tmp, f"ni{tag}", [npart, T], I32)
        outs = []
        TWO_PI = 2.0 * math.pi
        C1 = 6.28125
        C2 = TWO_PI - C1
        for nm, off in (("sin", 0.0), ("cos", math.pi / 2)):
            o = o_sin if nm == "sin" else o_cos
            P.op("dve", lambda e, off=off: e.tensor_scalar(out=a.ap, in0=ang.ap, scalar1=off, scalar2=None, op0=ALU.add), reads=[ang], writes=[a])
            P.op("dve", lambda e: e.tensor_scalar(out=t.ap, in0=a.ap, scalar1=1.0 / TWO_PI, scalar2=None, op0=ALU.mult), reads=[a], writes=[t])
            P.op("dve", lambda e: e.tensor_copy(out=ni.ap, in_=t.ap), reads=[t], writes=[ni])
            P.op("dve", lambda e: e.tensor_copy(out=t.ap, in_=ni.ap), reads=[ni], writes=[t])
            P.op("dve", lambda e: e.scalar_tensor_tensor(out=a.ap, in0=t.ap, scalar=-C1, in1=a.ap, op0=ALU.mult, op1=ALU.add), reads=[t, a], writes=[a])
            P.op("dve", lambda e: e.scalar_tensor_tensor(out=a.ap, in0=t.ap, scalar=-C2, in1=a.ap, op0=ALU.mult, op1=ALU.add), reads=[t, a], writes=[a])
            P.op("dve", lambda e: e.tensor_scalar(out=t.ap, in0=a.ap, scalar1=math.pi, scalar2=-TWO_PI, op0=ALU.is_gt, op1=ALU.mult), reads=[a], writes=[t])
            P.op("dve", lambda e: e.tensor_tensor(out=a.ap, in0=a.ap, in1=t.ap, op=ALU.add), reads=[t, a], writes=[a])
            P.op("dve", lambda e: e.tensor_scalar(out=t.ap, in0=a.ap, scalar1=-math.pi, scalar2=TWO_PI, op0=ALU.is_lt, op1=ALU.mult), reads=[a], writes=[t])
            P.op("dve", lambda e: e.tensor_tensor(out=a.ap, in0=a.ap, in1=t.ap, op=ALU.add), reads=[t, a], writes=[a])
            P.op("dve", lambda e: e.tensor_scalar(out=a.ap, in0=a.ap, scalar1=-math.pi, scalar2=math.pi, op0=ALU.max, op1=ALU.min), reads=[a], writes=[a])
            P.op("act", lambda e, o=o: e.activation(out=o.ap, in_=a.ap, func=AF.Sin), reads=[a], writes=[o])
            outs.append(o)
        return outs[1], outs[0]

    def rope_apply(tmp_r, x1, x2, d1, cos_ap, sin_ap, o1, o2, o_w, npart, n, scale=1.0):
        for (oa, fa, fb, opx) in ((o1, cos_ap, sin_ap, ALU.subtract), (o2, sin_ap, cos_ap, ALU.add)):
            ta = tmp_r.next()
            tb_ = tmp_r.next()
            P.op("dve", lambda e, ta=ta, fa=fa: e.tensor_tensor(out=ta.ap[0:npart, 0:n], in0=x1, in1=fa, op=ALU.mult), reads=d1, writes=[ta])
            P.op("dve", lambda e, tb_=tb_, fb=fb: e.tensor_tensor(out=tb_.ap[0:npart, 0:n], in0=x2, in1=fb, op=ALU.mult), reads=d1, writes=[tb_])
            P.op("dve", lambda e, ta=ta, tb_=tb_, opx=opx: e.tensor_tensor(out=ta.ap[0:npart, 0:n], in0=ta.ap[0:npart, 0:n], in1=tb_.ap[0:npart, 0:n], op=opx),
                 reads=[ta, tb_], writes=[ta])
            P.op("dve", lambda e, ta=ta, oa=oa: e.tensor_scalar(out=oa, in0=ta.ap[0:npart, 0:n], scalar1=scale, scalar2=None, op0=ALU.mult),
                 reads=[ta], writes=o_w)

    x1T = dscratch("x1T", [128, KC, T_OWN])
    OWN0 = T_ALL - T_OWN
    if upto >= 1 and L0:
      with ExitStack() as st:
        hT = salloc(st, "hT0", [128, KC, T_ALL], BF16)
        with ExitStack() as st2:
            prologue(st2, 0, xT_all, T_ALL, hT)
            if "h0" in dbg:
                d = dbg_tensor("h0", [128, KC, T_ALL], BF16)
                P.dma("sp", d.ap, hT.ap, reads=[hT], writes=[d])
            P.barrier()
        with ExitStack() as st_t:
            cosm, sinm = rope_tables(st, st_t, "m", 32, 0, T_ALL, pos_in)
            P.barrier()
        cqn = salloc(st, "cqn", [128, 4, T_OWN], BF16)
        ckvn = salloc(st, "ckvn", [128, 2, T_ALL], BF16)
        k1 = salloc(st, "k1", [32, T_ALL], BF16)
        k2 = salloc(st, "k2", [32, T_ALL], BF16)
        ring = Ring(st, "wr0_", 4, [128, KC, 128])
        toks_all = [(t, 512) for t in range(0, T_ALL, 512)]
        toks_own = [(t, 512) for t in range(OWN0, T_ALL, 512)]
        ISQ = 128 ** -0.5
        MSC = 192 ** -0.5
        with ExitStack() as st1:
            cq = salloc(st1, "cq", [128, 4, T_OWN], F32)
            ckv = salloc(st1, "ckv", [128, 2, T_ALL], F32)
            st1a = ExitStack()
            kr = salloc(st1a, "kr", [32, 2, T_ALL], F32)

            def ev_cq(ps, col, m, t0, n):
                P.op("act", lambda e: e.activation(out=cq.ap[:, col // 128, t0 - OWN0:t0 - OWN0 + n], in_=ps.ap[:, 0:n], func=AF.Copy),
                     reads=[ps], writes=[(cq, (col // 128, t0))])
            linT(ring, ev_w_in, 3072, 512, hT, KC, toks_own, ev_cq, piece=128)

            def ev_ckv(ps, col, m, t0, n):
                P.op("act", lambda e: e.activation(out=ckv.ap[:, col // 128, t0:t0 + n], in_=ps.ap[:, 0:n], func=AF.Copy),
                     reads=[ps], writes=[(ckv, (col // 128, t0))])
            linT(ring, ev_w_in, 3584, 256, hT, KC, toks_all, ev_ckv, piece=128)
            for half in range(2):
                def ev_kr(ps, col, m, t0, n, half=half):
                    P.op("act", lambda e: e.activation(out=kr.ap[:, half, t0:t0 + n], in_=ps.ap[0:32, 0:n], func=AF.Copy),
                         reads=[ps], writes=[(kr, (half, t0))])
                linT(ring, ev_w_in, 3840 + 32 * half, 32, hT, KC, toks_all, ev_kr, piece=32)
            ropetmp = Ring(st1a, "rt_", 4, [128, 512], F32)
            for (t0, n) in toks_all:
                rope_apply(ropetmp, kr.ap[:, 0, t0:t0 + n], kr.ap[:, 1, t0:t0 + n], [kr],
                           cosm.ap[:, t0:t0 + n], sinm.ap[:, t0:t0 + n],
                           k1.ap[:, t0:t0 + n], k2.ap[:, t0:t0 + n], [(k1, t0), (k2, t0)], 32, n)
            P.barrier()
            st1a.close()
            qng = salloc(st1, "qng", [128, 4], F32)
            P.dma("sp", qng.ap, ev_qn_in, writes=[qng])
            kvng = salloc(st1, "kvng", [128, 2], F32)
            P.dma("sp", kvng.ap, ev_kvn_in, writes=[kvng])

            def emit_cq(tm, c, tb):
                P.op("act", lambda e: e.activation(out=cqn.ap[:, c, tb * 512:(tb + 1) * 512], in_=tm.ap, func=AF.Copy, scale=qng.ap[:, c:c + 1]),
                     reads=[tm, qng], writes=[(cqn, (c, tb))])
            with ExitStack() as stn:
                rmsnorm_T(stn, "cq", lambda c, tb: (cq.ap[:, c, tb * 512:(tb + 1) * 512], [cq]), 4, T_OWN // 512, 512, emit_cq)
                P.barrier()

            def emit_ckv(tm, c, tb):
                P.op("act", lambda e: e.activation(out=ckvn.ap[:, c, tb * 512:(tb + 1) * 512], in_=tm.ap, func=AF.Copy, scale=kvng.ap[:, c:c + 1]),
                     reads=[tm, kvng], writes=[(ckvn, (c, tb))])
            with ExitStack() as stn:
                rmsnorm_T(stn, "ckv", lambda c, tb: (ckv.ap[:, c, tb * 512:(tb + 1) * 512], [ckv]), 2, T_ALL // 512, 256, emit_ckv)
            P.barrier()

        oT = salloc(st, "oT", [128, KC, T_OWN], BF16)
        with ExitStack() as st2:
            qh = salloc(st2, "sbq", [128, T_OWN], BF16)
            kh = salloc(st2, "sbk", [128, T_ALL], BF16)
            vh = salloc(st2, "sbv", [128, 16, 128], BF16)
            e_r = Ring(st2, "sbe_", 3, [128, 512], F32)
            sp_r = Ring(st2, "sbs_", 2, [128, 512], F32)
            L_r = Ring(st2, "sbl_", 2, [128, 512], BF16)
            lsf_r = Ring(st2, "sblsf_", 2, [128, 512], F32)
            lsb_r = Ring(st2, "sblsb_", 2, [128, 512], BF16)
            er_r = Ring(st2, "sber_", 2, [128, 512], F32)
            a_r = Ring(st2, "sba_", 2, [128, 512], BF16)
            def getw_l0():
                b = ring.next()
                return b, b.ap
            for h in range(8):
                if mode == "F":
                    mod_run(1, 6, getw_l0)

                def ev_q(ps, col, m, t0, n):
                    P.op("act", lambda e: e.activation(out=qh.ap[:, t0 - OWN0:t0 - OWN0 + n], in_=ps.ap[:, 0:n], func=AF.Copy, scale=ISQ),
                         reads=[ps], writes=[(qh, t0)])
                linT(ring, ev_w_in, h * 128, 128, hT, KC, toks_own, ev_q, piece=128)

                def ev_k(ps, col, m, t0, n):
                    P.op("act", lambda e: e.activation(out=kh.ap[:, t0:t0 + n], in_=ps.ap[:, 0:n], func=AF.Copy),
                         reads=[ps], writes=[(kh, t0)])
                linT(ring, ev_w_in, 1024 + h * 128, 128, hT, KC, toks_all, ev_k, piece=128)

                def ev_v(ps, col, cw, t0):
                    P.op("dve", lambda e: e.tensor_copy(out=vh.ap[:, t0 // 128, :], in_=ps.ap[:, 0:128]),
                         reads=[ps], writes=[(vh, t0 // 128)])
                linTok(ring, ev_w_in, 2048 + h * 128, 128, hT, KC, list(range(0, T_ALL, 128)), ev_v, piece=128)
                P.op("dve", lambda e: e.tensor_scalar(out=vh.ap[:, 0:8, :], in0=vh.ap[:, 0:8, :], scalar1=pvalid.ap[:, 0:1], scalar2=None, op0=ALU.mult),
                     reads=[vh, pvalid], writes=[vh])
                tiles = []
                for s in range(2):
                    nkb = 8 + 4 * s + 4
                    for kb in range(nkb - 1, -1, -1):
                        tiles.append((s, kb, kb == nkb - 1, kb == 0))
                stt = [dict() for _ in tiles]

                def sb_s1(i):
                    s, kb, first, last = tiles[i]
                    q0 = s * 512
                    T = stt[i]
                    psz = psn()
                    P.op("pe", lambda e: e.matmul(psz.ap, lhsT=kh.ap[:, kb * 128:(kb + 1) * 128], rhs=qh.ap[:, q0:q0 + 512], start=True, stop=True),
                         reads=[kh, qh], writes=[psz])
                    eb = e_r.next()
                    P.op("act", lambda e: e.activation(out=eb.ap, in_=psz.ap, func=AF.Exp), reads=[psz], writes=[eb])
                    jd = kb - (8 + 4 * s)
                    Lb = L_r.next()
                    if jd >= 0:
                        spb = sp_r.next()
                        P.op("act", lambda e: e.activation(out=spb.ap, in_=eb.ap, func=AF.Ln, bias=1.0), reads=[eb], writes=[spb])
                        P.op("dve", lambda e: e.tensor_tensor(out=Lb.ap, in0=spb.ap, in1=mstrict(jd), op=ALU.mult), reads=[spb, consts], writes=[Lb])
                        P.flag_last("dve")
                    else:
                        P.op("act", lambda e: e.activation(out=Lb.ap, in_=eb.ap, func=AF.Ln, bias=1.0), reads=[eb], writes=[Lb])
                        P.flag_last("act")
                    T.update(eb=eb, Lb=Lb, jd=jd)

                def sb_s2(i):
                    s, kb, first, last = tiles[i]
                    T = stt[i]
                    Lb = T["Lb"]
                    psr = psn()
                    P.op("pe", lambda e: e.matmul(psr.ap, lhsT=U_b.ap, rhs=Lb.ap, start=True, stop=first), reads=[Lb, U_b], writes=[psr])
                    if not first:
                        prev_b = stt[i - 1]["Lsb"]
                        P.op("pe", lambda e: e.matmul(psr.ap, lhsT=ones_b.ap, rhs=prev_b.ap, start=False, stop=True), reads=[prev_b, ones_b], writes=[psr])
                    erb = er_r.next()
                    P.op("act", lambda e: e.activation(out=erb.ap, in_=psr.ap, func=AF.Exp, scale=-1.0), reads=[psr], writes=[erb])
                    P.flag_last("act")
                    Lsf = lsf_r.next()
                    if first:
                        P.op("dve", lambda e: e.tensor_copy(out=Lsf.ap, in_=Lb.ap), reads=[Lb], writes=[Lsf])
                    else:
                        prev_f = stt[i - 1]["Lsf"]
                        P.op("dve", lambda e: e.tensor_tensor(out=Lsf.ap, in0=prev_f.ap, in1=Lb.ap, op=ALU.add), reads=[Lb, prev_f], writes=[Lsf])
                    Lsb = lsb_r.next()
                    P.op("dve", lambda e: e.tensor_copy(out=Lsb.ap, in_=Lsf.ap), reads=[Lsf], writes=[Lsb])
                    P.flag_last("dve")
                    T.update(erb=erb, Lsf=Lsf, Lsb=Lsb)

                def sb_s3(i):
                    s, kb, first, last = tiles[i]
                    q0 = s * 512
                    T = stt[i]
                    eb, erb, jd = T["eb"], T["erb"], T["jd"]
                    pso = PS[6 + s]
                    ab = a_r.next()
                    if jd >= 0:
                        P.op("dve", lambda e: e.tensor_tensor(out=erb.ap, in0=erb.ap, in1=mstrict(jd), op=ALU.mult), reads=[erb, consts], writes=[erb])
                    P.op("dve", lambda e: e.tensor_tensor(out=ab.ap, in0=eb.ap, in1=erb.ap, op=ALU.mult), reads=[eb, erb], writes=[ab])
                    P.op("pe", lambda e: e.matmul(pso.ap[:, 0:512], lhsT=vh.ap[:, kb, :], rhs=ab.ap, start=first, stop=last),
                         reads=[vh, ab], writes=[pso])
                    if last:
                        P.op("act", lambda e: e.activation(out=oT.ap[:, h, q0:q0 + 512], in_=pso.ap, func=AF.Copy), reads=[pso], writes=[(oT, (h, s))])
                nt = len(tiles)
                for step in range(nt + 2):
                    if step < nt:
                        sb_s1(step)
                    if 0 <= step - 1 < nt:
                        sb_s2(step - 1)
                    if 0 <= step - 2 < nt:
                        sb_s3(step - 2)
            P.barrier()

        with ExitStack() as st3:
            qn = salloc(st3, "mqn", [128, T_OWN], BF16)
            q1 = salloc(st3, "mq1", [32, T_OWN], BF16)
            q2 = salloc(st3, "mq2", [32, T_OWN], BF16)
            kn = salloc(st3, "mkn", [128, T_ALL], BF16)
            vm = salloc(st3, "mv", [128, 16, 128], BF16)
            ropetmp = Ring(st3, "rt3_", 4, [128, 512], F32)
            p_r = Ring(st3, "mp_", 4, [128, 512], BF16)
            pf_r = Ring(st3, "mpf_", 3, [128, 512], F32)
            rden = salloc(st3, "mrden", [128, 512], F32)
            toks_loc = [(0, 512), (512, 512)]
            wq = ev_w_q_up.rearrange("(kc p) n -> p kc n", p=128)
            for h in range(8):
                def ev_qn(ps, col, m, t0, n):
                    P.op("act", lambda e: e.activation(out=qn.ap[:, t0:t0 + n], in_=ps.ap[:, 0:n], func=AF.Copy, scale=MSC),
                         reads=[ps], writes=[(qn, t0)])
                linT(ring, ev_w_q_up, h * 192, 128, cqn, 4, toks_loc, ev_qn, piece=128)
                wb = wload(ring, wq[:, :, h * 192 + 128:h * 192 + 192], 4, 64)
                for (t0, n) in toks_loc:
                    ps1 = psn()
                    ps2 = psn()
                    for (psx, off) in ((ps1, 0), (ps2, 32)):
                        for kc in range(4):
                            P.op("pe", lambda e: e.matmul(psx.ap[0:32, 0:n], lhsT=wb.ap[:, kc, off:off + 32], rhs=cqn.ap[:, kc, t0:t0 + n],
                                                          start=(kc == 0), stop=(kc == 3)), reads=[wb, cqn], writes=[psx])
                    rope_apply(ropetmp, ps1.ap[0:32, 0:n], ps2.ap[0:32, 0:n], [ps1, ps2],
                               cosm.ap[:, OWN0 + t0:OWN0 + t0 + n], sinm.ap[:, OWN0 + t0:OWN0 + t0 + n],
                               q1.ap[:, t0:t0 + n], q2.ap[:, t0:t0 + n], [(q1, t0), (q2, t0)], 32, n, scale=MSC)

                def ev_kn(ps, col, m, t0, n):
                    P.op("act", lambda e: e.activation(out=kn.ap[:, t0:t0 + n], in_=ps.ap[:, 0:n], func=AF.Copy),
                         reads=[ps], writes=[(kn, t0)])
                linT(ring, ev_w_kv_up, h * 256, 128, ckvn, 2, toks_all, ev_kn, piece=128)

                def ev_vm(ps, col, cw, t0):
                    P.op("dve", lambda e: e.tensor_copy(out=vm.ap[:, t0 // 128, :], in_=ps.ap[:, 0:128]),
                         reads=[ps], writes=[(vm, t0 // 128)])
                linTok(ring, ev_w_kv_up, h * 256 + 128, 128, ckvn, 2, list(range(0, T_ALL, 128)), ev_vm, piece=128)
                P.op("dve", lambda e: e.tensor_scalar(out=vm.ap[:, 0:8, :], in0=vm.ap[:, 0:8, :], scalar1=pvalid.ap[:, 0:1], scalar2=None, op0=ALU.mult),
                     reads=[vm, pvalid], writes=[vm])
                for s in range(2):
                    q0 = s * 512
                    nkb = 8 + 4 * s + 4
                    pso = PS[6]
                    psd = PS[7]
                    pbs = {}

                    def m_s1(kb):
                        psz = psn()
                        ks = slice(kb * 128, (kb + 1) * 128)
                        P.op("pe", lambda e: e.matmul(psz.ap, lhsT=kn.ap[:, ks], rhs=qn.ap[:, q0:q0 + 512], start=True, stop=False),
                             reads=[kn, qn], writes=[psz])
                        P.op("pe", lambda e: e.matmul(psz.ap, lhsT=k1.ap[:, ks], rhs=q1.ap[:, q0:q0 + 512], start=False, stop=False),
                             reads=[k1, q1], writes=[psz])
                        P.op("pe", lambda e: e.matmul(psz.ap, lhsT=k2.ap[:, ks], rhs=q2.ap[:, q0:q0 + 512], start=False, stop=True),
                             reads=[k2, q2], writes=[psz])
                        jd = kb - (8 + 4 * s)
                        pb = p_r.next()
                        if jd >= 0:
                            pf = pf_r.next()
                            P.op("act", lambda e: e.activation(out=pf.ap, in_=psz.ap, func=AF.Exp), reads=[psz], writes=[pf])
                            P.op("dve", lambda e: e.tensor_tensor(out=pb.ap, in0=pf.ap, in1=mincl(jd), op=ALU.mult), reads=[pf, consts], writes=[pb])
                        else:
                            P.op("act", lambda e: e.activation(out=pb.ap, in_=psz.ap, func=AF.Exp), reads=[psz], writes=[pb])
                        pbs[kb] = pb

                    def m_s2(kb):
                        first = (kb == 0)
                        last = (kb == nkb - 1)
                        pb = pbs[kb]
                        onesx = ones_pv if kb < 8 else ones_b
                        P.op("pe", lambda e: e.matmul(psd.ap, lhsT=onesx.ap, rhs=pb.ap, start=first, stop=last), reads=[onesx, pb], writes=[psd])
                        P.op("pe", lambda e: e.matmul(pso.ap, lhsT=vm.ap[:, kb, :], rhs=pb.ap, start=first, stop=last), reads=[vm, pb], writes=[pso])
                    for step in range(nkb + 2):
                        if step < nkb:
                            m_s1(step)
                        if 0 <= step - 2 < nkb:
                            m_s2(step - 2)
                    P.op("dve", lambda e: e.reciprocal(out=rden.ap, in_=psd.ap), reads=[psd], writes=[rden])
                    P.op("dve", lambda e: e.tensor_tensor(out=oT.ap[:, 8 + h, q0:q0 + 512], in0=pso.ap, in1=rden.ap, op=ALU.mult),
                         reads=[pso, rden], writes=[(oT, (8 + h, s))])
            P.barrier()

        with ExitStack() as st4:
            if "o0" in dbg:
                d = dbg_tensor("o0", [128, KC, T_OWN], BF16)
                P.dma("sp", d.ap, oT.ap, reads=[oT], writes=[d])
            xo_r = Ring(st4, "xo_", 3, [128, 512], F32)
            res_r = Ring(st4, "res_", 3, [128, 512], F32)

            def ev_out(ps, col, m, t0, n):
                fc = col // 128
                xo = xo_r.next()
                P.dma("sp", xo.ap[:, 0:n], xT_all.ap[:, fc, OWN0 + t0:OWN0 + t0 + n], reads=[xT_all], writes=[xo])
                res = res_r.next()
                P.op("dve", lambda e: e.scalar_tensor_tensor(out=res.ap[:, 0:n], in0=ps.ap[:, 0:n], scalar=modG.ap[:, 0, fc:fc + 1], in1=xo.ap[:, 0:n],
                                                              op0=ALU.mult, op1=ALU.add), reads=[ps, xo, (modG, 0)], writes=[res])
                P.dma("sp", x1T.ap[:, fc, t0:t0 + n], res.ap[:, 0:n], reads=[res], writes=[(x1T, (fc, t0))])
            linT(ring, ev_w_out, 0, D, oT, KC, [(0, 512), (512, 512)], ev_out, piece=128)
            P.barrier()
    X.final_src = x1T
    if "x1" in dbg and L0:
        d = dbg_tensor("x1", [128, KC, T_OWN])
        P.dma("sp", d.ap, x1T.ap, reads=[x1T], writes=[d])


    need_moe = (upto >= 2 and L0) or (upto >= 5 and mode in ("B", "F"))
    if need_moe:
        moe_wr_in = din("moe_wr", [2, D, 36])
        moe_br_in = din("moe_br", [2, 1, 36])
        moe_w_gate = din("moe_w_gate", [2, 32, D, 512])
        moe_w_up = din("moe_w_up", [2, 32, D, 512])
        moe_w_down = din("moe_w_down", [2, 32, 512, D])
    BIG = 1.0e30
    AX = mybir.AxisListType.X

    def moe_layer(l, xin, xout, n_exp=32):
        li = 2 * l + 1
        with ExitStack() as st:
            hT = salloc(st, f"hTm{l}", [128, KC, T_OWN], BF16)
            comb = salloc(st, f"comb{l}", [128, 8, 32], F32)
            wr = salloc(st, f"wr{l}", [128, KC, 36], F32)
            P.dma("sp", wr.ap, moe_wr_in[l].rearrange("(kc p) n -> p kc n", p=128), writes=[wr])
            br = salloc(st, f"br{l}", [128, 36], F32)
            P.dma("sp", br.ap, moe_br_in[l].broadcast_to([128, 36]), writes=[br])
            with ExitStack() as stp:
                hf_r = Ring(stp, f"hf{l}_", 2, [128, 512], F32)
                sm = {k: salloc(stp, f"rt{l}_{k}", [128, n], F32) for k, n in
                      (("lg", 36), ("gmax", 1), ("ngmax", 1), ("ge", 4), ("gsum", 1), ("gw", 1), ("ohg", 4), ("pen", 4), ("ml", 32),
                       ("m1", 1), ("oh1", 32), ("ml2", 32), ("m2", 1), ("oh2", 32), ("d", 1), ("ed", 1), ("den", 1), ("rden", 1),
                       ("w1", 1), ("w2", 1), ("tmp", 32))}
                X.ps_mod = 4

                def hook(tm, c, tb):
                    hf = hf_r.next()
                    P.op("act", lambda e: e.activation(out=hf.ap, in_=tm.ap, func=AF.Identity,
                                                       scale=modA.ap[:, li, c:c + 1], bias=modB.ap[:, li, c:c + 1]),
                         reads=[tm, (modA, li), (modB, li)], writes=[hf])
                    P.op("dve", lambda e: e.tensor_copy(out=hT.ap[:, c, tb * 512:(tb + 1) * 512], in_=hf.ap), reads=[hf], writes=[(hT, (c, tb))])
                    for tt in range(4):
                        P.op("pe", lambda e: e.matmul(PS[4 + tt].ap[:, 0:36], lhsT=hf.ap[:, tt * 128:(tt + 1) * 128], rhs=wr.ap[:, c, :],
                                                      start=(c == 0), stop=(c == KC - 1)), reads=[hf, wr], writes=[PS[4 + tt]])
                    if c == KC - 1:
                        for tt in range(4):
                            gt = tb * 4 + tt
                            S = sm

                            def dv(fn, r, w):
                                P.op("dve", fn, reads=r, writes=w)
                            dv(lambda e: e.tensor_tensor(out=S["lg"].ap, in0=PS[4 + tt].ap[:, 0:36], in1=br.ap, op=ALU.add), [PS[4 + tt], br], [S["lg"]])
                            gl = S["lg"].ap[:, 0:4]
                            el = S["lg"].ap[:, 4:36]
                            dv(lambda e: e.reduce_max(out=S["gmax"].ap, in_=gl, axis=AX), [S["lg"]], [S["gmax"]])
                            dv(lambda e: e.tensor_scalar(out=S["ngmax"].ap, in0=S["gmax"].ap, scalar1=-1.0, scalar2=None, op0=ALU.mult), [S["gmax"]], [S["ngmax"]])
                            P.op("act", lambda e: e.activation(out=S["ge"].ap, in_=gl, func=AF.Exp, bias=S["ngmax"].ap[:, 0:1], accum_out=S["gsum"].ap),
                                 reads=[S["lg"], S["ngmax"]], writes=[S["ge"], S["gsum"]])
                            dv(lambda e: e.reciprocal(out=S["gw"].ap, in_=S["gsum"].ap), [S["gsum"]], [S["gw"]])
                            dv(lambda e: e.tensor_scalar(out=S["ohg"].ap, in0=gl, scalar1=S["gmax"].ap[:, 0:1], scalar2=None, op0=ALU.is_equal), [S["lg"], S["gmax"]], [S["ohg"]])
                            dv(lambda e: e.tensor_scalar(out=S["pen"].ap, in0=S["ohg"].ap, scalar1=BIG, scalar2=-BIG, op0=ALU.mult, op1=ALU.add), [S["ohg"]], [S["pen"]])
                            for g in range(4):
                                dv(lambda e: e.tensor_scalar(out=S["ml"].ap[:, g * 8:(g + 1) * 8], in0=S["lg"].ap[:, 4 + g * 8:4 + (g + 1) * 8],
                                                             scalar1=S["pen"].ap[:, g:g + 1], scalar2=None, op0=ALU.add), [S["lg"], S["pen"]], [S["ml"]])
                            dv(lambda e: e.reduce_max(out=S["m1"].ap, in_=S["ml"].ap, axis=AX), [S["ml"]], [S["m1"]])
                            dv(lambda e: e.tensor_scalar(out=S["oh1"].ap, in0=S["ml"].ap, scalar1=S["m1"].ap[:, 0:1], scalar2=None, op0=ALU.is_equal), [S["ml"], S["m1"]], [S["oh1"]])
                            dv(lambda e: e.scalar_tensor_tensor(out=S["ml2"].ap, in0=S["oh1"].ap, scalar=-BIG, in1=S["ml"].ap, op0=ALU.mult, op1=ALU.add), [S["oh1"], S["ml"]], [S["ml2"]])
                            dv(lambda e: e.reduce_max(out=S["m2"].ap, in_=S["ml2"].ap, axis=AX), [S["ml2"]], [S["m2"]])
                            dv(lambda e: e.tensor_scalar(out=S["oh2"].ap, in0=S["ml2"].ap, scalar1=S["m2"].ap[:, 0:1], scalar2=None, op0=ALU.is_equal), [S["ml2"], S["m2"]], [S["oh2"]])
                            dv(lambda e: e.tensor_tensor(out=S["d"].ap, in0=S["m2"].ap, in1=S["m1"].ap, op=ALU.subtract), [S["m1"], S["m2"]], [S["d"]])
                            P.op("act", lambda e: e.activation(out=S["ed"].ap, in_=S["d"].ap, func=AF.Exp), reads=[S["d"]], writes=[S["ed"]])
                            dv(lambda e: e.tensor_scalar(out=S["den"].ap, in0=S["ed"].ap, scalar1=1.0, scalar2=None, op0=ALU.add), [S["ed"]], [S["den"]])
                            dv(lambda e: e.reciprocal(out=S["rden"].ap, in_=S["den"].ap), [S["den"]], [S["rden"]])
                            dv(lambda e: e.tensor_tensor(out=S["w1"].ap, in0=S["rden"].ap, in1=S["gw"].ap, op=ALU.mult), [S["rden"], S["gw"]], [S["w1"]])
                            dv(lambda e: e.tensor_tensor(out=S["w2"].ap, in0=S["w1"].ap, in1=S["ed"].ap, op=ALU.mult), [S["w1"], S["ed"]], [S["w2"]])
                            dv(lambda e: e.tensor_scalar(out=S["tmp"].ap, in0=S["oh1"].ap, scalar1=S["w1"].ap[:, 0:1], scalar2=None, op0=ALU.mult), [S["oh1"], S["w1"]], [S["tmp"]])
                            dv(lambda e: e.scalar_tensor_tensor(out=comb.ap[:, gt, :], in0=S["oh2"].ap, scalar=S["w2"].ap[:, 0:1], in1=S["tmp"].ap, op0=ALU.mult, op1=ALU.add),
                               [S["oh2"], S["w2"], S["tmp"]], [(comb, gt)])
                prologue(stp, li, xin, T_OWN, hT, hook=hook)
                X.ps_mod = 6
                P.barrier()
            if f"comb{l}" in dbg:
                d = dbg_tensor(f"comb{l}", [128, 8, 32])
                P.dma("sp", d.ap, comb.ap, reads=[comb], writes=[d])
            if f"hm{l}" in dbg:
                d = dbg_tensor(f"hm{l}", [128, KC, T_OWN], BF16)
                P.dma("sp", d.ap, hT.ap, reads=[hT], writes=[d])
            yacc = salloc(st, f"yacc{l}", [128, KC, T_OWN], F32)
            with ExitStack() as ste:
                mring = Ring(ste, f"mw{l}_", 12, [128, 2048])
                aT = salloc(ste, f"aT{l}", [128, 4, T_OWN], BF16)
                sg_r = Ring(ste, f"sg{l}_", 2, [128, 512], F32)
                t1_r = Ring(ste, f"t1{l}_", 2, [128, 512], F32)
                cs_r = Ring(ste, f"cs{l}_", 2, [128, T_OWN], F32)
                dg_r = Ring(ste, f"dg{l}_", 2, [128, 128], F32)
                def getw_m():
                    b = mring.next()
                    return b, b.ap.rearrange("p (k n) -> p k n", n=128)
                X.ps_mod = 8
                for ex in range(n_exp):
                    if mode == "F" and l == 0:
                        mod_run(2, 3, getw_m)
                        if X.mod_todo.get(2, 0) >= 48:
                            mod_run(3, 3, getw_m)
                    cs = cs_r.next()
                    for half in range(2):
                        psc = psn()
                        for q in range(4):
                            gt = half * 4 + q
                            dg = dg_r.next()
                            P.op("dve", lambda e: e.tensor_scalar(out=dg.ap, in0=ident_f, scalar1=comb.ap[:, gt, ex:ex + 1], scalar2=None, op0=ALU.mult),
                                 reads=[consts, (comb, gt)], writes=[dg])
                            P.op("pe", lambda e: e.matmul(psc.ap[:, q * 128:(q + 1) * 128], lhsT=ones_f, rhs=dg.ap, start=True, stop=True),
                                 reads=[consts, dg], writes=[psc])
                        P.op("act", lambda e: e.activation(out=cs.ap[:, half * 512:(half + 1) * 512], in_=psc.ap, func=AF.Copy), reads=[psc], writes=[(cs, half)])
                    wgv = moe_w_gate[l, ex].rearrange("(kc p) n -> p kc n", p=128)
                    wuv = moe_w_up[l, ex].rearrange("(kc p) n -> p kc n", p=128)
                    for hc in range(4):
                        wg = mring.next()
                        P.dma("pool", wg.ap.rearrange("p (k n) -> p k n", n=128), wgv[:, :, hc * 128:(hc + 1) * 128], writes=[wg])
                        wu = mring.next()
                        P.dma("pool", wu.ap.rearrange("p (k n) -> p k n", n=128), wuv[:, :, hc * 128:(hc + 1) * 128], writes=[wu])
                        for th in range(2):
                            psg = psn()
                            psu = psn()
                            for (psx, wx) in ((psg, wg), (psu, wu)):
                                for kc in range(KC):
                                    P.op("pe", lambda e: e.matmul(psx.ap, lhsT=wx.ap[:, kc * 128:(kc + 1) * 128], rhs=hT.ap[:, kc, th * 512:(th + 1) * 512],
                                                                  start=(kc == 0), stop=(kc == KC - 1)), reads=[wx, hT], writes=[psx])
                            sg = sg_r.next()
                            P.op("act", lambda e: e.activation(out=sg.ap, in_=psg.ap, func=AF.Silu), reads=[psg], writes=[sg])
                            t1 = t1_r.next()
                            P.op("dve", lambda e: e.tensor_tensor(out=t1.ap, in0=psu.ap, in1=sg.ap, op=ALU.mult), reads=[psu, sg], writes=[t1])
                            P.op("dve", lambda e: e.tensor_tensor(out=aT.ap[:, hc, th * 512:(th + 1) * 512], in0=t1.ap, in1=cs.ap[:, th * 512:(th + 1) * 512], op=ALU.mult),
                                 reads=[t1, (cs, th)], writes=[(aT, (hc, th))])
                    wds = []
                    for hc in range(4):
                        wd = mring.next()
                        P.dma("pool", wd.ap, moe_w_down[l, ex, hc * 128:(hc + 1) * 128, :], writes=[wd])
                        wds.append(wd)
                    for fc in range(KC):
                        for th in range(2):
                            ps = psn()
                            for hc in range(4):
                                P.op("pe", lambda e: e.matmul(ps.ap, lhsT=wds[hc].ap[:, fc * 128:(fc + 1) * 128], rhs=aT.ap[:, hc, th * 512:(th + 1) * 512],
                                                              start=(hc == 0), stop=(hc == 3)), reads=[wds[hc], (aT, (hc, th))], writes=[ps])
                            ysl = yacc.ap[:, fc, th * 512:(th + 1) * 512]
                            if ex == 0:
                                P.op("dve", lambda e: e.tensor_copy(out=ysl, in_=ps.ap), reads=[ps], writes=[(yacc, (fc, th))])
                            else:
                                P.op("dve", lambda e: e.tensor_tensor(out=ysl, in0=ps.ap, in1=ysl, op=ALU.add), reads=[ps, (yacc, (fc, th))], writes=[(yacc, (fc, th))])
                X.ps_mod = 6
                P.barrier()
            with ExitStack() as sto:
                xo_r = Ring(sto, f"mxo{l}_", 3, [128, 512], F32)
                res_r = Ring(sto, f"mres{l}_", 3, [128, 512], F32)
                for fc in range(KC):
                    for th in range(2):
                        xo = xo_r.next()
                        P.dma("sp", xo.ap, xin.ap[:, fc, th * 512:(th + 1) * 512], reads=[xin], writes=[xo])
                        res = res_r.next()
                        P.op("dve", lambda e: e.scalar_tensor_tensor(out=res.ap, in0=yacc.ap[:, fc, th * 512:(th + 1) * 512], scalar=modG.ap[:, li, fc:fc + 1],
                                                                      in1=xo.ap, op0=ALU.mult, op1=ALU.add), reads=[(yacc, (fc, th)), xo, (modG, li)], writes=[res])
                        P.dma("sp", xout.ap[:, fc, th * 512:(th + 1) * 512], res.ap, reads=[res], writes=[(xout, (fc, th))])
                P.barrier()

    if mode == "B" or SKIP_L0:
        x2T = Buf("x2T", None, din("x2T_in", [128, KC, T_OWN]))
    else:
        x2T = dscratch("x2T", [128, KC, T_OWN], out=(mode == "A"))
    if upto >= 2 and L0:
        if mode == "F":
            with ExitStack() as stq:
                rq = Ring(stq, "wq1r", 2, [128, KC, 128])
                mod_run(1, None, lambda: (lambda b: (b, b.ap))(rq.next()))
        moe_layer(0, x1T, x2T, n_exp=X_NEXP)
        if mode == "F":
            with ExitStack() as stq:
                rq = Ring(stq, "wq2r", 2, [128, KC, 128])
                mod_run(2, None, lambda: (lambda b: (b, b.ap))(rq.next()))
                mod_run(3, None, lambda: (lambda b: (b, b.ap))(rq.next()))
                P.barrier()
        X.final_src = x2T
    if "x2" in dbg and L0:
        d = dbg_tensor("x2", [128, KC, T_OWN])
        P.dma("sp", d.ap, x2T.ap, reads=[x2T], writes=[d])


    od_w_in = din("od_w_in", [D, 12288])
    od_w_out = din("od_w_out", [4096, D])
    ret_c_in = din("ret_consts", [128, 8 + 2048])
    GAM = [1.0 - 2.0 ** (-5.0 - h) for h in range(8)]
    KSC = 256 ** -0.5
    pos_own = pos_in[:, OWN0:T_ALL]
    toks_loc = [(0, 512), (512, 512)]

    def ret_common(st, cached=False):
        R = Ctx()
        R.hT = salloc(st, "hT1", [128, KC, T_OWN], BF16)
        R.rc = salloc(st, "retc", [128, 8 + 2048], F32)
        P.dma("sp", R.rc.ap, ret_c_in, writes=[R.rc])
        if cached:
            P.dma("sp", R.hT.ap, c_hT.ap, reads=[c_hT], writes=[R.hT])
            R.sin = salloc(st, "sinr2", [128, T_OWN], F32)
            R.cos = salloc(st, "cosr2", [128, T_OWN], F32)
            P.dma("sp", R.cos.ap, c_cs.ap[0], reads=[(c_cs, 0)], writes=[R.cos])
            P.dma("sp", R.sin.ap, c_cs.ap[1], reads=[(c_cs, 1)], writes=[R.sin])
        else:
            with ExitStack() as stp:
                prologue(stp, 2, x2T, T_OWN, R.hT)
                P.barrier()
            with ExitStack() as st_t:
                R.cos, R.sin = rope_tables(st, st_t, "r", 128, 1, T_OWN, pos_own)
                P.barrier()
        R.ring = Ring(st, "wr1_", 4, [128, KC, 128])
        R.ring512 = Ring(st, "wr1b_", 2, [128, KC, 512])
        R.kT = salloc(st, "rkT", [128, 2, T_OWN], BF16)
        R.kd = salloc(st, "rkd", [128, 8, 256], BF16)
        R.v = salloc(st, "rv", [128, 8, 512], BF16)
        R.S = salloc(st, "rS", [128, 2, 512], F32)
        R.ropetmp = Ring(st, "rt1_", 4, [128, 512], F32)
        return R

    def ret_proj_rope(R, col0, outT, scale):
        wv = od_w_in.rearrange("(kc p) n -> p kc n", p=128)
        w1 = wload(R.ring, wv[:, :, col0:col0 + 128], KC, 128)
        w2 = wload(R.ring, wv[:, :, col0 + 128:col0 + 256], KC, 128)
        for (t0, n) in toks_loc:
            ps1 = psn()
            ps2 = psn()
            for (psx, wx) in ((ps1, w1), (ps2, w2)):
                for kc in range(KC):
                    P.op("pe", lambda e: e.matmul(psx.ap[:, 0:n], lhsT=wx.ap[:, kc, :], rhs=R.hT.ap[:, kc, t0:t0 + n], start=(kc == 0), stop=(kc == KC - 1)),
                         reads=[wx, R.hT], writes=[psx])
            rope_apply(R.ropetmp, ps1.ap[:, 0:n], ps2.ap[:, 0:n], [ps1, ps2], R.cos.ap[:, t0:t0 + n], R.sin.ap[:, t0:t0 + n],
                       outT.ap[:, 0, t0:t0 + n], outT.ap[:, 1, t0:t0 + n], [(outT, t0)], 128, n, scale=scale)

    def ret_kv(R, h):
        ret_proj_rope(R, 2048 + h * 256, R.kT, KSC)

        def ev_v(ps, col, cw, t0):
            P.op("act", lambda e: e.activation(out=R.v.ap[:, t0 // 128, col:col + cw], in_=ps.ap[:, 0:cw], func=AF.Copy),
                 reads=[ps], writes=[(R.v, t0 // 128)])
        linTok(R.ring512, od_w_in, 4096 + h * 512, 512, R.hT, KC, list(range(0, T_OWN, 128)), ev_v, piece=512)
        for gt in range(8):
            for dc in range(2):
                ps = psn()
                P.op("pe", lambda e: e.matmul(ps.ap[:, 0:128], lhsT=R.kT.ap[:, dc, gt * 128:(gt + 1) * 128], rhs=ident_b.ap, start=True, stop=True),
                     reads=[R.kT, ident_b], writes=[ps])
                P.op("dve", lambda e: e.tensor_scalar(out=R.kd.ap[:, gt, dc * 128:(dc + 1) * 128], in0=ps.ap[:, 0:128], scalar1=R.rc.ap[:, h:h + 1], scalar2=None, op0=ALU.mult),
                     reads=[ps, R.rc], writes=[(R.kd, gt)])

    def ret_state_update(R, h, gt, first):
        for dc in range(2):
            ps = psn()
            P.op("pe", lambda e: e.matmul(ps.ap, lhsT=R.kd.ap[:, gt, dc * 128:(dc + 1) * 128], rhs=R.v.ap[:, gt, :], start=True, stop=True),
                 reads=[(R.kd, gt), (R.v, gt)], writes=[ps])
            if first:
                P.op("dve", lambda e: e.tensor_copy(out=R.S.ap[:, dc, :], in_=ps.ap), reads=[ps], writes=[(R.S, dc)])
            else:
                P.op("dve", lambda e: e.scalar_tensor_tensor(out=R.S.ap[:, dc, :], in0=R.S.ap[:, dc, :], scalar=GAM[h] ** 128, in1=ps.ap, op0=ALU.mult, op1=ALU.add),
                     reads=[ps, (R.S, dc)], writes=[(R.S, dc)])

    state_out = dscratch("state_out", [8, 128, 2, 512], F32, out=(mode == "A"))
    x3T = dscratch("x3T", [128, KC, T_OWN])
    x4T = dscratch("x4T", [128, KC, T_OWN])

    CACHE = (mode == "F")
    if CACHE:
        c_kT = dscratch("c_kT", [8, 128, 2, T_OWN], BF16)
        c_kd = dscratch("c_kd", [8, 128, 8, 256], BF16)
        c_v = dscratch("c_v", [8, 128, 8, 512], BF16)
        c_hT = dscratch("c_hT", [128, KC, T_OWN], BF16)
        c_cs = dscratch("c_cs", [2, 128, T_OWN], F32)
    if upto >= 3 and mode in ("A", "F"):
        with ExitStack() as st:
            R = ret_common(st)
            if CACHE:
                P.dma("sp", c_hT.ap, R.hT.ap, reads=[R.hT], writes=[c_hT])
                P.dma("sp", c_cs.ap[0], R.cos.ap, reads=[R.cos], writes=[(c_cs, 0)])
                P.dma("sp", c_cs.ap[1], R.sin.ap, reads=[R.sin], writes=[(c_cs, 1)])
            for h in range(NHEADS):
                ret_kv(R, h)
                if CACHE:
                    P.dma("sp", c_kT.ap[h], R.kT.ap, reads=[R.kT], writes=[(c_kT, h)])
                    P.dma("sp", c_kd.ap[h], R.kd.ap, reads=[R.kd], writes=[(c_kd, h)])
                    P.dma("sp", c_v.ap[h], R.v.ap, reads=[R.v], writes=[(c_v, h)])
                for gt in range(8):
                    ret_state_update(R, h, gt, gt == 0)
                P.dma("sp", state_out.ap[h], R.S.ap, reads=[R.S], writes=[(state_out, h)])
            P.barrier()

    if mode == "B":
        state_in = Buf("state_in", None, din("state_in", [8, 128, 2, 512]))
    elif mode == "F":
        state_in_h = []
        for h in range(NHEADS):
            gt_ = nc.dram_tensor(f"state_gath{h}", [256, 1024], F32, kind="Internal")
            gath = Buf(f"state_gath{h}", gt_, gt_.ap())
            if NO_CC:
                P.dma("sp", gath.ap[0:128, :], state_out.ap[h].rearrange("p d e -> p (d e)"), reads=[(state_out, h)], writes=[gath])
            else:
                P.op("pool", lambda e: e.collective_compute("AllGather", ALU.bypass, replica_groups=[[0, 1], [2, 3], [4, 5], [6, 7]],
                                                            ins=[state_out.ap[h].rearrange("p d e -> p (d e)")], outs=[gath.ap]),
                     reads=[(state_out, h)], writes=[gath])
                P.flag_last("pool")
            state_in_h.append(gath)
    og_s = dscratch("og_s", [128, 32, T_OWN], BF16)
    if upto >= 4 and mode in ("B", "F"):
        with ExitStack() as st:
            R = ret_common(st, cached=CACHE)
            ogh_r = Ring(st, "rogh_", 2, [128, 4, T_OWN], BF16)
            qT = salloc(st, "rqT", [128, 2, T_OWN], BF16)
            qdT = salloc(st, "rqdT", [128, 2, T_OWN], BF16)
            gT = salloc(st, "rgT", [128, 4, T_OWN], BF16)
            Sb = salloc(st, "rSb", [128, 8, 1024], BF16)
            sc_r = Ring(st, "rsc_", 3, [128, 128], BF16)
            sq_r = Ring(st, "rsq_", 3, [128, 512], F32)
            rs_r = Ring(st, "rrs_", 2, [128, 128], F32)
            rstd_r = Ring(st, "rrstd_", 2, [128, 128], F32)
            tmp_r = Ring(st, "rtmp_", 3, [128, 128], F32)
            for h in range(NHEADS):
                if CACHE:
                    P.dma("sp", R.kT.ap, c_kT.ap[h], reads=[(c_kT, h)], writes=[R.kT])
                    P.dma("sp", R.kd.ap, c_kd.ap[h], reads=[(c_kd, h)], writes=[R.kd])
                    P.dma("sp", R.v.ap, c_v.ap[h], reads=[(c_v, h)], writes=[R.v])
                else:
                    ret_kv(R, h)
                ret_proj_rope(R, h * 256, qT, 1.0)
                for gt in range(8):
                    for dc in range(2):
                        P.op("dve", lambda e: e.tensor_tensor(out=qdT.ap[:, dc, gt * 128:(gt + 1) * 128], in0=qT.ap[:, dc, gt * 128:(gt + 1) * 128],
                                                               in1=R.rc.ap[:, 8 + h * 128:8 + (h + 1) * 128], op=ALU.mult), reads=[qT, R.rc], writes=[(qdT, gt)])

                def ev_g(ps, col, m, t0, n):
                    P.op("act", lambda e: e.activation(out=gT.ap[:, col // 128, t0:t0 + n], in_=ps.ap[:, 0:n], func=AF.Silu),
                         reads=[ps], writes=[(gT, (col // 128, t0))])
                linT(R.ring, od_w_in, 8192 + h * 512, 512, R.hT, KC, toks_loc, ev_g, piece=128)
                if mode == "F":
                    P.dma("sp", R.S.ap, state_in_h[h].ap[0:128, :].rearrange("p (d e) -> p d e", e=512), reads=[state_in_h[h]], writes=[R.S])
                else:
                    P.dma("sp", R.S.ap, state_in.ap[h], reads=[(state_in, h)], writes=[R.S])
                P.op("dve", lambda e: e.tensor_scalar(out=R.S.ap, in0=R.S.ap, scalar1=pvalid.ap[:, 0:1], scalar2=None, op0=ALU.mult), reads=[R.S, pvalid], writes=[R.S])
                for gt in range(8):
                    P.op("act", lambda e: e.activation(out=Sb.ap[:, gt, :], in_=R.S.ap.rearrange("p d e -> p (d e)"), func=AF.Copy), reads=[R.S], writes=[(Sb, gt)])
                    if gt < 7:
                        ret_state_update(R, h, gt, False)
                ogh = ogh_r.next()
                cst = [dict() for _ in range(8)]

                def r_t1(gt):
                    ts = slice(gt * 128, (gt + 1) * 128)
                    pss = psn()
                    for dc in range(2):
                        P.op("pe", lambda e: e.matmul(pss.ap[:, 0:128], lhsT=R.kT.ap[:, dc, ts], rhs=qT.ap[:, dc, ts], start=(dc == 0), stop=(dc == 1)),
                             reads=[R.kT, qT], writes=[pss])
                    sc = sc_r.next()
                    P.op("dve", lambda e: e.tensor_tensor(out=sc.ap, in0=pss.ap[:, 0:128], in1=R.rc.ap[:, 8 + 1024 + h * 128:8 + 1024 + (h + 1) * 128], op=ALU.mult),
                         reads=[pss, R.rc], writes=[sc])
                    P.flag_last("dve")
                    cst[gt]["sc"] = sc

                def r_t2(gt):
                    ts = slice(gt * 128, (gt + 1) * 128)
                    sc = cst[gt]["sc"]
                    pso = psn()
                    for ec in range(4):
                        es = slice(ec * 128, (ec + 1) * 128)
                        P.op("pe", lambda e: e.matmul(pso.ap[:, es], lhsT=R.v.ap[:, gt, es], rhs=sc.ap, start=True, stop=False), reads=[(R.v, gt), sc], writes=[pso])
                        P.op("pe", lambda e: e.matmul(pso.ap[:, es], lhsT=Sb.ap[:, gt, ec * 128:(ec + 1) * 128], rhs=qdT.ap[:, 0, ts], start=False, stop=False),
                             reads=[(Sb, gt), (qdT, gt)], writes=[pso])
                        P.op("pe", lambda e: e.matmul(pso.ap[:, es], lhsT=Sb.ap[:, gt, 512 + ec * 128:512 + (ec + 1) * 128], rhs=qdT.ap[:, 1, ts], start=False, stop=True),
                             reads=[(Sb, gt), (qdT, gt)], writes=[pso])
                    sq = sq_r.next()
                    P.op("act", lambda e: e.activation(out=sq.ap, in_=pso.ap, func=AF.Square), reads=[pso], writes=[sq])
                    P.flag_last("act")
                    cst[gt].update(pso=pso, sq=sq)

                def r_t3(gt):
                    ts = slice(gt * 128, (gt + 1) * 128)
                    pso, sq = cst[gt]["pso"], cst[gt]["sq"]
                    psq = psn()
                    for ec in range(4):
                        P.op("pe", lambda e: e.matmul(psq.ap[:, 0:128], lhsT=ones_f, rhs=sq.ap[:, ec * 128:(ec + 1) * 128], start=(ec == 0), stop=(ec == 3)),
                             reads=[sq, consts], writes=[psq])
                    rs = rs_r.next()
                    P.op("act", lambda e: e.activation(out=rs.ap, in_=psq.ap[:, 0:128], func=AF.Sqrt, scale=1.0 / 512, bias=EPS), reads=[psq], writes=[rs])
                    rstd = rstd_r.next()
                    P.op("dve", lambda e: e.reciprocal(out=rstd.ap, in_=rs.ap), reads=[rs], writes=[rstd])
                    for ec in range(4):
                        tm = tmp_r.next()
                        P.op("dve", lambda e: e.tensor_tensor(out=tm.ap, in0=pso.ap[:, ec * 128:(ec + 1) * 128], in1=rstd.ap, op=ALU.mult), reads=[pso, rstd], writes=[tm])
                        P.op("dve", lambda e: e.tensor_tensor(out=ogh.ap[:, ec, ts], in0=tm.ap, in1=gT.ap[:, ec, ts], op=ALU.mult),
                             reads=[tm, gT], writes=[(ogh, (ec, gt))])
                for step in range(8 + 2):
                    if step < 8:
                        r_t1(step)
                    if 0 <= step - 1 < 8:
                        r_t2(step - 1)
                    if 0 <= step - 2 < 8:
                        r_t3(step - 2)
                P.dma("sp", og_s.ap[:, h * 4:(h + 1) * 4, :], ogh.ap, reads=[ogh], writes=[(og_s, h)])
            P.barrier()
        with ExitStack() as st:
            ogT = salloc(st, "ogT", [128, 32, T_OWN], BF16)
            P.dma("sp", ogT.ap, og_s.ap, reads=[og_s], writes=[ogT])
            ring32 = Ring(st, "wr32_", 3, [128, 32, 128])
            xo_r = Ring(st, "rxo_", 3, [128, 512], F32)
            res_r = Ring(st, "rres_", 3, [128, 512], F32)

            def ev_out1(ps, col, m, t0, n):
                fc = col // 128
                xo = xo_r.next()
                P.dma("sp", xo.ap[:, 0:n], x2T.ap[:, fc, t0:t0 + n], reads=[x2T], writes=[xo])
                res = res_r.next()
                P.op("dve", lambda e: e.scalar_tensor_tensor(out=res.ap[:, 0:n], in0=ps.ap[:, 0:n], scalar=modG.ap[:, 2, fc:fc + 1], in1=xo.ap[:, 0:n],
                                                              op0=ALU.mult, op1=ALU.add), reads=[ps, xo, (modG, 2)], writes=[res])
                P.dma("sp", x3T.ap[:, fc, t0:t0 + n], res.ap[:, 0:n], reads=[res], writes=[(x3T, (fc, t0))])
            linT(ring32, od_w_out, 0, D, ogT, 32, toks_loc, ev_out1, piece=128)
            P.barrier()
    if "x3" in dbg:
        d = dbg_tensor("x3", [128, KC, T_OWN])
        P.dma("sp", d.ap, x3T.ap, reads=[x3T], writes=[d])
    if upto >= 5 and mode in ("B", "F"):
        moe_layer(1, x3T, x4T, n_exp=X_NEXP)
        X.final_src = x4T
    if "x4" in dbg:
        d = dbg_tensor("x4", [128, KC, T_OWN])
        P.dma("sp", d.ap, x4T.ap, reads=[x4T], writes=[d])
    if upto >= 6 and mode in ("B", "F"):
        with ExitStack() as st:
            xs_r = Ring(st, "fxs_", 2, [128, KC, 512], F32)
            fo_r = Ring(st, "fo_", 3, [128, 512], F32)
            cur = {}

            def getx(c, tb):
                if c == 0:
                    xs = xs_r.next()
                    P.dma("sp", xs.ap, x4T.ap[:, :, tb * 512:(tb + 1) * 512], reads=[x4T], writes=[xs])
                    cur["xs"] = xs
                return cur["xs"].ap[:, c, :], [cur["xs"]]

            def emit(tm, c, tb):
                fo = fo_r.next()
                P.op("act", lambda e: e.activation(out=fo.ap, in_=tm.ap, func=AF.Copy, scale=norms.ap[:, 4, c:c + 1]), reads=[tm, norms], writes=[fo])
                P.dma("sp", outT.ap[:, c, tb * 512:(tb + 1) * 512], fo.ap, reads=[fo], writes=[(outT, (c, tb))])
            rmsnorm_T(st, "fin", getx, KC, T_OWN // 512, D, emit)
            P.barrier()
        X.final_src = None
    X.P = P
    X.nc = nc
    X.dbg_out = dbg_out
    return X, locals()


def _fm(v):
    v = np.asarray(v, np.float32)
    n = v.shape[-1] // 128
    return np.ascontiguousarray(v.reshape(n, 128).T)


def _xT(xb):
    T = xb.shape[0]
    return np.ascontiguousarray(xb.T.reshape(KC, 128, T).transpose(1, 0, 2))


def shared_inputs(I):
    S = {}
    S["consts"] = host_consts()
    S["w_mod"] = np.ascontiguousarray(np.stack([I["w_mod_mix"][0], I["w_mod_ffn"][0], I["w_mod_mix"][1], I["w_mod_ffn"][1]]))
    bm = [I["b_mod_mix"][0], I["b_mod_ffn"][0], I["b_mod_mix"][1], I["b_mod_ffn"][1]]
    S["b_mod"] = np.ascontiguousarray(np.stack([_fm(b) for b in bm], axis=1))
    nm = [I["norm_mix"][0], I["norm_ffn"][0], I["norm_mix"][1], I["norm_ffn"][1], I["final_norm"]]
    S["norms"] = np.ascontiguousarray(np.stack([_fm(b) for b in nm], axis=1))
    S["ev_w_in"] = np.ascontiguousarray(I["ev_w_in"][0])
    S["ev_q_norm"] = _fm(I["ev_q_norm"][0])
    S["ev_w_q_up"] = np.ascontiguousarray(I["ev_w_q_up"][0])
    S["ev_kv_norm"] = _fm(I["ev_kv_norm"][0])
    S["ev_w_kv_up"] = np.ascontiguousarray(I["ev_w_kv_up"][0])
    S["ev_w_out"] = np.ascontiguousarray(I["ev_w_out"][0])
    invf = np.zeros((128, 2), np.float32)
    p = np.arange(128)
    invf[:, 0] = np.exp(-math.log(10000.0) * (p % 32).astype(np.float32) / np.float32(32)).astype(np.float32)
    invf[:, 1] = np.exp(-math.log(10000.0) * p.astype(np.float32) / np.float32(128)).astype(np.float32)
    S["invf"] = invf
    rc = np.zeros((128, 8 + 2048), np.float64)
    idx = np.arange(128, dtype=np.float64)
    for h in range(8):
        lg = np.log1p(-(2.0 ** (-5.0 - h)))
        rc[:, h] = np.exp((127.0 - idx) * lg)
        rc[:, 8 + h * 128:8 + (h + 1) * 128] = np.exp((idx + 1.0) * lg)[None, :]
        rel = idx[None, :] - idx[:, None]
        rc[:, 8 + 1024 + h * 128:8 + 1024 + (h + 1) * 128] = np.where(rel >= 0, np.exp(np.maximum(rel, 0.0) * lg), 0.0)
    S["ret_consts"] = rc.astype(np.float32)
    S["od_w_in"] = np.ascontiguousarray(I["od_w_in"][0])
    S["od_w_out"] = np.ascontiguousarray(I["od_w_out"][0])
    S["moe_wr"] = np.ascontiguousarray(np.concatenate([I["moe_w_group"], I["moe_w_expert"]], axis=-1))
    S["moe_br"] = np.ascontiguousarray(np.concatenate([I["moe_b_group"], I["moe_b_expert"]], axis=-1)[:, None, :])
    S["moe_w_gate"] = np.asarray(I["moe_w_gate"])
    S["moe_w_up"] = np.asarray(I["moe_w_up"])
    S["moe_w_down"] = np.asarray(I["moe_w_down"])
    return S


def core_inputs(I, core):
    b, hf = core // 2, core % 2
    x = np.asarray(I["x"][b], np.float32)
    pos = np.asarray(I["positions"][b], np.int32)
    M = {}
    if hf == 0:
        xa = np.concatenate([np.zeros((T_OWN, D), np.float32), x[:T_OWN]], axis=0)
        pa = np.concatenate([np.zeros((T_OWN,), np.int32), pos[:T_OWN]])
    else:
        xa = x
        pa = pos
    M["xT_all"] = _xT(xa)
    M["pos_all"] = np.ascontiguousarray(pa.reshape(1, T_ALL))
    M["pvalid"] = np.full((128, 1), float(hf), np.float32)
    M["cond"] = _fm(I["c"][b])
    return M


def _tok(a):
    a = np.asarray(a)
    p, c, t = a.shape
    return np.ascontiguousarray(a.transpose(2, 1, 0).reshape(t, c * p))


FUSED = True


def kernel(**inputs):
    I = {k: np.asarray(v) for k, v in inputs.items()}
    n = 8
    S = shared_inputs(I)
    cores = [core_inputs(I, c) for c in range(n)]
    if FUSED:
        X, _ = build(mode="F")
        X.P.finish()
        maps = [{k: (cores[c][k] if k in cores[c] else S[k]) for k in X.in_names} for c in range(n)]
        res = run_bass_kernel_spmd(X.nc, maps, core_ids=list(range(n)))
        outs = [res.results[c]["outT"] for c in range(n)]
    else:
        XA, _ = build(upto=3, mode="A")
        XA.P.finish()
        maps = [{k: (cores[c][k] if k in cores[c] else S[k]) for k in XA.in_names} for c in range(n)]
        resA = run_bass_kernel_spmd(XA.nc, maps, core_ids=list(range(n)))
        XB, _ = build(mode="B")
        XB.P.finish()
        maps = []
        for c in range(n):
            M = dict(cores[c])
            M["x2T_in"] = np.asarray(resA.results[c]["x2T"])
            M["state_in"] = (np.asarray(resA.results[c - 1]["state_out"]) if c % 2 == 1
                             else np.zeros((8, 128, 2, 512), np.float32))
            maps.append({k: (M[k] if k in M else S[k]) for k in XB.in_names})
        resB = run_bass_kernel_spmd(XB.nc, maps, core_ids=list(range(n)))
        outs = [resB.results[c]["outT"] for c in range(n)]
    out = np.zeros((4, 2048, 2048), np.float32)
    for c in range(n):
        b, hf = c // 2, c % 2
        out[b, hf * T_OWN:(hf + 1) * T_OWN, :] = _tok(outs[c])
    return out
```
